# Optimizing a Trainium2 kernel written in Bass

```python
import math
import jax, jax.numpy as jnp
from jax import lax
import numpy as np


D_MODEL = 2048
BATCH = 4
SEQ = 8192
DEPTH = 1

Q_BLOCK = 128
HEAD_DIM = 128
SB_HEADS = 8
SB_WIDTH = SB_HEADS * HEAD_DIM
NSA_HEADS = 8
NSA_KV_GROUPS = 2
NSA_HPG = NSA_HEADS // NSA_KV_GROUPS
NSA_WIDTH = NSA_HEADS * HEAD_DIM
NSA_KV_WIDTH = NSA_KV_GROUPS * HEAD_DIM
CMP_LEN = 32
CMP_STRIDE = 16
CMP_HIDDEN = 256
SLC_BLOCK = 64
SLC_TOPN = 16
WINDOW = 512
REL_BUCKETS = 32
REL_MAX_DIST = 128
N_GROUPS = 8
EXPERTS_PER_GROUP = 8
N_EXPERTS = N_GROUPS * EXPERTS_PER_GROUP
TOPK_IN_GROUP = 2
EXPERT_HIDDEN = 1408
MOE_BLOCK = 128
RMS_EPS = 1e-6
FORCED_SCORE = 1e4
IN_SIZES = (SB_WIDTH, SB_WIDTH, SB_WIDTH, NSA_WIDTH,
            NSA_KV_WIDTH, NSA_KV_WIDTH, NSA_KV_WIDTH, NSA_KV_WIDTH, NSA_KV_WIDTH, NSA_KV_WIDTH,
            3 * NSA_HEADS, D_MODEL, D_MODEL)
IN_COLS = 3 * SB_WIDTH + NSA_WIDTH + 6 * NSA_KV_WIDTH + 3 * NSA_HEADS + 2 * D_MODEL

kernel_name = 'hybrid_stickbreak_nsa_hmoe'


def rmsnorm(x, g):
    xf = x.astype(jnp.float32)
    y = xf * lax.rsqrt(jnp.mean(xf * xf, axis=-1, keepdims=True) + RMS_EPS)
    return (y * g.astype(jnp.float32)).astype(x.dtype)


def modulate(xn, shift, scale):
    return xn * (1.0 + scale[:, None, :]) + shift[:, None, :]


def t5_bucket(dist):
    n = jnp.maximum(dist, 0)
    max_exact = REL_BUCKETS // 2
    nf = jnp.maximum(n, 1).astype(jnp.float32)
    large = max_exact + (jnp.log(nf / max_exact) / math.log(REL_MAX_DIST / max_exact)
                         * (REL_BUCKETS - max_exact)).astype(jnp.int32)
    large = jnp.minimum(large, REL_BUCKETS - 1)
    return jnp.where(n < max_exact, n, large)


def masked_softmax(s, mask):
    s = jnp.where(mask, s.astype(jnp.float32), -jnp.inf)
    m = jnp.max(s, axis=-1, keepdims=True)
    m = jnp.where(jnp.isfinite(m), m, 0.0)
    e = jnp.where(mask, jnp.exp(s - m), 0.0)
    return e / jnp.maximum(jnp.sum(e, axis=-1, keepdims=True), 1e-30)


def stick_breaking_attention(q, k, v):
    B, H, S, dh = q.shape
    scale = dh ** -0.5
    spos = jnp.arange(S)

    def block(i):
        t0 = i * Q_BLOCK
        qb = lax.dynamic_slice_in_dim(q, t0, Q_BLOCK, axis=2)
        z = jnp.einsum('bhqd,bhkd->bhqk', qb, k).astype(jnp.float32) * scale
        tpos = t0 + jnp.arange(Q_BLOCK)
        causal = spos[None, :] < tpos[:, None]
        log_1m = jnp.where(causal, jax.nn.log_sigmoid(-z), 0.0)
        after = lax.cumsum(log_1m, axis=3, reverse=True) - log_1m
        a = jnp.where(causal, jnp.exp(jax.nn.log_sigmoid(z) + after), 0.0)
        return jnp.einsum('bhqk,bhkd->bhqd', a.astype(v.dtype), v)

    o = lax.map(block, jnp.arange(S // Q_BLOCK))
    return o.transpose(1, 0, 3, 2, 4).reshape(B, S, H * dh)


def compress_blocks(kv, pe, w1, w2):
    B, G, S, dh = kv.shape
    chunks = kv.reshape(B, G, S // CMP_STRIDE, CMP_STRIDE, dh)
    blocks = jnp.concatenate([chunks[:, :, :-1], chunks[:, :, 1:]], axis=3) + pe
    flat = blocks.reshape(B, G, S // CMP_STRIDE - 1, CMP_LEN * dh)
    return jax.nn.gelu(flat @ w1) @ w2


def nsa_attention(q, k_cmp, v_cmp, k_slc, v_slc, k_swa, v_swa, gates, rel_bias):
    B, G, HPG, S, dh = q.shape
    scale = dh ** -0.5
    n_cmp = k_cmp.shape[2]
    n_slc = S // SLC_BLOCK
    n_sel = min(SLC_TOPN, n_slc)
    tbl = rel_bias.astype(jnp.float32).reshape(REL_BUCKETS, G, HPG)
    tbl_t = tbl.transpose(1, 2, 0)
    cmp_start = jnp.arange(n_cmp) * CMP_STRIDE
    cmp_end = cmp_start + CMP_LEN - 1
    slc_start = jnp.arange(n_slc) * SLC_BLOCK
    lo = jnp.maximum(cmp_start[:, None], slc_start[None, :])
    hi = jnp.minimum(cmp_start[:, None] + CMP_LEN, slc_start[None, :] + SLC_BLOCK)
    overlap = jnp.clip(hi - lo, 0).astype(jnp.float32) / CMP_LEN
    ks_blk = k_slc.reshape(B, G, n_slc, SLC_BLOCK, dh)
    vs_blk = v_slc.reshape(B, G, n_slc, SLC_BLOCK, dh)
    pad = ((0, 0), (0, 0), (WINDOW, 0), (0, 0))
    kw_pad = jnp.pad(k_swa, pad)
    vw_pad = jnp.pad(v_swa, pad)
    b_ix = jnp.arange(B)[:, None, None, None]
    g_ix = jnp.arange(G)[None, :, None, None]
    g6 = jnp.arange(G).reshape(1, G, 1, 1, 1, 1)
    h6 = jnp.arange(HPG).reshape(1, 1, HPG, 1, 1, 1)
    slc_offs = jnp.arange(SLC_BLOCK)
    blk_ids = jnp.arange(n_slc)

    def block(i):
        t0 = i * Q_BLOCK
        tpos = t0 + jnp.arange(Q_BLOCK)
        qb = lax.dynamic_slice_in_dim(q, t0, Q_BLOCK, axis=3)
        dist_c = tpos[:, None] - cmp_end[None, :]
        bias_c = tbl[t5_bucket(dist_c)].transpose(2, 3, 0, 1)
        s_c = jnp.einsum('bghqd,bgnd->bghqn', qb, k_cmp).astype(jnp.float32) * scale + bias_c
        p_c = masked_softmax(s_c, dist_c >= 0)
        o_c = jnp.einsum('bghqn,bgnd->bghqd', p_c.astype(v_cmp.dtype), v_cmp)
        imp = jnp.einsum('bghqn,nj->bgqj', p_c, overlap)
        cur = tpos // SLC_BLOCK
        forced = ((blk_ids[None, :] == 0) | (blk_ids[None, :] == cur[:, None])
                  | (blk_ids[None, :] == cur[:, None] - 1))
        valid_b = slc_start[None, :] <= tpos[:, None]
        score = jnp.where(forced, FORCED_SCORE, jnp.where(valid_b, imp, -jnp.inf))
        _, idx = lax.top_k(score, n_sel)
        k_sel = ks_blk[b_ix, g_ix, idx]
        v_sel = vs_blk[b_ix, g_ix, idx]
        pos = idx[..., None] * SLC_BLOCK + slc_offs
        dist_s = tpos[:, None, None] - pos
        bias_s = tbl_t[g6, h6, t5_bucket(dist_s)[:, :, None]]
        s_s = jnp.einsum('bghqd,bgqkld->bghqkl', qb, k_sel).astype(jnp.float32) * scale + bias_s
        n_tok = n_sel * SLC_BLOCK
        p_s = masked_softmax(s_s.reshape(B, G, HPG, Q_BLOCK, n_tok),
                             (dist_s >= 0)[:, :, None].reshape(B, G, 1, Q_BLOCK, n_tok))
        o_s = jnp.einsum('bghqn,bgqnd->bghqd', p_s.astype(v_slc.dtype),
                         v_sel.reshape(B, G, Q_BLOCK, n_tok, dh))
        kw = lax.dynamic_slice_in_dim(kw_pad, t0, WINDOW + Q_BLOCK, axis=2)
        vw = lax.dynamic_slice_in_dim(vw_pad, t0, WINDOW + Q_BLOCK, axis=2)
        wpos = t0 - WINDOW + jnp.arange(WINDOW + Q_BLOCK)
        dist_w = tpos[:, None] - wpos[None, :]
        valid_w = (dist_w >= 0) & (dist_w < WINDOW) & (wpos[None, :] >= 0)
        bias_w = tbl[t5_bucket(dist_w)].transpose(2, 3, 0, 1)
        s_w = jnp.einsum('bghqd,bgkd->bghqk', qb, kw).astype(jnp.float32) * scale + bias_w
        p_w = masked_softmax(s_w, valid_w)
        o_w = jnp.einsum('bghqk,bgkd->bghqd', p_w.astype(v_swa.dtype), vw)
        g = lax.dynamic_slice_in_dim(gates, t0, Q_BLOCK, axis=3)
        return g[..., 0:1] * o_c + g[..., 1:2] * o_s + g[..., 2:3] * o_w

    o = lax.map(block, jnp.arange(S // Q_BLOCK))
    return o.transpose(1, 0, 4, 2, 3, 5).reshape(B, S, G * HPG * dh)


def hierarchical_moe(h, w_grp, b_grp, w_exp, b_exp, w1, w3, w2):
    B, S, D = h.shape
    T = B * S
    xt = h.reshape(T, D)
    lg = (xt @ w_grp).astype(jnp.float32) + b_grp.astype(jnp.float32)
    pg = jax.nn.softmax(lg, axis=-1)
    pg_sel, g_sel = lax.top_k(pg, 1)
    le = ((xt @ w_exp).astype(jnp.float32) + b_exp.astype(jnp.float32)).reshape(T, N_GROUPS, EXPERTS_PER_GROUP)
    le = jnp.take_along_axis(le, g_sel[:, :, None], axis=1)[:, 0]
    pe = jax.nn.softmax(le, axis=-1)
    w_top, i_top = lax.top_k(pe, TOPK_IN_GROUP)
    w_top = w_top / jnp.sum(w_top, axis=-1, keepdims=True) * pg_sel
    e_id = g_sel * EXPERTS_PER_GROUP + i_top
    N = T * TOPK_IN_GROUP
    e_flat = e_id.reshape(N)
    tok = jnp.repeat(jnp.arange(T, dtype=jnp.int32), TOPK_IN_GROUP)
    w_flat = w_top.reshape(N)
    order = jnp.argsort(e_flat)
    e_sorted = e_flat[order]
    counts = jnp.bincount(e_flat, length=N_EXPERTS)
    start = jnp.cumsum(counts) - counts
    padded = (counts + MOE_BLOCK - 1) // MOE_BLOCK * MOE_BLOCK
    pend = jnp.cumsum(padded)
    pstart = pend - padded
    dest = pstart[e_sorted] + (jnp.arange(N) - start[e_sorted])
    n_rows = (N + N_EXPERTS * (MOE_BLOCK - 1) + MOE_BLOCK - 1) // MOE_BLOCK * MOE_BLOCK
    n_blk = n_rows // MOE_BLOCK
    row_tok = jnp.full((n_rows,), T, jnp.int32).at[dest].set(tok[order])
    row_w = jnp.zeros((n_rows,), jnp.float32).at[dest].set(w_flat[order])
    blk_e = jnp.minimum(jnp.searchsorted(pend, jnp.arange(n_blk) * MOE_BLOCK, side='right'), N_EXPERTS - 1)
    x_pad = jnp.concatenate([xt, jnp.zeros((1, D), xt.dtype)], axis=0)

    def run(args):
        toks, wts, e = args
        xs = x_pad[toks]
        hid = jax.nn.silu(xs @ w1[e]) * (xs @ w3[e])
        return (hid @ w2[e]) * wts[:, None].astype(xs.dtype)

    y = lax.map(run, (row_tok.reshape(n_blk, MOE_BLOCK), row_w.reshape(n_blk, MOE_BLOCK), blk_e))
    out = jnp.zeros((T + 1, D), h.dtype).at[row_tok].add(y.reshape(n_rows, D).astype(h.dtype))[:T]
    return out.reshape(B, S, D)


def setup_inputs(seed: int = 0) -> dict:
    key = jax.random.key(seed)
    ks = jax.random.split(key, 26)
    f32 = jnp.float32
    L = DEPTH
    D = D_MODEL

    def nrm(k, shape, scale):
        return jax.random.normal(k, shape, f32) * scale

    flat_cmp = CMP_LEN * HEAD_DIM
    return {
        'x': nrm(ks[0], (BATCH, SEQ, D), 1.0),
        'c': nrm(ks[1], (BATCH, D), 1.0),
        'ada_w': nrm(ks[2], (L, D, 6 * D), 0.5 * D ** -0.5),
        'ada_b': nrm(ks[3], (L, 6 * D), 0.02),
        'norm1_g': 1.0 + nrm(ks[4], (L, D), 0.02),
        'norm2_g': 1.0 + nrm(ks[5], (L, D), 0.02),
        'normf_g': 1.0 + nrm(ks[6], (D,), 0.02),
        'w_in': nrm(ks[7], (L, D, IN_COLS), D ** -0.5),
        'rel_bias': nrm(ks[8], (REL_BUCKETS, NSA_HEADS), 0.5),
        'cmp_pe_k': nrm(ks[9], (L, CMP_LEN, HEAD_DIM), 0.1),
        'cmp_w1_k': nrm(ks[10], (L, flat_cmp, CMP_HIDDEN), flat_cmp ** -0.5),
        'cmp_w2_k': nrm(ks[11], (L, CMP_HIDDEN, HEAD_DIM), CMP_HIDDEN ** -0.5),
        'cmp_pe_v': nrm(ks[12], (L, CMP_LEN, HEAD_DIM), 0.1),
        'cmp_w1_v': nrm(ks[13], (L, flat_cmp, CMP_HIDDEN), flat_cmp ** -0.5),
        'cmp_w2_v': nrm(ks[14], (L, CMP_HIDDEN, HEAD_DIM), CMP_HIDDEN ** -0.5),
        'w_branch_a': nrm(ks[15], (L, SB_WIDTH, D), SB_WIDTH ** -0.5),
        'w_branch_b': nrm(ks[16], (L, NSA_WIDTH, D), NSA_WIDTH ** -0.5),
        'w_out': nrm(ks[17], (L, D, D), D ** -0.5),
        'router_w_grp': nrm(ks[18], (L, D, N_GROUPS), D ** -0.5),
        'router_b_grp': nrm(ks[19], (L, N_GROUPS), 0.01),
        'router_w_exp': nrm(ks[20], (L, D, N_EXPERTS), D ** -0.5),
        'router_b_exp': nrm(ks[21], (L, N_EXPERTS), 0.01),
        'expert_w1': nrm(ks[22], (L, N_EXPERTS, D, EXPERT_HIDDEN), D ** -0.5),
        'expert_w3': nrm(ks[23], (L, N_EXPERTS, D, EXPERT_HIDDEN), D ** -0.5),
        'expert_w2': nrm(ks[24], (L, N_EXPERTS, EXPERT_HIDDEN, D), EXPERT_HIDDEN ** -0.5),
    }


def reference(x, c, ada_w, ada_b, norm1_g, norm2_g, normf_g, w_in, rel_bias,
              cmp_pe_k, cmp_w1_k, cmp_w2_k, cmp_pe_v, cmp_w1_v, cmp_w2_v,
              w_branch_a, w_branch_b, w_out, router_w_grp, router_b_grp,
              router_w_exp, router_b_exp, expert_w1, expert_w3, expert_w2):
    B, S, D = x.shape
    G, HPG, dh = NSA_KV_GROUPS, NSA_HPG, HEAD_DIM
    offsets = []
    acc = 0
    for size in IN_SIZES[:-1]:
        acc += size
        offsets.append(acc)
    c_act = jax.nn.silu(c)
    for layer in range(DEPTH):
        mod = c_act @ ada_w[layer] + ada_b[layer]
        sh1, sc1, gt1, sh2, sc2, gt2 = jnp.split(mod, 6, axis=-1)
        xn = modulate(rmsnorm(x, norm1_g[layer]), sh1, sc1)
        proj = xn @ w_in[layer]
        (qa, ka, va, qb, kcp, vcp, ksl, vsl, ksw, vsw,
         g_nsa, g_a, g_b) = jnp.split(proj, offsets, axis=-1)
        heads = lambda t: t.reshape(B, S, SB_HEADS, dh).transpose(0, 2, 1, 3)
        o_a = stick_breaking_attention(heads(qa), heads(ka), heads(va))
        kvh = lambda t: t.reshape(B, S, G, dh).transpose(0, 2, 1, 3)
        q_nsa = qb.reshape(B, S, G, HPG, dh).transpose(0, 2, 3, 1, 4)
        k_c = compress_blocks(kvh(kcp), cmp_pe_k[layer], cmp_w1_k[layer], cmp_w2_k[layer])
        v_c = compress_blocks(kvh(vcp), cmp_pe_v[layer], cmp_w1_v[layer], cmp_w2_v[layer])
        gates = jax.nn.sigmoid(g_nsa).reshape(B, S, G, HPG, 3).transpose(0, 2, 3, 1, 4)
        o_b = nsa_attention(q_nsa, k_c, v_c, kvh(ksl), kvh(vsl), kvh(ksw), kvh(vsw),
                            gates, rel_bias)
        merged = (jax.nn.sigmoid(g_a) * (o_a @ w_branch_a[layer])
                  + jax.nn.sigmoid(g_b) * (o_b @ w_branch_b[layer]))
        x = x + gt1[:, None, :] * (merged @ w_out[layer])
        hn = modulate(rmsnorm(x, norm2_g[layer]), sh2, sc2)
        x = x + gt2[:, None, :] * hierarchical_moe(
            hn, router_w_grp[layer], router_b_grp[layer], router_w_exp[layer], router_b_exp[layer],
            expert_w1[layer], expert_w3[layer], expert_w2[layer])
    return rmsnorm(x, normf_g)
```

```python
import contextlib
import numpy as np
import ml_dtypes
import concourse.bass as bass
import concourse.mybir as mybir
from concourse.bass_utils import run_bass_kernel_spmd

F32 = mybir.dt.float32
BF16 = mybir.dt.bfloat16
I32 = mybir.dt.int32
U32 = mybir.dt.uint32
AF = mybir.ActivationFunctionType
ALU = mybir.AluOpType
AX = mybir.AxisListType

D = 2048
NDC = 16
DH = 128
IN_COLS = 9752
NEXP = 64
EH = 1408
NFC = 11
SCALE = 128 ** -0.5
NEG = -30000.0


class Buf:
    __slots__ = ("name", "last_w", "readers")

    def __init__(self, name=""):
        self.name = name
        self.last_w = None
        self.readers = []


class Sched:
    def __init__(self, nc, stack):
        self.nc = nc
        self.stack = stack
        self.engs = {"pe": nc.tensor, "act": nc.scalar, "dve": nc.vector, "pool": nc.gpsimd, "sp": nc.sync}
        self.sems = {}
        self.cnt = {}
        self.seen = {e: {} for e in self.engs}
        for e in ("pe", "act", "dve", "pool"):
            self.sems[e] = stack.enter_context(nc.semaphore("s_" + e))
            self.cnt[e] = 0
        self.dsems = {}
        self.ninst = 0

    def dma_sem(self, key):
        if key not in self.dsems:
            self.dsems[key] = self.stack.enter_context(self.nc.semaphore("d_%s" % (key,)))
            self.cnt[("d", key)] = 0
        return key

    def _semobj(self, k):
        return self.sems[k] if k in self.sems else self.dsems[k[1]]

    def issue(self, e, fn, reads=(), writes=(), dma=None, pe_acc=False):
        eng = self.engs[e]
        need = {}
        deps = []
        for b in reads:
            if b.last_w is not None:
                deps.append(b.last_w)
        for b in writes:
            if b.last_w is not None:
                deps.append(b.last_w)
            deps.extend(b.readers)
        for (k, v) in deps:
            if pe_acc and k == "pe" and e == "pe":
                continue
            if need.get(k, 0) < v:
                need[k] = v
        for k, v in need.items():
            if self.seen[e].get(k, 0) < v:
                eng.wait_ge(self._semobj(k), v)
                self.seen[e][k] = v
        if dma is not None and isinstance(dma, str) and dma[0] == "u" and dma[1:].isdigit():
            self.dma_sem(dma)
            kk0 = ("d", dma)
            if self.cnt[kk0] > 0 and self.seen[e].get(kk0, 0) < self.cnt[kk0]:
                eng.wait_ge(self.dsems[dma], self.cnt[kk0])
                self.seen[e][kk0] = self.cnt[kk0]
        inst = fn(eng)
        self.ninst += 1
        if dma is not None:
            self.dma_sem(dma)
            kk = ("d", dma)
            self.cnt[kk] += 16
            inst.then_inc(self.dsems[dma], 16)
            op = (kk, self.cnt[kk])
        else:
            self.cnt[e] += 1
            inst.then_inc(self.sems[e], 1)
            op = (e, self.cnt[e])
        for b in writes:
            b.last_w = op
            b.readers = []
        for b in reads:
            if b not in writes:
                b.readers.append(op)
                if len(b.readers) > 48:
                    mx = {}
                    for (k, v) in b.readers:
                        if mx.get(k, 0) < v:
                            mx[k] = v
                    b.readers = list(mx.items())
        return op

    def wait_bufs(self, e, bufs):
        eng = self.engs[e]
        for b in bufs:
            for dep in ([b.last_w] if b.last_w is not None else []) + list(b.readers):
                k, v = dep
                if self.seen[e].get(k, 0) < v:
                    eng.wait_ge(self._semobj(k), v)
                    self.seen[e][k] = v


class T:
    n = 0

    def __init__(self, st, nc, name, shape, dtype, psum=False):
        T.n += 1
        nm = "t%d_%s" % (T.n, name)
        if psum:
            self.t = st.enter_context(nc.psum_tensor(nm, shape, dtype))
        else:
            self.t = st.enter_context(nc.sbuf_tensor(nm, shape, dtype))
        self.b = Buf(name)

    def __getitem__(self, idx):
        return self.t[idx]


class DR:
    def __init__(self, nc, name, shape, dtype, kind="Internal"):
        self.h = nc.dram_tensor(name, list(shape), dtype, kind=kind)
        self.ap = self.h.ap()
        self.bufs = {}
        self.name = name

    def buf(self, key=0):
        if key not in self.bufs:
            self.bufs[key] = Buf("%s_%s" % (self.name, key))
        return self.bufs[key]

    def allbufs(self):
        return list(self.bufs.values())


def own_tiles(S, half):
    NT = S // 512
    return [t for t in range(NT) if ((t % 4) in (0, 3)) == (half == 0)]


class Ctx:
    pass


def build_program(S, CAP, taps=(), upto=99, nhalf=1):
    nc = bass.Bass("TRN2", target_bir_lowering=False)
    g = Ctx()
    g.nc = nc
    g.S, g.CAP = S, CAP
    g.SV = S + 512
    g.NTV = g.SV // 512
    g.NKB = g.SV // 128
    g.NQT = (g.NTV - 1) // 2
    g.NQ = g.NQT * 512
    g.NQB = g.NQ // 128
    g.NCV = g.SV // 16 - 1
    g.NSV = g.SV // 64
    g.taps = set(taps)
    g._u = [0]

    def uniq():
        g._u[0] += 1
        return 'u%d' % (g._u[0] % 16)

    g.uniq = uniq
    g.upto = upto

    g.sfx = ""

    def dr(name, shape, dtype, kind=None):
        if kind is None:
            kind = "ExternalOutput" if name in g.taps else "Internal"
        return DR(nc, name + g.sfx, shape, dtype, kind)

    g.dr = dr
    inp = {}

    def ein(name, shape, dtype=F32):
        inp[name] = DR(nc, name, shape, dtype, "ExternalInput")
        return inp[name]

    g.inp = inp
    sfxs = [""] if nhalf == 1 else ["_h0", "_h1"]
    for sf in sfxs:
        ein("xv" + sf, [g.SV, D])
        ein("c_f32" + sf, [128, 1024])
        ein("c_bf" + sf, [128, 4096], BF16)
    ein("cT", [128, 16])
    ein("ada_w", [D, 6 * D])
    ein("ada_bT", [128, 96])
    ein("g1T", [128, 16])
    ein("g2row", [1, D])
    ein("gfrow", [1, D])
    ein("w_in", [D, IN_COLS])
    ein("relb", [32, 8])
    ein("pe_kT", [128, 32])
    ein("pe_vT", [128, 32])
    ein("cw1k", [4096, 256])
    ein("cw2k", [256, 128])
    ein("cw1v", [4096, 256])
    ein("cw2v", [256, 128])
    ein("wba", [1024, D])
    ein("wbb", [1024, D])
    ein("wout", [D, D])
    ein("wr", [D, 72])
    ein("br", [1, 72])
    if upto >= 7:
        ein("ew1", [NEXP, NFC, 128, NDC * 128])
        ein("ew3", [NEXP, NFC, 128, NDC * 128])
        ein("ew2", [NEXP, EH, D])
    ein("c_idf", [128, 128])
    ein("c_idb", [128, 128], BF16)
    ein("c_exp", [128, g.SV], BF16)
    ein("c_oh", [128, OH_W], BF16)
    g.outs = [DR(nc, "out" + sf, [g.NQ, D], F32, "ExternalOutput") for sf in sfxs]
    g.tapdr = []
    names = list(inp.keys())

    with contextlib.ExitStack() as st:
        g.sch = Sched(nc, st)
        for hi, sf in enumerate(sfxs):
            g.sfx = sf
            g.out = g.outs[hi]
            for nm in ("xv", "c_f32", "c_bf"):
                inp[nm] = inp[nm + sf]
            with contextlib.ExitStack() as hst:
                g.st = hst
                phase0(g)
                if upto >= 1:
                    phase1(g)
                if upto >= 3:
                    phase2(g)
                if upto >= 4:
                    phase3(g)
                if upto >= 5:
                    phase4(g)
                if upto >= 6:
                    phase5(g)
                if upto >= 7:
                    phase6(g)
                    phase7(g)
                barrier(g)
        finish(g)
    nc._inp_names = names
    return nc


def finish(g):
    sch = g.sch
    outs = list(g.outs) + [d for d in g.tapdr]
    for d in outs:
        sch.wait_bufs("sp", d.allbufs())


def phase0(g):
    nc, sch, st = g.nc, g.sch, g.st
    S = sch.issue
    g.idf = T(st, nc, "idf", [128, 128], F32)
    g.idb = T(st, nc, "idb", [128, 128], BF16)
    g.cf = T(st, nc, "cf", [128, 1024], F32)
    g.cb = T(st, nc, "cb", [128, 4096], BF16)
    S("sp", lambda e: e.dma_start(out=g.idf[:], in_=g.inp["c_idf"].ap), writes=[g.idf.b], dma=g.uniq())
    S("sp", lambda e: e.dma_start(out=g.idb[:], in_=g.inp["c_idb"].ap), writes=[g.idb.b], dma=g.uniq())
    S("sp", lambda e: e.dma_start(out=g.cf[:], in_=g.inp["c_f32"].ap), writes=[g.cf.b], dma=g.uniq())
    S("sp", lambda e: e.dma_start(out=g.cb[:], in_=g.inp["c_bf"].ap), writes=[g.cb.b], dma=g.uniq())
    g.modT = T(st, nc, "modT", [128, 96], F32)
    g.a1T = T(st, nc, "a1T", [128, 16], F32)
    g.modD = g.dr("modD", [96, 128], F32)
    if "modD" in g.taps:
        g.tapdr.append(g.modD)
    with contextlib.ExitStack() as ls:
        cT = T(ls, nc, "cT", [128, 16], F32)
        cact = T(ls, nc, "cact", [128, 16], F32)
        abT = T(ls, nc, "abT", [128, 96], F32)
        g1T = T(ls, nc, "g1Tt", [128, 16], F32)
        stg = [T(ls, nc, "adaw%d" % i, [128, 16, 512], F32) for i in range(2)]
        mps = T(ls, nc, "modps", [128, 96], F32, psum=True)
        tps = T(ls, nc, "modtp", [128, 128], F32, psum=True)
        mrow = T(ls, nc, "mrow", [96, 128], F32)
        S("sp", lambda e: e.dma_start(out=cT[:], in_=g.inp["cT"].ap), writes=[cT.b], dma=g.uniq())
        S("sp", lambda e: e.dma_start(out=abT[:], in_=g.inp["ada_bT"].ap), writes=[abT.b], dma=g.uniq())
        S("sp", lambda e: e.dma_start(out=g1T[:], in_=g.inp["g1T"].ap), writes=[g1T.b], dma=g.uniq())
        S("act", lambda e: e.activation(out=cact[:], in_=cT[:], func=AF.Silu), reads=[cT.b], writes=[cact.b])
        aw = g.inp["ada_w"].ap
        for slab in range(24):
            sg = stg[slab % 2]
            src = aw[:, slab * 512:(slab + 1) * 512].rearrange("(dc p) j -> p dc j", p=128)
            S("sp", lambda e, sg=sg, src=src: e.dma_start(out=sg[:], in_=src), writes=[sg.b], dma="adaw%d" % (slab % 2))
            for jl in range(4):
                jc = slab * 4 + jl
                for dc in range(16):
                    S("pe", lambda e, sg=sg, jl=jl, jc=jc, dc=dc: e.matmul(
                        mps[:, jc:jc + 1], sg[:, dc, jl * 128:(jl + 1) * 128], cact[:, dc:dc + 1],
                        start=(dc == 0), stop=(dc == 15)),
                      reads=[sg.b, cact.b], writes=[mps.b], pe_acc=True)
        S("dve", lambda e: e.tensor_tensor(out=g.modT[:], in0=mps[:], in1=abT[:], op=ALU.add),
          reads=[mps.b, abT.b], writes=[g.modT.b])
        S("dve", lambda e: e.tensor_scalar(out=g.a1T[:], in0=g.modT[:, 16:32], scalar1=1.0, scalar2=None, op0=ALU.add),
          reads=[g.modT.b], writes=[g.a1T.b])
        S("dve", lambda e: e.tensor_tensor(out=g.a1T[:], in0=g.a1T[:], in1=g1T[:], op=ALU.mult),
          reads=[g.a1T.b, g1T.b], writes=[g.a1T.b])
        S("pe", lambda e: e.transpose(tps[0:96, :], g.modT[:], g.idf[:]), reads=[g.modT.b, g.idf.b], writes=[tps.b])
        S("act", lambda e: e.copy(mrow[:], tps[0:96, :]), reads=[tps.b], writes=[mrow.b])
        S("sp", lambda e: e.dma_start(out=g.modD.ap, in_=mrow[:]), reads=[mrow.b], writes=[g.modD.buf()], dma=g.uniq())


def load_rows(g, st, names):
    nc, sch = g.nc, g.sch
    S = sch.issue
    md = g.modD.ap

    def rowsrc(k):
        return md[k * 16:(k + 1) * 16, :].rearrange("(o a) b -> o (a b)", o=1).partition_broadcast(128)

    idx = {"gt1": 2, "sh2": 3, "a2": 4, "gt2": 5}
    for nm in names:
        if nm == "gf":
            S("sp", lambda e: e.dma_start(out=g.rows["gf"][:], in_=g.inp["gfrow"].ap.partition_broadcast(128)), writes=[g.rows["gf"].b], dma=g.uniq())
        else:
            S("sp", lambda e, nm=nm: e.dma_start(out=g.rows[nm][:], in_=rowsrc(idx[nm])), reads=[g.modD.buf()], writes=[g.rows[nm].b], dma=g.uniq())
    if "a2" in names:
        with contextlib.ExitStack() as ls:
            g2r = T(ls, nc, "g2r", [128, D], F32)
            S("sp", lambda e: e.dma_start(out=g2r[:], in_=g.inp["g2row"].ap.partition_broadcast(128)), writes=[g2r.b], dma=g.uniq())
            S("dve", lambda e: e.tensor_scalar(out=g.rows["a2"][:], in0=g.rows["a2"][:], scalar1=1.0, scalar2=None, op0=ALU.add),
              reads=[g.rows["a2"].b], writes=[g.rows["a2"].b])
            S("dve", lambda e: e.tensor_tensor(out=g.rows["a2"][:], in0=g.rows["a2"][:], in1=g2r[:], op=ALU.mult),
              reads=[g.rows["a2"].b, g2r.b], writes=[g.rows["a2"].b])
            barrier(g)


def barrier(g):
    sch = g.sch
    for e, eng in sch.engs.items():
        for k in list(sch.sems.keys()):
            v = sch.cnt[k]
            if v > 0 and sch.seen[e].get(k, 0) < v:
                eng.wait_ge(sch.sems[k], v)
                sch.seen[e][k] = v
        for key in list(sch.dsems.keys()):
            kk = ("d", key)
            v = sch.cnt[kk]
            if v > 0 and sch.seen[e].get(kk, 0) < v:
                eng.wait_ge(sch.dsems[key], v)
                sch.seen[e][kk] = v


def phase1(g):
    nc, sch, st = g.nc, g.sch, g.st
    S = sch.issue
    SV, NTV, NKB, NQT, NQ, NQB = g.SV, g.NTV, g.NKB, g.NQT, g.NQ, g.NQB
    g.XNT = g.dr("XNT", [NTV, 128, 16 * 512], BF16)
    xv = g.inp["xv"].ap
    sh1 = g.modT
    with contextlib.ExitStack() as ls:
        xt = [T(ls, nc, "xt%d" % i, [128, D], F32) for i in range(2)]
        junk = T(ls, nc, "junk", [128, D], BF16)
        yb = [T(ls, nc, "yb%d" % i, [128, D], BF16) for i in range(2)]
        ss = [T(ls, nc, "ss%d" % i, [128, 1], F32) for i in range(2)]
        xn = [T(ls, nc, "xn%d" % i, [128, 16, 512], BF16) for i in range(2)]
        tp = [T(ls, nc, "tp%d" % i, [128, 2048], BF16, psum=True) for i in range(2)]
        for tb in range(NKB):
            Tt, sub = tb // 4, tb % 4
            x_, y_, s_, p_ = xt[tb % 2], yb[tb % 2], ss[tb % 2], tp[tb % 2]
            xo = xn[Tt % 2]
            S("sp", lambda e, x_=x_, tb=tb: e.dma_start(out=x_[:], in_=xv[tb * 128:(tb + 1) * 128, :]),
              writes=[x_.b], dma="xt%d" % (tb % 2))
            S("act", lambda e, x_=x_, s_=s_: e.activation(out=junk[:], in_=x_[:], func=AF.Square, accum_out=s_[:]),
              reads=[x_.b], writes=[junk.b, s_.b])
            S("dve", lambda e, s_=s_: e.tensor_scalar(out=s_[:], in0=s_[:], scalar1=1.0 / D, scalar2=1e-6, op0=ALU.mult, op1=ALU.add),
              reads=[s_.b], writes=[s_.b])
            S("act", lambda e, s_=s_: e.activation(out=s_[:], in_=s_[:], func=AF.Sqrt), reads=[s_.b], writes=[s_.b])
            S("dve", lambda e, s_=s_: e.reciprocal(out=s_[:], in_=s_[:]), reads=[s_.b], writes=[s_.b])
            S("act", lambda e, x_=x_, y_=y_, s_=s_: e.activation(out=y_[:], in_=x_[:], func=AF.Identity, scale=s_[:, 0:1]),
              reads=[x_.b, s_.b], writes=[y_.b])
            for dc in range(16):
                S("pe", lambda e, y_=y_, p_=p_, dc=dc: e.transpose(p_[:, dc * 128:(dc + 1) * 128], y_[:, dc * 128:(dc + 1) * 128], g.idb[:]),
                  reads=[y_.b, g.idb.b], writes=[p_.b], pe_acc=True)
            for dc in range(16):
                eng = "act" if dc % 2 == 0 else "dve"
                if eng == "act":
                    S("act", lambda e, p_=p_, xo=xo, dc=dc, sub=sub: e.activation(
                        out=xo[:, dc, sub * 128:(sub + 1) * 128], in_=p_[:, dc * 128:(dc + 1) * 128], func=AF.Identity,
                        scale=g.a1T[:, dc:dc + 1], bias=sh1[:, dc:dc + 1]),
                      reads=[p_.b, g.a1T.b, sh1.b], writes=[xo.b])
                else:
                    S("dve", lambda e, p_=p_, xo=xo, dc=dc, sub=sub: e.tensor_scalar(
                        out=xo[:, dc, sub * 128:(sub + 1) * 128], in0=p_[:, dc * 128:(dc + 1) * 128],
                        scalar1=g.a1T[:, dc:dc + 1], scalar2=sh1[:, dc:dc + 1], op0=ALU.mult, op1=ALU.add),
                      reads=[p_.b, g.a1T.b, sh1.b], writes=[xo.b])
            if sub == 3:
                S("sp", lambda e, xo=xo, Tt=Tt: e.dma_start(out=g.XNT.ap[Tt], in_=xo[:].rearrange("p a b -> p (a b)")),
                  reads=[xo.b], writes=[g.XNT.buf(Tt)], dma="xnt_st")
        barrier(g)
    if "XNT" in g.taps:
        g.tapdr.append(g.XNT)
    if g.upto < 2:
        return
    dr = g.dr
    g.QAT = dr("QAT", [8, 128, NQ], BF16)
    g.KAT = dr("KAT", [8, 128, SV], BF16)
    g.VA = dr("VA", [8, 128, NKB, 128], BF16)
    g.QBT = dr("QBT", [8, 128, NQ], BF16)
    g.CPT = dr("CPT", [4, 128, SV], BF16)
    g.KSLT = dr("KSLT", [2, 128, SV], BF16)
    g.VSL = dr("VSL", [2, 128, NKB, 128], BF16)
    g.KSWT = dr("KSWT", [2, 128, SV], BF16)
    g.VSW = dr("VSW", [2, 128, NKB, 128], BF16)
    g.GN = dr("GN", [NQB, 128, 24], F32)
    g.GAT = dr("GAT", [16, 128, NQ], BF16)
    g.GBT = dr("GBT", [16, 128, NQ], BF16)
    for nm in ("QAT", "KAT", "VA", "QBT", "CPT", "KSLT", "VSL", "KSWT", "VSW", "GN", "GAT", "GBT"):
        if nm in g.taps:
            g.tapdr.append(getattr(g, nm))

    def fm_store(dst, i0, own):
        def f(stg, ncc, tk):
            t0 = tk * 512
            return lambda e: e.dma_start(out=dst.ap[i0:i0 + ncc, :, t0:t0 + 512].rearrange("h p t -> p h t"), in_=stg[:, 0:ncc, :])
        return f

    def tmv_store(dst, i0):
        def f(stg, nh, tk):
            return [(lambda e, h=h: e.dma_start(
                out=dst.ap[i0 + h, :, 4 * tk:4 * tk + 4, :],
                in_=stg[:, :, h * 128:(h + 1) * 128])) for h in range(nh)]
        return f

    jobs = []
    for j in range(2):
        jobs.append((j * 512, 512, "fm", True, False, fm_store(g.QAT, 4 * j, True), g.QAT))
    for j in range(2):
        jobs.append((1024 + j * 512, 512, "fm", False, False, fm_store(g.KAT, 4 * j, False), g.KAT))
    for j in range(2):
        jobs.append((2048 + j * 512, 512, "tm", False, False, tmv_store(g.VA, 4 * j), g.VA))
    for j in range(2):
        jobs.append((3072 + j * 512, 512, "fm", True, False, fm_store(g.QBT, 4 * j, True), g.QBT))
    jobs.append((4096, 512, "fm", False, False, fm_store(g.CPT, 0, False), g.CPT))
    jobs.append((4608, 256, "fm", False, False, fm_store(g.KSLT, 0, False), g.KSLT))
    jobs.append((4864, 256, "tm", False, False, tmv_store(g.VSL, 0), g.VSL))
    jobs.append((5120, 256, "fm", False, False, fm_store(g.KSWT, 0, False), g.KSWT))
    jobs.append((5376, 256, "tm", False, False, tmv_store(g.VSW, 0), g.VSW))
    jobs.append((5632, 24, "gn", True, True, None, g.GN))
    for j in range(4):
        jobs.append((5656 + j * 512, 512, "fm", True, True, fm_store(g.GAT, 4 * j, True), g.GAT))
    for j in range(4):
        jobs.append((7704 + j * 512, 512, "fm", True, True, fm_store(g.GBT, 4 * j, True), g.GBT))

    win = g.inp["w_in"].ap
    with contextlib.ExitStack() as ls:
        wst = [T(ls, nc, "wst%d" % i, [128, 16, 512], F32) for i in range(2)]
        wb = [T(ls, nc, "wb%d" % i, [128, 16, 512], BF16) for i in range(2)]
        xn = [T(ls, nc, "xnl%d" % i, [128, 16, 512], BF16) for i in range(2)]
        ostg = [T(ls, nc, "ostg%d" % i, [128, 4, 512], BF16) for i in range(2)]
        gstg = [T(ls, nc, "gstg%d" % i, [128, 4, 24], F32) for i in range(2)]
        ps = [T(ls, nc, "pps%d" % i, [128, 512], F32, psum=True) for i in range(8)]
        nload = 0
        nout = 0
        for ji, (c0, w, kind, own, sig, store, dst) in enumerate(jobs):
            ws_, wb_ = wst[ji % 2], wb[ji % 2]
            src = win[:, c0:c0 + w].rearrange("(dc p) j -> p dc j", p=128)
            S("sp", lambda e, ws_=ws_, src=src, w=w: e.dma_start(out=ws_[:, :, 0:w], in_=src), writes=[ws_.b], dma="wst%d" % (ji % 2))
            for (eng, d0, d1) in (("act", 0, 6), ("dve", 6, 11), ("pool", 11, 16)):
                if eng == "act":
                    S("act", lambda e, ws_=ws_, wb_=wb_, d0=d0, d1=d1, w=w: e.copy(wb_[:, d0:d1, 0:w], ws_[:, d0:d1, 0:w]),
                      reads=[ws_.b], writes=[wb_.b])
                else:
                    S(eng, lambda e, ws_=ws_, wb_=wb_, d0=d0, d1=d1, w=w: e.tensor_copy(wb_[:, d0:d1, 0:w], ws_[:, d0:d1, 0:w]),
                      reads=[ws_.b], writes=[wb_.b])
            tiles = [(k, 2 * k + 1) for k in range(NQT)] if own else [(t, t) for t in range(NTV)]
            for (tk, tv) in tiles:
                xl = xn[nload % 2]
                S("sp", lambda e, xl=xl, tv=tv: e.dma_start(out=xl[:].rearrange("p a b -> p (a b)"), in_=g.XNT.ap[tv]),
                  reads=[g.XNT.buf(tv)], writes=[xl.b], dma="xnl%d" % (nload % 2))
                nload += 1
                pb = [ps[(nout % 2) * 4 + i] for i in range(4)]
                og = ostg[nout % 2]
                gg = gstg[nout % 2]
                nout += 1
                if kind == "fm":
                    ncc = w // 128
                    for cc in range(ncc):
                        for dc in range(16):
                            S("pe", lambda e, cc=cc, dc=dc, xl=xl, wb_=wb_, pb=pb: e.matmul(
                                pb[cc][:, :], wb_[:, dc, cc * 128:(cc + 1) * 128], xl[:, dc, :], start=(dc == 0), stop=(dc == 15)),
                              reads=[wb_.b, xl.b], writes=[pb[cc].b], pe_acc=True)
                    for cc in range(ncc):
                        if sig:
                            S("act", lambda e, cc=cc, og=og, pb=pb: e.activation(out=og[:, cc, :], in_=pb[cc][:, :], func=AF.Sigmoid),
                              reads=[pb[cc].b], writes=[og.b])
                        elif cc % 2 == 0:
                            S("act", lambda e, cc=cc, og=og, pb=pb: e.copy(og[:, cc, :], pb[cc][:, :]), reads=[pb[cc].b], writes=[og.b])
                        else:
                            S("dve", lambda e, cc=cc, og=og, pb=pb: e.tensor_copy(og[:, cc, :], pb[cc][:, :]), reads=[pb[cc].b], writes=[og.b])
                    S("sp", store(og, ncc, tk), reads=[og.b], writes=[dst.buf(tk)], dma="pst")
                elif kind == "tm":
                    for sub in range(4):
                        for dc in range(16):
                            S("pe", lambda e, sub=sub, dc=dc, xl=xl, wb_=wb_, pb=pb, w=w: e.matmul(
                                pb[sub][:, 0:w], xl[:, dc, sub * 128:(sub + 1) * 128], wb_[:, dc, 0:w], start=(dc == 0), stop=(dc == 15)),
                              reads=[wb_.b, xl.b], writes=[pb[sub].b], pe_acc=True)
                    for sub in range(4):
                        if sub % 2 == 0:
                            S("act", lambda e, sub=sub, og=og, pb=pb, w=w: e.copy(og[:, sub, 0:w], pb[sub][:, 0:w]), reads=[pb[sub].b], writes=[og.b])
                        else:
                            S("dve", lambda e, sub=sub, og=og, pb=pb, w=w: e.tensor_copy(og[:, sub, 0:w], pb[sub][:, 0:w]), reads=[pb[sub].b], writes=[og.b])
                    for fn in store(og, w // 128, tk):
                        S("sp", fn, reads=[og.b], writes=[dst.buf(tk)], dma="pst")
                else:
                    for sub in range(4):
                        for dc in range(16):
                            S("pe", lambda e, sub=sub, dc=dc, xl=xl, wb_=wb_, pb=pb, w=w: e.matmul(
                                pb[sub][:, 0:w], xl[:, dc, sub * 128:(sub + 1) * 128], wb_[:, dc, 0:w], start=(dc == 0), stop=(dc == 15)),
                              reads=[wb_.b, xl.b], writes=[pb[sub].b], pe_acc=True)
                    for sub in range(4):
                        S("act", lambda e, sub=sub, gg=gg, pb=pb, w=w: e.activation(out=gg[:, sub, :], in_=pb[sub][:, 0:w], func=AF.Sigmoid),
                          reads=[pb[sub].b], writes=[gg.b])
                    S("sp", lambda e, gg=gg, tk=tk: e.dma_start(out=g.GN.ap[4 * tk:4 * tk + 4].rearrange("s p c -> p s c"), in_=gg[:]),
                      reads=[gg.b], writes=[g.GN.buf(tk)], dma="pst")
        barrier(g)


CF_KB0, CF_ZERO, CF_ONE, CF_CPAD = 0, 4, 5, 8
CB_CAUS, CB_NEGU, CB_NEG1 = 0, 2048, 2176


def phase2(g):
    nc, sch, st = g.nc, g.sch, g.st
    S = sch.issue
    SV, NCV = g.SV, g.NCV
    NCH = (NCV + 127) // 128
    g.NCH = NCH
    g.kcmpT = [T(st, nc, "kcmpT%d" % i, [128, NCH * 128], BF16) for i in range(2)]
    g.vcmp = [T(st, nc, "vcmp%d" % i, [128, NCH, 128], BF16) for i in range(2)]
    nrs = [(n0, min(512, NCV - n0)) for n0 in range(0, NCV, 512)]
    with contextlib.ExitStack() as ls:
        w1s = T(ls, nc, "w1s", [128, 32, 256], F32)
        w1b = T(ls, nc, "w1b", [128, 32, 256], BF16)
        w2s = T(ls, nc, "w2s", [128, 2, 128], F32)
        w2b = T(ls, nc, "w2b", [128, 2, 128], BF16)
        pes = T(ls, nc, "pes", [128, 32], F32)
        peb = T(ls, nc, "peb", [128, 32], BF16)
        cv = T(ls, nc, "cv", [128, 2], F32)
        src = [T(ls, nc, "csrc%d" % i, [128, SV], BF16) for i in range(2)]
        u = T(ls, nc, "cu", [128, 512], F32)
        u2 = T(ls, nc, "cu2", [128, 512], F32)
        gl = [T(ls, nc, "gl%d" % i, [128, NCH * 128], BF16) for i in range(2)]
        hps = [T(ls, nc, "hps%d" % i, [128, 512], F32, psum=True) for i in range(2)]
        cps = T(ls, nc, "cps", [128, 2], F32, psum=True)
        ops = T(ls, nc, "cops", [128, 512], F32, psum=True)
        nsrc = 0
        for kv in range(2):
            w1d = g.inp["cw1k" if kv == 0 else "cw1v"].ap
            w2d = g.inp["cw2k" if kv == 0 else "cw2v"].ap
            ped = g.inp["pe_kT" if kv == 0 else "pe_vT"].ap
            S("sp", lambda e, w1d=w1d: e.dma_start(out=w1s[:], in_=w1d.rearrange("(p d) j -> d p j", d=128)), writes=[w1s.b], dma="cw1")
            S("sp", lambda e, w2d=w2d: e.dma_start(out=w2s[:], in_=w2d.rearrange("(c j) d -> j c d", j=128)), writes=[w2s.b], dma="cw2")
            S("sp", lambda e, ped=ped: e.dma_start(out=pes[:], in_=ped), writes=[pes.b], dma="cpe")
            S("act", lambda e: e.copy(w1b[:, 0:16, :], w1s[:, 0:16, :]), reads=[w1s.b], writes=[w1b.b])
            S("dve", lambda e: e.tensor_copy(w1b[:, 16:32, :], w1s[:, 16:32, :]), reads=[w1s.b], writes=[w1b.b])
            S("dve", lambda e: e.tensor_copy(w2b[:], w2s[:]), reads=[w2s.b], writes=[w2b.b])
            S("dve", lambda e: e.tensor_copy(peb[:], pes[:]), reads=[pes.b], writes=[peb.b])
            for jc in range(2):
                for p in range(32):
                    S("pe", lambda e, jc=jc, p=p: e.matmul(cps[:, jc:jc + 1], w1b[:, p, jc * 128:(jc + 1) * 128], peb[:, p:p + 1],
                                                           start=(p == 0), stop=(p == 31)),
                      reads=[w1b.b, peb.b], writes=[cps.b], pe_acc=True)
            S("dve", lambda e: e.tensor_copy(cv[:], cps[:]), reads=[cps.b], writes=[cv.b])
            for gi in range(2):
                sr = src[nsrc % 2]
                nsrc += 1
                S("sp", lambda e, sr=sr, kv=kv, gi=gi: e.dma_start(out=sr[:], in_=g.CPT.ap[kv * 2 + gi]),
                  reads=g.CPT.allbufs(), writes=[sr.b], dma="csrc%d" % (nsrc % 2))
                srv = sr[:].rearrange("p (n s) -> p n s", s=16)
                for jc in range(2):
                    for (n0, nn) in nrs:
                        hp = hps[(jc + n0 // 512) % 2]
                        for p in range(32):
                            S("pe", lambda e, hp=hp, jc=jc, p=p, n0=n0, nn=nn, srv=srv: e.matmul(
                                hp[:, 0:nn], w1b[:, p, jc * 128:(jc + 1) * 128], srv[:, n0 + p // 16:n0 + p // 16 + nn, p % 16],
                                start=(p == 0), stop=(p == 31)),
                              reads=[w1b.b, sr.b], writes=[hp.b], pe_acc=True)
                        S("dve", lambda e, hp=hp, jc=jc, nn=nn: e.tensor_scalar(out=u[:, 0:nn], in0=hp[:, 0:nn], scalar1=cv[:, jc:jc + 1], scalar2=None, op0=ALU.add),
                          reads=[hp.b, cv.b], writes=[u.b])
                        S("dve", lambda e, nn=nn: e.tensor_tensor(out=u2[:, 0:nn], in0=u[:, 0:nn], in1=u[:, 0:nn], op=ALU.mult), reads=[u.b], writes=[u2.b])
                        S("dve", lambda e, nn=nn: e.tensor_scalar(out=u2[:, 0:nn], in0=u2[:, 0:nn], scalar1=0.044715, scalar2=1.0, op0=ALU.mult, op1=ALU.add),
                          reads=[u2.b], writes=[u2.b])
                        S("dve", lambda e, nn=nn: e.tensor_tensor(out=u2[:, 0:nn], in0=u2[:, 0:nn], in1=u[:, 0:nn], op=ALU.mult), reads=[u.b, u2.b], writes=[u2.b])
                        S("act", lambda e, nn=nn: e.activation(out=u2[:, 0:nn], in_=u2[:, 0:nn], func=AF.Sigmoid, scale=1.5957691216057308),
                          reads=[u2.b], writes=[u2.b])
                        S("dve", lambda e, jc=jc, n0=n0, nn=nn: e.tensor_tensor(out=gl[jc][:, n0:n0 + nn], in0=u2[:, 0:nn], in1=u[:, 0:nn], op=ALU.mult),
                          reads=[u.b, u2.b], writes=[gl[jc].b])
                if kv == 0:
                    for (n0, nn) in nrs:
                        for jc in range(2):
                            S("pe", lambda e, jc=jc, n0=n0, nn=nn: e.matmul(ops[:, 0:nn], w2b[:, jc, :], gl[jc][:, n0:n0 + nn], start=(jc == 0), stop=(jc == 1)),
                              reads=[w2b.b, gl[jc].b], writes=[ops.b], pe_acc=True)
                        S("act", lambda e, gi=gi, n0=n0, nn=nn: e.copy(g.kcmpT[gi][:, n0:n0 + nn], ops[:, 0:nn]), reads=[ops.b], writes=[g.kcmpT[gi].b])
                else:
                    for ncx in range(NCH):
                        kk = min(128, NCV - ncx * 128)
                        for jc in range(2):
                            S("pe", lambda e, jc=jc, ncx=ncx, kk=kk: e.matmul(ops[0:kk, 0:128], gl[jc][:, ncx * 128:ncx * 128 + kk], w2b[:, jc, :],
                                                                            start=(jc == 0), stop=(jc == 1)),
                              reads=[w2b.b, gl[jc].b], writes=[ops.b], pe_acc=True)
                        S("act", lambda e, gi=gi, ncx=ncx, kk=kk: e.copy(g.vcmp[gi][0:kk, ncx, :], ops[0:kk, 0:128]), reads=[ops.b], writes=[g.vcmp[gi].b])
        barrier(g)
    if "KCMP" in g.taps:
        g.KCMP = g.dr("KCMP", [2, 128, NCH * 128], BF16)
        g.VCMP = g.dr("VCMP", [2, 128, NCH, 128], BF16)
        g.tapdr += [g.KCMP, g.VCMP]
        for gi in range(2):
            S("sp", lambda e, gi=gi: e.dma_start(out=g.KCMP.ap[gi], in_=g.kcmpT[gi][:]), reads=[g.kcmpT[gi].b], writes=[g.KCMP.buf(gi)], dma=g.uniq())
            S("sp", lambda e, gi=gi: e.dma_start(out=g.VCMP.ap[gi], in_=g.vcmp[gi][:]), reads=[g.vcmp[gi].b], writes=[g.VCMP.buf(gi)], dma=g.uniq())


def phase3(g):
    nc, sch, st = g.nc, g.sch, g.st
    S = sch.issue
    SV, NKB, NQT, NQ = g.SV, g.NKB, g.NQT, g.NQ
    g.OAT = g.dr("OAT", [8, 128, NQ], BF16)
    if "OAT" in g.taps:
        g.tapdr.append(g.OAT)
    cb, cf = g.cb, g.cf
    with contextlib.ExitStack() as ls:
        kT = T(ls, nc, "sb_kT", [128, 2, SV], BF16)
        vv = T(ls, nc, "sb_v", [128, 2, NKB, 128], BF16)
        qT = T(ls, nc, "sb_qT", [128, 2, NQ], BF16)
        ex = [T(ls, nc, "sb_e%d" % i, [128, 2, 512], F32) for i in range(2)]
        sp = [T(ls, nc, "sb_sp%d" % i, [128, 2, 512], BF16) for i in range(2)]
        t1 = [T(ls, nc, "sb_t%d" % i, [128, 2, 512], F32) for i in range(2)]
        aa = [T(ls, nc, "sb_a%d" % i, [128, 2, 512], BF16) for i in range(2)]
        r32 = T(ls, nc, "sb_r32", [128, 2, 512], F32)
        r16 = [T(ls, nc, "sb_r16_%d" % i, [128, 2, 512], BF16) for i in range(2)]
        osb = [T(ls, nc, "sb_o%d" % i, [128, 2, 512], BF16) for i in range(2)]
        zps = [[T(ls, nc, "sb_z%d_%d" % (i, h), [128, 512], F32, psum=True) for h in range(2)] for i in range(2)]
        aps = [T(ls, nc, "sb_ap%d" % h, [128, 512], F32, psum=True) for h in range(2)]
        ops = [T(ls, nc, "sb_op%d" % h, [128, 512], F32, psum=True) for h in range(2)]
        negU = cb[:, CB_NEGU:CB_NEGU + 128]
        neg1 = cb[:, CB_NEG1:CB_NEG1 + 128]
        ng = 0
        for hp in range(4):
            S("sp", lambda e, hp=hp: e.dma_start(out=kT[:], in_=g.KAT.ap[2 * hp:2 * hp + 2].rearrange("h p t -> p h t")),
              reads=g.KAT.allbufs(), writes=[kT.b], dma="sbk")
            S("sp", lambda e, hp=hp: e.dma_start(out=vv[:], in_=g.VA.ap[2 * hp:2 * hp + 2].rearrange("h p t d -> p h t d")),
              reads=g.VA.allbufs(), writes=[vv.b], dma="sbv")
            S("sp", lambda e, hp=hp: e.dma_start(out=qT[:], in_=g.QAT.ap[2 * hp:2 * hp + 2].rearrange("h p t -> p h t")),
              reads=g.QAT.allbufs(), writes=[qT.b], dma="sbq")
            for k in range(NQT):
                tv = 2 * k + 1
                jts = list(range(4 * tv + 3, -1, -1))
                for ji, jt in enumerate(jts):
                    i2 = ng % 2
                    ng += 1
                    e_, sp_, t_, a_, z_ = ex[i2], sp[i2], t1[i2], aa[i2], zps[i2]
                    first, last = (ji == 0), (ji == len(jts) - 1)
                    dg = jt - 4 * tv
                    kb = cf[:, CF_KB0 + jt:CF_KB0 + jt + 1] if jt < 4 else cf[:, CF_ZERO:CF_ZERO + 1]
                    for h in range(2):
                        S("pe", lambda e, h=h, jt=jt, k=k, z_=z_: e.matmul(z_[h][:, :], kT[:, h, jt * 128:(jt + 1) * 128], qT[:, h, k * 512:(k + 1) * 512],
                                                                        start=True, stop=True),
                          reads=[kT.b, qT.b], writes=[z_[h].b])
                    for h in range(2):
                        S("act", lambda e, h=h, z_=z_, e_=e_, kb=kb: e.activation(out=e_[:, h, :], in_=z_[h][:, :], func=AF.Exp, scale=SCALE, bias=kb),
                          reads=[z_[h].b, cf.b], writes=[e_.b])
                    S("act", lambda e, e_=e_, sp_=sp_: e.activation(out=sp_[:], in_=e_[:], func=AF.Ln, bias=cf[:, CF_ONE:CF_ONE + 1]),
                      reads=[e_.b, cf.b], writes=[sp_.b])
                    if dg >= 0:
                        for h in range(2):
                            S("pool", lambda e, h=h, sp_=sp_, dg=dg: e.tensor_tensor(out=sp_[:, h, :], in0=sp_[:, h, :], in1=cb[:, CB_CAUS + dg * 512:CB_CAUS + (dg + 1) * 512], op=ALU.mult),
                              reads=[sp_.b, cb.b], writes=[sp_.b])
                    rprev = r16[(ji + 1) % 2]
                    for h in range(2):
                        S("pe", lambda e, h=h, sp_=sp_, first=first: e.matmul(aps[h][:, :], negU, sp_[:, h, :], start=True, stop=first),
                          reads=[cb.b, sp_.b], writes=[aps[h].b])
                        if not first:
                            S("pe", lambda e, h=h, rprev=rprev: e.matmul(aps[h][:, :], neg1, rprev[:, h, :], start=False, stop=True),
                              reads=[cb.b, rprev.b], writes=[aps[h].b], pe_acc=True)
                    for h in range(2):
                        S("dve", lambda e, h=h, z_=z_, sp_=sp_, t_=t_: e.scalar_tensor_tensor(out=t_[:, h, :], in0=z_[h][:, :], scalar=SCALE, in1=sp_[:, h, :],
                                                                                       op0=ALU.mult, op1=ALU.subtract),
                          reads=[z_[h].b, sp_.b], writes=[t_.b])
                        S("dve", lambda e, h=h, t_=t_: e.tensor_tensor(out=t_[:, h, :], in0=aps[h][:, :], in1=t_[:, h, :], op=ALU.add),
                          reads=[aps[h].b, t_.b], writes=[t_.b])
                    S("act", lambda e, t_=t_, a_=a_, kb=kb: e.activation(out=a_[:], in_=t_[:], func=AF.Exp, bias=kb),
                      reads=[t_.b, cf.b], writes=[a_.b])
                    if dg >= 0:
                        for h in range(2):
                            S("pool", lambda e, h=h, a_=a_, dg=dg: e.tensor_tensor(out=a_[:, h, :], in0=a_[:, h, :], in1=cb[:, CB_CAUS + dg * 512:CB_CAUS + (dg + 1) * 512], op=ALU.mult),
                              reads=[a_.b, cb.b], writes=[a_.b])
                    if not last:
                        rcur = r16[ji % 2]
                        if first:
                            S("pool", lambda e, sp_=sp_: e.tensor_copy(r32[:], sp_[:]), reads=[sp_.b], writes=[r32.b])
                        else:
                            S("pool", lambda e, sp_=sp_: e.tensor_tensor(out=r32[:], in0=r32[:], in1=sp_[:], op=ALU.add), reads=[sp_.b, r32.b], writes=[r32.b])
                        S("pool", lambda e, rcur=rcur: e.tensor_copy(rcur[:], r32[:]), reads=[r32.b], writes=[rcur.b])
                    for h in range(2):
                        S("pe", lambda e, h=h, jt=jt, a_=a_, first=first, last=last: e.matmul(ops[h][:, :], vv[:, h, jt, :], a_[:, h, :], start=first, stop=last),
                          reads=[vv.b, a_.b], writes=[ops[h].b], pe_acc=not first)
                o_ = osb[(hp * NQT + k) % 2]
                S("act", lambda e, o_=o_: e.copy(o_[:, 0, :], ops[0][:, :]), reads=[ops[0].b], writes=[o_.b])
                S("dve", lambda e, o_=o_: e.tensor_copy(o_[:, 1, :], ops[1][:, :]), reads=[ops[1].b], writes=[o_.b])
                S("sp", lambda e, o_=o_, hp=hp, k=k: e.dma_start(out=g.OAT.ap[2 * hp:2 * hp + 2, :, k * 512:(k + 1) * 512].rearrange("h p t -> p h t"), in_=o_[:]),
                  reads=[o_.b], writes=[g.OAT.buf((hp, k))], dma="sbo")
        barrier(g)


CB_WM4, CB_ONESROW, CB_CPADROW, CB_WI, CB_ONESCOL, CB_ONES = 2304, 2816, 2944, 2976, 3264, 3328
CF_PADNEG, CF_F0 = 16, 176
OH_S0, OH_S1, OH_BC, OH_W = 0, 33 * 128, 33 * 128 + 32 * 128, 33 * 128 + 32 * 128 + 33 * 17


def phase4(g):
    nc, sch, st = g.nc, g.sch, g.st
    S = sch.issue
    SV, NKB, NQT, NQ, NQB, NCV, NSV, NCH = g.SV, g.NKB, g.NQT, g.NQ, g.NQB, g.NCV, g.NSV, g.NCH
    NCVP = NSV * 4
    KA = min(128, NSV)
    KB = NSV - KA
    g.OBT = g.dr("OBT", [8, 128, NQ], BF16)
    if "OBT" in g.taps:
        g.tapdr.append(g.OBT)
    cb, cf, idb = g.cb, g.cf, g.idb
    with contextlib.ExitStack() as ls:
        BS0 = T(ls, nc, "BS0", [128, 8, 128], BF16)
        BS1 = T(ls, nc, "BS1", [128, 8, 128], BF16)
        BCq = T(ls, nc, "BCq", [128, 8, 17], BF16)
        BCT = T(ls, nc, "BCT", [17, 8, 128], BF16)
        bmax = T(ls, nc, "bmax", [128, 1], F32)
        exp_c = T(ls, nc, "expc", [128, SV], BF16)
        S("sp", lambda e: e.dma_start(out=exp_c[:], in_=g.inp["c_exp"].ap), writes=[exp_c.b], dma=g.uniq())
        with contextlib.ExitStack() as l2:
            oh = T(l2, nc, "oh", [128, OH_W], BF16)
            tv = T(l2, nc, "tv", [128, 256], F32)
            td = T(l2, nc, "td", [128, 256], F32)
            acc0 = T(l2, nc, "acc0", [128, 128], F32)
            acc1 = T(l2, nc, "acc1", [128, 128], F32)
            acc2 = T(l2, nc, "acc2", [128, 17], F32)
            tps = T(l2, nc, "bctps", [128, 128], F32, psum=True)
            S("sp", lambda e: e.dma_start(out=oh[:], in_=g.inp["c_oh"].ap), writes=[oh.b], dma=g.uniq())
            S("sp", lambda e: e.dma_start(out=tv[:], in_=g.inp["relb"].ap.rearrange("(o b) h -> o (b h)", o=1).partition_broadcast(128)),
              writes=[tv.b], dma=g.uniq())
            tv3 = tv[:].rearrange("p (b h) -> p b h", h=8)
            td3 = td[:].rearrange("p (b h) -> p b h", h=8)
            for h in range(8):
                S("dve", lambda e, h=h: e.tensor_scalar(out=td3[:, :, h], in0=tv3[:, :, h], scalar1=tv[:, 248 + h:249 + h], scalar2=None, op0=ALU.subtract),
                  reads=[tv.b], writes=[td.b])
            S("dve", lambda e: e.tensor_reduce(out=bmax[:], in_=td[:], axis=AX.X, op=ALU.max, apply_absolute_value=True), reads=[td.b], writes=[bmax.b])
            S("dve", lambda e: e.tensor_scalar(out=td[:], in0=td[:], scalar1=1.0 / SCALE, scalar2=None, op0=ALU.mult), reads=[td.b], writes=[td.b])
            ohs0 = oh[:, OH_S0:OH_S0 + 33 * 128].rearrange("p (b q) -> p b q", q=128)
            ohs1 = oh[:, OH_S1:OH_S1 + 32 * 128].rearrange("p (b q) -> p b q", q=128)
            ohbc = oh[:, OH_BC:OH_BC + 33 * 17].rearrange("p (b q) -> p b q", q=17)
            for h in range(8):
                S("dve", lambda e: e.tensor_scalar(out=acc0[:], in0=ohs0[:, 32, :], scalar1=NEG, scalar2=None, op0=ALU.mult), reads=[oh.b], writes=[acc0.b])
                S("pool", lambda e: e.tensor_scalar(out=acc2[:], in0=ohbc[:, 32, :], scalar1=NEG, scalar2=None, op0=ALU.mult), reads=[oh.b], writes=[acc2.b])
                for b in range(31):
                    sc_ = td[:, b * 8 + h:b * 8 + h + 1]
                    S("dve", lambda e, b=b, sc_=sc_: e.scalar_tensor_tensor(out=acc0[:], in0=ohs0[:, b, :], scalar=sc_, in1=acc0[:], op0=ALU.mult, op1=ALU.add),
                      reads=[oh.b, td.b, acc0.b], writes=[acc0.b])
                    if b == 0:
                        S("dve", lambda e, b=b, sc_=sc_: e.tensor_scalar(out=acc1[:], in0=ohs1[:, b, :], scalar1=sc_, scalar2=None, op0=ALU.mult),
                          reads=[oh.b, td.b], writes=[acc1.b])
                    else:
                        S("dve", lambda e, b=b, sc_=sc_: e.scalar_tensor_tensor(out=acc1[:], in0=ohs1[:, b, :], scalar=sc_, in1=acc1[:], op0=ALU.mult, op1=ALU.add),
                          reads=[oh.b, td.b, acc1.b], writes=[acc1.b])
                    S("dve", lambda e, b=b, sc_=sc_: e.scalar_tensor_tensor(out=acc2[:], in0=ohbc[:, b, :], scalar=sc_, in1=acc2[:], op0=ALU.mult, op1=ALU.add),
                      reads=[oh.b, td.b, acc2.b], writes=[acc2.b])
                S("act", lambda e, h=h: e.copy(BS0[:, h, :], acc0[:]), reads=[acc0.b], writes=[BS0.b])
                S("act", lambda e, h=h: e.copy(BS1[:, h, :], acc1[:]), reads=[acc1.b], writes=[BS1.b])
                S("act", lambda e, h=h: e.copy(BCq[:, h, :], acc2[:]), reads=[acc2.b], writes=[BCq.b])
                S("pe", lambda e: e.transpose(tps[0:17, :], acc2[:], g.idf[:]), reads=[acc2.b, g.idf.b], writes=[tps.b])
                S("act", lambda e, h=h: e.copy(BCT[:, h, :], tps[0:17, :]), reads=[tps.b], writes=[BCT.b])
            barrier(g)
        kslT = T(ls, nc, "kslT", [128, SV], BF16)
        vsl = T(ls, nc, "vsl", [128, NKB, 128], BF16)
        kswT = T(ls, nc, "kswT", [128, SV], BF16)
        vsw = T(ls, nc, "vsw", [128, NKB, 128], BF16)
        qT = T(ls, nc, "nqT", [128, 4, NQ], BF16)
        sq = T(ls, nc, "nsq", [128, 2048], BF16)
        mx = T(ls, nc, "nmx", [128, 4], F32)
        nmk = T(ls, nc, "nmk", [128, 8], F32)
        ec = T(ls, nc, "n_ec", [128, 4, NCVP], F32)
        zc = T(ls, nc, "n_zc", [128, 4], F32)
        pc = T(ls, nc, "n_pc", [128, NCVP], F32)
        imp = T(ls, nc, "n_imp", [128, NSV], F32)
        sc2 = T(ls, nc, "n_sc2", [128, NSV], F32)
        m8 = T(ls, nc, "n_m8", [128, 16], F32)
        nm = T(ls, nc, "n_nm", [128, NSV], BF16)
        nmTA = T(ls, nc, "n_nmTA", [128, 4, 128], BF16)
        nmTB = T(ls, nc, "n_nmTB", [8, 4, 128], BF16)
        pT = [T(ls, nc, "n_pT%d" % i, [128, 512], BF16) for i in range(2)]
        gn = T(ls, nc, "n_gn", [128, 24], F32)
        zz = T(ls, nc, "n_zz", [128, 12], F32)
        coef = T(ls, nc, "n_coef", [128, 12], F32)
        otmp = T(ls, nc, "n_otmp", [128, 128], F32)
        ob = T(ls, nc, "n_ob", [128, 4, 128], BF16)
        obT = [T(ls, nc, "n_obT%d" % i, [128, 4, 128], BF16) for i in range(2)]
        scq = T(ls, nc, "n_scq", [128, 512], F32, psum=True)
        trp = T(ls, nc, "n_trp", [128, 1024], BF16, psum=True)
        stp = [T(ls, nc, "n_st%d" % i, [128, 512], F32, psum=True) for i in range(2)]
        opb = [T(ls, nc, "n_o%d" % i, [128, 512], F32, psum=True) for i in range(3)]
        zpb = T(ls, nc, "n_z", [128, 512], F32, psum=True)
        ones_m = cb[:, CB_ONES:CB_ONES + 128]
        onescol = cb[:, CB_ONESCOL:CB_ONESCOL + 1]
        nst = [0]

        def maxnorm(src_fn, total, col, srcbuf):
            for c0 in range(0, total, 512):
                w = min(512, total - c0)
                S("dve", lambda e, c0=c0, w=w: e.tensor_tensor(out=sq[:, 0:w], in0=src_fn(c0, w), in1=src_fn(c0, w), op=ALU.mult), reads=[srcbuf], writes=[sq.b])
                S("pe", lambda e, w=w: e.matmul(scq[:, 0:w], ones_m, sq[:, 0:w], start=True, stop=True), reads=[cb.b, sq.b], writes=[scq.b])
                S("dve", lambda e, w=w: e.tensor_reduce(out=mx[:, 2:3], in_=scq[:, 0:w], axis=AX.X, op=ALU.max), reads=[scq.b], writes=[mx.b])
                S("dve", lambda e, col=col: e.tensor_tensor(out=mx[:, col:col + 1], in0=mx[:, col:col + 1], in1=mx[:, 2:3], op=ALU.max), reads=[mx.b], writes=[mx.b])

        for gi in range(2):
            S("sp", lambda e, gi=gi: e.dma_start(out=kslT[:], in_=g.KSLT.ap[gi]), reads=g.KSLT.allbufs(), writes=[kslT.b], dma="n_k1")
            S("sp", lambda e, gi=gi: e.dma_start(out=vsl[:], in_=g.VSL.ap[gi]), reads=g.VSL.allbufs(), writes=[vsl.b], dma="n_v1")
            S("sp", lambda e, gi=gi: e.dma_start(out=kswT[:], in_=g.KSWT.ap[gi]), reads=g.KSWT.allbufs(), writes=[kswT.b], dma="n_k2")
            S("sp", lambda e, gi=gi: e.dma_start(out=vsw[:], in_=g.VSW.ap[gi]), reads=g.VSW.allbufs(), writes=[vsw.b], dma="n_v2")
            S("sp", lambda e, gi=gi: e.dma_start(out=qT[:], in_=g.QBT.ap[4 * gi:4 * gi + 4].rearrange("h p t -> p h t")),
              reads=g.QBT.allbufs(), writes=[qT.b], dma="n_q")
            S("dve", lambda e: e.memset(mx[:], 0.0), writes=[mx.b])
            S("dve", lambda e: e.memset(pc[:], 0.0), writes=[pc.b])
            for h in range(4):
                maxnorm(lambda c0, w, h=h: qT[:, h, c0:c0 + w], NQ, 0, qT.b)
            maxnorm(lambda c0, w: kslT[:, c0:c0 + w], SV, 1, kslT.b)
            maxnorm(lambda c0, w: kswT[:, c0:c0 + w], SV, 1, kswT.b)
            maxnorm(lambda c0, w, gi=gi: g.kcmpT[gi][:, c0:c0 + w], NCV, 1, g.kcmpT[gi].b)
            S("dve", lambda e: e.tensor_tensor(out=mx[:, 2:3], in0=mx[:, 0:1], in1=mx[:, 1:2], op=ALU.mult), reads=[mx.b], writes=[mx.b])
            S("act", lambda e: e.activation(out=mx[:, 2:3], in_=mx[:, 2:3], func=AF.Sqrt), reads=[mx.b], writes=[mx.b])
            S("dve", lambda e: e.scalar_tensor_tensor(out=mx[:, 3:4], in0=mx[:, 2:3], scalar=-SCALE * 1.02, in1=bmax[:], op0=ALU.mult, op1=ALU.subtract),
              reads=[mx.b, bmax.b], writes=[mx.b])
            for j in range(4):
                S("dve", lambda e, j=j: e.tensor_tensor(out=nmk[:, j:j + 1], in0=mx[:, 3:4], in1=cf[:, CF_KB0 + j:CF_KB0 + j + 1], op=ALU.add),
                  reads=[mx.b, cf.b], writes=[nmk.b])
            S("dve", lambda e: e.tensor_copy(nmk[:, 4:5], mx[:, 3:4]), reads=[mx.b], writes=[nmk.b])
            S("dve", lambda e: e.tensor_tensor(out=nmk[:, 5:6], in0=mx[:, 3:4], in1=cf[:, CF_CPAD:CF_CPAD + 1], op=ALU.add), reads=[mx.b, cf.b], writes=[nmk.b])
            negM = nmk[:, 4:5]

            for k in range(NQT):
                for c in range(4):
                    iv = 4 * (2 * k + 1) + c
                    lb = 4 * k + c
                    q4 = qT[:, :, lb * 128:(lb + 1) * 128]
                    ncols = min(8 * iv + 7, NCV)
                    n0 = 8 * iv - 10
                    S("sp", lambda e, lb=lb: e.dma_start(out=gn[:], in_=g.GN.ap[lb]), reads=g.GN.allbufs(), writes=[gn.b], dma="n_gn")
                    for h in range(4):
                        S("pe", lambda e, h=h, lb=lb, ncols=ncols: e.matmul(scq[:, 0:ncols], qT[:, h, lb * 128:(lb + 1) * 128], g.kcmpT[gi][:, 0:ncols], start=True, stop=False),
                          reads=[qT.b, g.kcmpT[gi].b], writes=[scq.b])
                        S("pe", lambda e, h=h, n0=n0: e.matmul(scq[:, n0:n0 + 17], idb[:], BCq[:, 4 * gi + h, :], start=False, stop=False),
                          reads=[idb.b, BCq.b], writes=[scq.b], pe_acc=True)
                        S("pe", lambda e: e.matmul(scq[:, 0:32], cb[0:1, CB_ONESROW:CB_ONESROW + 128], cb[0:1, CB_CPADROW:CB_CPADROW + 32], start=False, stop=True),
                          reads=[cb.b], writes=[scq.b], pe_acc=True)
                        S("act", lambda e, h=h, ncols=ncols: e.activation(out=ec[:, h, 0:ncols], in_=scq[:, 0:ncols], func=AF.Exp, scale=SCALE, bias=negM, accum_out=zc[:, h:h + 1]),
                          reads=[scq.b, nmk.b], writes=[ec.b, zc.b])
                    S("dve", lambda e: e.tensor_scalar(out=zc[:], in0=zc[:], scalar1=1e-30, scalar2=None, op0=ALU.max), reads=[zc.b], writes=[zc.b])
                    S("dve", lambda e: e.reciprocal(out=zc[:], in_=zc[:]), reads=[zc.b], writes=[zc.b])
                    S("dve", lambda e, ncols=ncols: e.tensor_scalar(out=pc[:, 0:ncols], in0=ec[:, 0, 0:ncols], scalar1=zc[:, 0:1], scalar2=None, op0=ALU.mult),
                      reads=[ec.b, zc.b], writes=[pc.b])
                    for h in range(1, 4):
                        S("dve", lambda e, h=h, ncols=ncols: e.scalar_tensor_tensor(out=pc[:, 0:ncols], in0=ec[:, h, 0:ncols], scalar=zc[:, h:h + 1], in1=pc[:, 0:ncols],
                                                                                 op0=ALU.mult, op1=ALU.add),
                          reads=[ec.b, zc.b, pc.b], writes=[pc.b])
                    pcv = pc[:].rearrange("p (j f) -> p j f", f=4)
                    S("dve", lambda e: e.tensor_reduce(out=imp[:], in_=pcv, axis=AX.X, op=ALU.add), reads=[pc.b], writes=[imp.b])
                    S("dve", lambda e: e.scalar_tensor_tensor(out=imp[:], in0=pcv[:, :, 3], scalar=-0.5, in1=imp[:], op0=ALU.mult, op1=ALU.add),
                      reads=[pc.b, imp.b], writes=[imp.b])
                    S("dve", lambda e: e.scalar_tensor_tensor(out=imp[:, 1:NSV], in0=pcv[:, 0:NSV - 1, 3], scalar=0.5, in1=imp[:, 1:NSV], op0=ALU.mult, op1=ALU.add),
                      reads=[pc.b, imp.b], writes=[imp.b])
                    S("dve", lambda e: e.tensor_tensor(out=imp[:], in0=imp[:], in1=cf[:, CF_PADNEG:CF_PADNEG + NSV], op=ALU.add), reads=[imp.b, cf.b], writes=[imp.b])
                    S("dve", lambda e: e.tensor_tensor(out=imp[:], in0=imp[:], in1=cf[:, CF_F0:CF_F0 + NSV], op=ALU.max), reads=[imp.b, cf.b], writes=[imp.b])
                    if 2 * iv + 2 < NSV:
                        S("dve", lambda e, iv=iv: e.memset(imp[:, 2 * iv + 2:NSV], -1e30), writes=[imp.b])
                    S("dve", lambda e, iv=iv: e.memset(imp[0:64, 2 * iv + 1:2 * iv + 2], -1e30), writes=[imp.b])
                    S("dve", lambda e, iv=iv: e.memset(imp[64:128, 2 * iv + 1:2 * iv + 2], 1e4), writes=[imp.b])
                    S("dve", lambda e, iv=iv: e.memset(imp[:, 2 * iv:2 * iv + 1], 1e4), writes=[imp.b])
                    S("dve", lambda e, iv=iv: e.memset(imp[0:64, 2 * iv - 1:2 * iv], 1e4), writes=[imp.b])
                    S("dve", lambda e: e.max(out=m8[:, 0:8], in_=imp[:]), reads=[imp.b], writes=[m8.b])
                    S("dve", lambda e: e.match_replace(out=sc2[:], in_to_replace=m8[:, 0:8], in_values=imp[:], imm_value=-3e38), reads=[imp.b, m8.b], writes=[sc2.b])
                    S("dve", lambda e: e.max(out=m8[:, 8:16], in_=sc2[:]), reads=[sc2.b], writes=[m8.b])
                    S("dve", lambda e: e.tensor_scalar(out=nm[:], in0=imp[:], scalar1=m8[:, 15:16], scalar2=NEG, op0=ALU.is_lt, op1=ALU.mult),
                      reads=[imp.b, m8.b], writes=[nm.b])
                    trv = trp[:, 0:512].bitcast(F32) if False else None
                    S("pe", lambda e: e.matmul(scq[0:KA, 0:128], nm[:, 0:KA], idb[:], start=True, stop=True), reads=[nm.b, idb.b], writes=[scq.b])
                    for h in range(4):
                        eng = "act" if h % 2 == 0 else "dve"
                        if eng == "act":
                            S("act", lambda e, h=h: e.copy(nmTA[0:KA, h, :], scq[0:KA, 0:128]), reads=[scq.b], writes=[nmTA.b])
                        else:
                            S("dve", lambda e, h=h: e.tensor_copy(nmTA[0:KA, h, :], scq[0:KA, 0:128]), reads=[scq.b], writes=[nmTA.b])
                    if KB > 0:
                        S("pe", lambda e: e.matmul(scq[0:KB, 128:256], nm[:, KA:KA + KB], idb[:], start=True, stop=True), reads=[nm.b, idb.b], writes=[scq.b])
                        for h in range(4):
                            S("dve", lambda e, h=h: e.tensor_copy(nmTB[0:KB, h, :], scq[0:KB, 128:256]), reads=[scq.b], writes=[nmTB.b])

                    def tile_tail(br, first, kk, bias_ap, vrhs, vbuf):
                        st_ = stp[nst[0] % 2]
                        p_ = pT[nst[0] % 2]
                        nst[0] += 1
                        return st_, p_

                    def run_tile(br, first, kk, mms, bias_ap, vrhs, vbuf):
                        st_ = stp[nst[0] % 2]
                        p_ = pT[nst[0] % 2]
                        nst[0] += 1
                        for mi, (lhsT, rhs, rbufs) in enumerate(mms):
                            S("pe", lambda e, lhsT=lhsT, rhs=rhs, mi=mi, st_=st_, kk=kk: e.matmul(st_[0:kk, :], lhsT, rhs, start=(mi == 0), stop=(mi == len(mms) - 1)),
                              reads=rbufs, writes=[st_.b], pe_acc=(mi > 0))
                        S("act", lambda e, st_=st_, p_=p_, kk=kk, bias_ap=bias_ap: e.activation(out=p_[0:kk, :], in_=st_[0:kk, :], func=AF.Exp, scale=SCALE, bias=bias_ap),
                          reads=[st_.b, nmk.b], writes=[p_.b])
                        for h in range(4):
                            S("pe", lambda e, h=h, p_=p_, kk=kk, br=br, first=first, vrhs=vrhs: e.matmul(
                                opb[br][:, h * 128:(h + 1) * 128], p_[0:kk, h * 128:(h + 1) * 128], vrhs, start=(first and h == 0), stop=False),
                              reads=[p_.b, vbuf], writes=[opb[br].b], pe_acc=not (first and h == 0))
                        for h in range(4):
                            S("pe", lambda e, h=h, p_=p_, kk=kk, br=br, first=first: e.matmul(
                                zpb[:, br * 4 + h:br * 4 + h + 1], p_[0:kk, h * 128:(h + 1) * 128], cb[0:kk, CB_ONESCOL:CB_ONESCOL + 1],
                                start=(first and h == 0 and br == 0), stop=False),
                              reads=[p_.b, cb.b], writes=[zpb.b], pe_acc=not (first and h == 0 and br == 0))

                    nchunks = (ncols + 127) // 128
                    for ncx in range(nchunks):
                        kk = min(128, ncols - 128 * ncx)
                        mms = [(g.kcmpT[gi][:, ncx * 128:ncx * 128 + kk], q4, [g.kcmpT[gi].b, qT.b])]
                        dlt = n0 - 128 * ncx
                        if dlt > -17 and dlt < kk:
                            mms.append((cb[0:17, CB_WI + 128 - dlt:CB_WI + 128 - dlt + kk], BCT[:, 4 * gi:4 * gi + 4, :], [cb.b, BCT.b]))
                        run_tile(0, ncx == 0, kk, mms, nmk[0:kk, 5:6] if ncx == 0 else nmk[0:kk, 4:5], g.vcmp[gi][0:kk, ncx, :], g.vcmp[gi].b)
                    for jt in range(iv + 1):
                        mms = [(kslT[:, jt * 128:(jt + 1) * 128], q4, [kslT.b, qT.b])]
                        if jt < 64 or KB == 0:
                            mms.append((exp_c[0:KA, jt * 128:(jt + 1) * 128], nmTA[0:KA, :, :], [exp_c.b, nmTA.b]))
                        else:
                            mms.append((exp_c[0:KB, jt * 128:(jt + 1) * 128], nmTB[0:KB, :, :], [exp_c.b, nmTB.b]))
                        if jt == iv:
                            mms.append((idb[:], BS0[:, 4 * gi:4 * gi + 4, :], [idb.b, BS0.b]))
                        elif jt == iv - 1:
                            mms.append((idb[:], BS1[:, 4 * gi:4 * gi + 4, :], [idb.b, BS1.b]))
                        run_tile(1, jt == 0, 128, mms, nmk[:, jt:jt + 1] if jt < 4 else nmk[:, 4:5], vsl[:, jt, :], vsl.b)
                    for jt in range(iv - 4, iv + 1):
                        mms = [(kswT[:, jt * 128:(jt + 1) * 128], q4, [kswT.b, qT.b])]
                        if jt == iv:
                            mms.append((idb[:], BS0[:, 4 * gi:4 * gi + 4, :], [idb.b, BS0.b]))
                        elif jt == iv - 1:
                            mms.append((idb[:], BS1[:, 4 * gi:4 * gi + 4, :], [idb.b, BS1.b]))
                        elif jt == iv - 4:
                            mms.append((idb[:], cb[:, CB_WM4:CB_WM4 + 512], [idb.b, cb.b]))
                        run_tile(2, jt == iv - 4, 128, mms, nmk[:, jt:jt + 1] if jt < 4 else nmk[:, 4:5], vsw[:, jt, :], vsw.b)
                    S("dve", lambda e: e.tensor_scalar(out=zz[:], in0=zpb[:, 0:12], scalar1=1e-30, scalar2=None, op0=ALU.max), reads=[zpb.b], writes=[zz.b])
                    S("dve", lambda e: e.reciprocal(out=zz[:], in_=zz[:]), reads=[zz.b], writes=[zz.b])
                    S("dve", lambda e: e.tensor_tensor(out=coef[:].rearrange("p (b h) -> p b h", h=4), in0=zz[:].rearrange("p (b h) -> p b h", h=4),
                                                      in1=gn[:, gi * 12:(gi + 1) * 12].rearrange("p (h b) -> p b h", b=3), op=ALU.mult),
                      reads=[zz.b, gn.b], writes=[coef.b])
                    for h in range(4):
                        S("dve", lambda e, h=h: e.tensor_scalar(out=otmp[:], in0=opb[0][:, h * 128:(h + 1) * 128], scalar1=coef[:, h:h + 1], scalar2=None, op0=ALU.mult),
                          reads=[opb[0].b, coef.b], writes=[otmp.b])
                        S("dve", lambda e, h=h: e.scalar_tensor_tensor(out=otmp[:], in0=opb[1][:, h * 128:(h + 1) * 128], scalar=coef[:, 4 + h:5 + h], in1=otmp[:], op0=ALU.mult, op1=ALU.add),
                          reads=[opb[1].b, coef.b, otmp.b], writes=[otmp.b])
                        S("dve", lambda e, h=h: e.scalar_tensor_tensor(out=ob[:, h, :], in0=opb[2][:, h * 128:(h + 1) * 128], scalar=coef[:, 8 + h:9 + h], in1=otmp[:], op0=ALU.mult, op1=ALU.add),
                          reads=[opb[2].b, coef.b, otmp.b], writes=[ob.b])
                    for h in range(4):
                        S("pe", lambda e, h=h: e.transpose(trp[:, h * 128:(h + 1) * 128], ob[:, h, :], idb[:]), reads=[ob.b, idb.b], writes=[trp.b], pe_acc=(h > 0))
                    oT_ = obT[(gi * NQB + lb) % 2]
                    S("act", lambda e, oT_=oT_: e.copy(oT_[:].rearrange("p h q -> p (h q)"), trp[:, 0:512]), reads=[trp.b], writes=[oT_.b])
                    S("sp", lambda e, oT_=oT_, lb=lb: e.dma_start(out=g.OBT.ap[4 * gi:4 * gi + 4, :, lb * 128:(lb + 1) * 128].rearrange("h p t -> p h t"), in_=oT_[:]),
                      reads=[oT_.b], writes=[g.OBT.buf((gi, lb))], dma="n_o")
        barrier(g)


CF_EOFF = 336
CB_LTRI = 3456


def phase5(g):
    nc, sch, st = g.nc, g.sch, g.st
    S = sch.issue
    NQT, NQ, NQB, CAP = g.NQT, g.NQ, g.NQB, g.CAP
    cb, cf, idb, idf = g.cb, g.cf, g.idb, g.idf
    g.MT = g.dr("MT", [16, 128, NQ], BF16)
    g.X1 = g.dr("X1", [NQ, D], F32)
    g.XB = g.dr("XB", [NEXP * CAP, D], BF16)
    g.RT = g.dr("RT", [NQB, 128, 4], F32)
    for nm_ in ("MT", "X1", "RT"):
        if nm_ in g.taps:
            g.tapdr.append(getattr(g, nm_))
    g.breg = nc.gpsimd.to_reg(NEXP * CAP - 1)
    g.wts = T(st, nc, "wts", [128, NQB, 2], F32)
    g.dst = T(st, nc, "dst", [128, NQB, 2], I32)
    with contextlib.ExitStack() as ls:
        wab = T(ls, nc, "wab", [128, 8, D], BF16)
        wbb = T(ls, nc, "wbb", [128, 8, D], BF16)
        with contextlib.ExitStack() as l2:
            stg = [T(l2, nc, "wstg%d" % i, [128, 8, 512], F32) for i in range(2)]
            n = 0
            for (wd, wt) in ((g.inp["wba"].ap, wab), (g.inp["wbb"].ap, wbb)):
                for cs in range(4):
                    sg = stg[n % 2]
                    S("sp", lambda e, sg=sg, wd=wd, cs=cs: e.dma_start(out=sg[:], in_=wd[:, cs * 512:(cs + 1) * 512].rearrange("(c p) j -> p c j", p=128)),
                      writes=[sg.b], dma="wstg%d" % (n % 2))
                    S("act", lambda e, sg=sg, wt=wt, cs=cs: e.copy(wt[:, 0:4, cs * 512:(cs + 1) * 512], sg[:, 0:4, :]), reads=[sg.b], writes=[wt.b])
                    S("dve", lambda e, sg=sg, wt=wt, cs=cs: e.tensor_copy(wt[:, 4:8, cs * 512:(cs + 1) * 512], sg[:, 4:8, :]), reads=[sg.b], writes=[wt.b])
                    n += 1
            barrier(g)
        oa = [T(ls, nc, "m_oa%d" % i, [128, 8, 512], BF16) for i in range(2)]
        obt = [T(ls, nc, "m_ob%d" % i, [128, 8, 512], BF16) for i in range(2)]
        ga = [T(ls, nc, "m_ga%d" % i, [128, 4, 512], BF16) for i in range(2)]
        gb = [T(ls, nc, "m_gb%d" % i, [128, 4, 512], BF16) for i in range(2)]
        mo = [T(ls, nc, "m_mo%d" % i, [128, 4, 512], BF16) for i in range(2)]
        ta = [T(ls, nc, "m_ta%d" % i, [128, 512], F32) for i in range(2)]
        tb_ = [T(ls, nc, "m_tb%d" % i, [128, 512], F32) for i in range(2)]
        psA = [T(ls, nc, "m_pa%d" % i, [128, 512], F32, psum=True) for i in range(2)]
        psB = [T(ls, nc, "m_pb%d" % i, [128, 512], F32, psum=True) for i in range(2)]
        nq = 0
        for k in range(NQT):
            oa_, ob_ = oa[k % 2], obt[k % 2]
            S("sp", lambda e, oa_=oa_, k=k: e.dma_start(out=oa_[:], in_=g.OAT.ap[:, :, k * 512:(k + 1) * 512].rearrange("h p t -> p h t")),
              reads=g.OAT.allbufs(), writes=[oa_.b], dma="m_oa%d" % (k % 2))
            S("sp", lambda e, ob_=ob_, k=k: e.dma_start(out=ob_[:], in_=g.OBT.ap[:, :, k * 512:(k + 1) * 512].rearrange("h p t -> p h t")),
              reads=g.OBT.allbufs(), writes=[ob_.b], dma="m_ob%d" % (k % 2))
            for dq in range(4):
                ga_, gb_, mo_ = ga[nq % 2], gb[nq % 2], mo[nq % 2]
                S("sp", lambda e, ga_=ga_, k=k, dq=dq: e.dma_start(out=ga_[:], in_=g.GAT.ap[4 * dq:4 * dq + 4, :, k * 512:(k + 1) * 512].rearrange("c p t -> p c t")),
                  reads=g.GAT.allbufs(), writes=[ga_.b], dma="m_ga%d" % (nq % 2))
                S("sp", lambda e, gb_=gb_, k=k, dq=dq: e.dma_start(out=gb_[:], in_=g.GBT.ap[4 * dq:4 * dq + 4, :, k * 512:(k + 1) * 512].rearrange("c p t -> p c t")),
                  reads=g.GBT.allbufs(), writes=[gb_.b], dma="m_gb%d" % (nq % 2))
                nq += 1
                for dl in range(4):
                    dc = 4 * dq + dl
                    pa, pb = psA[dc % 2], psB[dc % 2]
                    ta_, tb2 = ta[dc % 2], tb_[dc % 2]
                    for hc in range(8):
                        S("pe", lambda e, pa=pa, hc=hc, dc=dc, oa_=oa_: e.matmul(pa[:, :], wab[:, hc, dc * 128:(dc + 1) * 128], oa_[:, hc, :], start=(hc == 0), stop=(hc == 7)),
                          reads=[wab.b, oa_.b], writes=[pa.b], pe_acc=True)
                    for hc in range(8):
                        S("pe", lambda e, pb=pb, hc=hc, dc=dc, ob_=ob_: e.matmul(pb[:, :], wbb[:, hc, dc * 128:(dc + 1) * 128], ob_[:, hc, :], start=(hc == 0), stop=(hc == 7)),
                          reads=[wbb.b, ob_.b], writes=[pb.b], pe_acc=True)
                    S("dve", lambda e, pa=pa, ta_=ta_, ga_=ga_, dl=dl: e.tensor_tensor(out=ta_[:], in0=pa[:, :], in1=ga_[:, dl, :], op=ALU.mult), reads=[pa.b, ga_.b], writes=[ta_.b])
                    S("dve", lambda e, pb=pb, tb2=tb2, gb_=gb_, dl=dl: e.tensor_tensor(out=tb2[:], in0=pb[:, :], in1=gb_[:, dl, :], op=ALU.mult), reads=[pb.b, gb_.b], writes=[tb2.b])
                    S("pool", lambda e, ta_=ta_, tb2=tb2, mo_=mo_, dl=dl: e.tensor_tensor(out=mo_[:, dl, :], in0=ta_[:], in1=tb2[:], op=ALU.add), reads=[ta_.b, tb2.b], writes=[mo_.b])
                S("sp", lambda e, mo_=mo_, k=k, dq=dq: e.dma_start(out=g.MT.ap[4 * dq:4 * dq + 4, :, k * 512:(k + 1) * 512].rearrange("c p t -> p c t"), in_=mo_[:]),
                  reads=[mo_.b], writes=[g.MT.buf((k, dq))], dma="m_st")
        barrier(g)
    with contextlib.ExitStack() as ls:
        wob = T(ls, nc, "wob", [128, 16, D], BF16)
        g.rows = {}
        for nm_ in ("gt1", "a2", "sh2"):
            g.rows[nm_] = T(ls, nc, "row_" + nm_, [128, D], F32)
        load_rows(g, ls, ("gt1", "a2", "sh2"))
        with contextlib.ExitStack() as l2:
            stg = [T(l2, nc, "wostg%d" % i, [128, 16, 512], F32) for i in range(2)]
            for cs in range(4):
                sg = stg[cs % 2]
                S("sp", lambda e, sg=sg, cs=cs: e.dma_start(out=sg[:], in_=g.inp["wout"].ap[:, cs * 512:(cs + 1) * 512].rearrange("(c p) j -> p c j", p=128)),
                  writes=[sg.b], dma="wostg%d" % (cs % 2))
                S("act", lambda e, sg=sg, cs=cs: e.copy(wob[:, 0:8, cs * 512:(cs + 1) * 512], sg[:, 0:8, :]), reads=[sg.b], writes=[wob.b])
                S("dve", lambda e, sg=sg, cs=cs: e.tensor_copy(wob[:, 8:16, cs * 512:(cs + 1) * 512], sg[:, 8:16, :]), reads=[sg.b], writes=[wob.b])
            barrier(g)
        wr = T(ls, nc, "wr", [128, 16, 72], F32)
        brr = T(ls, nc, "brr", [128, 72], F32)
        S("sp", lambda e: e.dma_start(out=wr[:], in_=g.inp["wr"].ap.rearrange("(c p) j -> p c j", p=128)), writes=[wr.b], dma=g.uniq())
        S("sp", lambda e: e.dma_start(out=brr[:], in_=g.inp["br"].ap.partition_broadcast(128)), writes=[brr.b], dma=g.uniq())
        mt = [T(ls, nc, "r_mt%d" % i, [128, 16, 128], BF16) for i in range(2)]
        xb = [T(ls, nc, "r_xb%d" % i, [128, D], F32) for i in range(2)]
        hn = T(ls, nc, "r_hn", [128, D], F32)
        hnb = [T(ls, nc, "r_hnb%d" % i, [128, D], BF16) for i in range(2)]
        hnT = T(ls, nc, "r_hnT", [128, 16, 128], F32)
        junk = T(ls, nc, "r_junk", [128, D], BF16)
        ss = T(ls, nc, "r_ss", [128, 1], F32)
        lg = T(ls, nc, "r_lg", [128, 72], F32)
        sm = T(ls, nc, "r_sm", [128, 16], F32)
        oh8 = T(ls, nc, "r_oh8", [128, 8], F32)
        lem = T(ls, nc, "r_lem", [128, 64], F32)
        top8 = T(ls, nc, "r_top8", [128, 8], F32)
        A1 = T(ls, nc, "r_A1", [128, 64], F32)
        A2 = T(ls, nc, "r_A2", [128, 64], F32)
        Ab = T(ls, nc, "r_Ab", [128, 64], BF16)
        Acum = T(ls, nc, "r_Acum", [128, 64], BF16)
        t64 = T(ls, nc, "r_t64", [128, 64], F32)
        j64 = T(ls, nc, "r_j64", [128, 64], F32)
        dsf = T(ls, nc, "r_dsf", [128, 8], F32)
        psy = [T(ls, nc, "r_py%d" % i, [128, 512], F32, psum=True) for i in range(4)]
        pst = [T(ls, nc, "r_pt%d" % i, [128, 512], F32, psum=True) for i in range(2)]
        psl = T(ls, nc, "r_pl", [128, 512], F32, psum=True)
        S("dve", lambda e: e.memset(Acum[:], 0.0), writes=[Acum.b])
        ltri = cb[:, CB_LTRI:CB_LTRI + 128]
        ones_m = cb[:, CB_ONES:CB_ONES + 128]
        xv = g.inp["xv"].ap
        for lb in range(NQB):
            k, c = lb // 4, lb % 4
            r0 = (2 * k + 1) * 512 + c * 128
            mt_, xb_, hnb_ = mt[lb % 2], xb[lb % 2], hnb[lb % 2]
            S("sp", lambda e, mt_=mt_, lb=lb: e.dma_start(out=mt_[:], in_=g.MT.ap[:, :, lb * 128:(lb + 1) * 128].rearrange("c p t -> p c t")),
              reads=g.MT.allbufs(), writes=[mt_.b], dma="r_mt%d" % (lb % 2))
            S("sp", lambda e, xb_=xb_, r0=r0: e.dma_start(out=xb_[:], in_=xv[r0:r0 + 128, :]), writes=[xb_.b], dma="r_xb%d" % (lb % 2))
            for oc in range(4):
                for dc in range(16):
                    S("pe", lambda e, oc=oc, dc=dc, mt_=mt_: e.matmul(psy[oc][:, :], mt_[:, dc, :], wob[:, dc, oc * 512:(oc + 1) * 512], start=(dc == 0), stop=(dc == 15)),
                      reads=[mt_.b, wob.b], writes=[psy[oc].b], pe_acc=True)
                S("dve", lambda e, oc=oc: e.tensor_tensor(out=hn[:, oc * 512:(oc + 1) * 512], in0=psy[oc][:, :], in1=g.rows["gt1"][:, oc * 512:(oc + 1) * 512], op=ALU.mult),
                  reads=[psy[oc].b, g.rows["gt1"].b], writes=[hn.b])
            S("pool", lambda e, xb_=xb_: e.tensor_tensor(out=xb_[:], in0=xb_[:], in1=hn[:], op=ALU.add), reads=[xb_.b, hn.b], writes=[xb_.b])
            S("sp", lambda e, xb_=xb_, lb=lb: e.dma_start(out=g.X1.ap[lb * 128:(lb + 1) * 128, :], in_=xb_[:]), reads=[xb_.b], writes=[g.X1.buf(lb)], dma="r_x1")
            S("act", lambda e, xb_=xb_: e.activation(out=junk[:], in_=xb_[:], func=AF.Square, accum_out=ss[:]), reads=[xb_.b], writes=[junk.b, ss.b])
            S("dve", lambda e: e.tensor_scalar(out=ss[:], in0=ss[:], scalar1=1.0 / D, scalar2=1e-6, op0=ALU.mult, op1=ALU.add), reads=[ss.b], writes=[ss.b])
            S("act", lambda e: e.activation(out=ss[:], in_=ss[:], func=AF.Sqrt), reads=[ss.b], writes=[ss.b])
            S("dve", lambda e: e.reciprocal(out=ss[:], in_=ss[:]), reads=[ss.b], writes=[ss.b])
            S("dve", lambda e, xb_=xb_: e.scalar_tensor_tensor(out=hn[:], in0=xb_[:], scalar=ss[:, 0:1], in1=g.rows["a2"][:], op0=ALU.mult, op1=ALU.mult),
              reads=[xb_.b, ss.b, g.rows["a2"].b], writes=[hn.b])
            S("pool", lambda e: e.tensor_tensor(out=hn[:], in0=hn[:], in1=g.rows["sh2"][:], op=ALU.add), reads=[hn.b, g.rows["sh2"].b], writes=[hn.b])
            S("act", lambda e, hnb_=hnb_: e.copy(hnb_[:], hn[:]), reads=[hn.b], writes=[hnb_.b])
            for q4 in range(4):
                pt = pst[q4 % 2]
                for j in range(4):
                    dc = q4 * 4 + j
                    S("pe", lambda e, pt=pt, j=j, dc=dc: e.transpose(pt[:, j * 128:(j + 1) * 128], hn[:, dc * 128:(dc + 1) * 128], idf[:]),
                      reads=[hn.b, idf.b], writes=[pt.b], pe_acc=(j > 0))
                if q4 % 2 == 0:
                    S("act", lambda e, pt=pt, q4=q4: e.copy(hnT[:, q4 * 4:(q4 + 1) * 4, :].rearrange("p a b -> p (a b)"), pt[:, :]), reads=[pt.b], writes=[hnT.b])
                else:
                    S("dve", lambda e, pt=pt, q4=q4: e.tensor_copy(hnT[:, q4 * 4:(q4 + 1) * 4, :].rearrange("p a b -> p (a b)"), pt[:, :]), reads=[pt.b], writes=[hnT.b])
            for dc in range(16):
                S("pe", lambda e, dc=dc: e.matmul(psl[:, 0:72], hnT[:, dc, :], wr[:, dc, :], start=(dc == 0), stop=(dc == 15)),
                  reads=[hnT.b, wr.b], writes=[psl.b], pe_acc=(dc > 0))
            S("dve", lambda e: e.tensor_tensor(out=lg[:], in0=psl[:, 0:72], in1=brr[:], op=ALU.add), reads=[psl.b, brr.b], writes=[lg.b])
            S("dve", lambda e: e.tensor_reduce(out=sm[:, 0:1], in_=lg[:, 0:8], axis=AX.X, op=ALU.max), reads=[lg.b], writes=[sm.b])
            S("dve", lambda e: e.tensor_scalar(out=sm[:, 1:2], in0=sm[:, 0:1], scalar1=-1.0, scalar2=None, op0=ALU.mult), reads=[sm.b], writes=[sm.b])
            S("dve", lambda e: e.tensor_scalar(out=oh8[:], in0=lg[:, 0:8], scalar1=sm[:, 0:1], scalar2=None, op0=ALU.is_equal), reads=[lg.b, sm.b], writes=[oh8.b])
            S("act", lambda e: e.activation(out=j64[:, 0:8], in_=lg[:, 0:8], func=AF.Exp, bias=sm[:, 1:2], accum_out=sm[:, 2:3]), reads=[lg.b, sm.b], writes=[j64.b, sm.b])
            S("dve", lambda e: e.tensor_scalar(out=oh8[:], in0=oh8[:], scalar1=-1.0, scalar2=1e30, op0=ALU.add, op1=ALU.mult), reads=[oh8.b], writes=[oh8.b])
            for gq in range(8):
                S("dve", lambda e, gq=gq: e.tensor_scalar(out=lem[:, gq * 8:(gq + 1) * 8], in0=lg[:, 8 + gq * 8:16 + gq * 8], scalar1=oh8[:, gq:gq + 1], scalar2=None, op0=ALU.add),
                  reads=[lg.b, oh8.b], writes=[lem.b])
            S("dve", lambda e: e.max(out=top8[:], in_=lem[:]), reads=[lem.b], writes=[top8.b])
            S("dve", lambda e: e.tensor_scalar(out=A1[:], in0=lem[:], scalar1=top8[:, 0:1], scalar2=None, op0=ALU.is_equal), reads=[lem.b, top8.b], writes=[A1.b])
            S("dve", lambda e: e.tensor_scalar(out=A2[:], in0=lem[:], scalar1=top8[:, 1:2], scalar2=None, op0=ALU.is_equal), reads=[lem.b, top8.b], writes=[A2.b])
            S("dve", lambda e: e.tensor_tensor(out=Ab[:], in0=A1[:], in1=A2[:], op=ALU.add), reads=[A1.b, A2.b], writes=[Ab.b])
            S("dve", lambda e: e.tensor_scalar(out=sm[:, 3:4], in0=top8[:, 0:1], scalar1=-1.0, scalar2=None, op0=ALU.mult), reads=[top8.b], writes=[sm.b])
            S("act", lambda e: e.activation(out=sm[:, 4:5], in_=top8[:, 1:2], func=AF.Exp, bias=sm[:, 3:4]), reads=[top8.b, sm.b], writes=[sm.b])
            S("dve", lambda e: e.scalar_tensor_tensor(out=sm[:, 5:6], in0=sm[:, 4:5], scalar=1.0, in1=sm[:, 2:3], op0=ALU.add, op1=ALU.mult), reads=[sm.b], writes=[sm.b])
            S("dve", lambda e: e.reciprocal(out=sm[:, 6:7], in_=sm[:, 5:6]), reads=[sm.b], writes=[sm.b])
            S("dve", lambda e: e.tensor_tensor(out=sm[:, 7:8], in0=sm[:, 6:7], in1=sm[:, 4:5], op=ALU.mult), reads=[sm.b], writes=[sm.b])
            S("pe", lambda e, lb=lb: e.matmul(psl[:, 128:192], ltri, Ab[:], start=True, stop=(lb == 0)), reads=[cb.b, Ab.b], writes=[psl.b])
            if lb > 0:
                S("pe", lambda e: e.matmul(psl[:, 128:192], ones_m, Acum[:], start=False, stop=True), reads=[cb.b, Acum.b], writes=[psl.b], pe_acc=True)
            S("dve", lambda e: e.tensor_tensor(out=t64[:], in0=psl[:, 128:192], in1=cf[:, CF_EOFF:CF_EOFF + 64], op=ALU.add), reads=[psl.b, cf.b], writes=[t64.b])
            for j, Aj in enumerate((A1, A2)):
                S("dve", lambda e, Aj=Aj, j=j: e.scalar_tensor_tensor(out=j64[:], in0=t64[:], scalar=1.0, in1=Aj[:], op0=ALU.mult, op1=ALU.mult, accum_out=dsf[:, j:j + 1]),
                  reads=[t64.b, Aj.b], writes=[j64.b, dsf.b])
                S("dve", lambda e, Aj=Aj, j=j: e.scalar_tensor_tensor(out=j64[:], in0=psl[:, 128:192], scalar=1.0, in1=Aj[:], op0=ALU.mult, op1=ALU.mult, accum_out=dsf[:, 2 + j:3 + j]),
                  reads=[psl.b, Aj.b], writes=[j64.b, dsf.b])
                S("dve", lambda e, j=j: e.tensor_scalar(out=dsf[:, 4 + j:5 + j], in0=dsf[:, 2 + j:3 + j], scalar1=CAP - 0.5, scalar2=1e9, op0=ALU.is_ge, op1=ALU.mult),
                  reads=[dsf.b], writes=[dsf.b])
                S("dve", lambda e, j=j: e.tensor_tensor(out=dsf[:, j:j + 1], in0=dsf[:, j:j + 1], in1=dsf[:, 4 + j:5 + j], op=ALU.add), reads=[dsf.b], writes=[dsf.b])
            S("dve", lambda e: e.tensor_tensor(out=Acum[:], in0=Acum[:], in1=Ab[:], op=ALU.add), reads=[Acum.b, Ab.b], writes=[Acum.b])
            S("dve", lambda e, lb=lb: e.tensor_copy(g.dst[:, lb, :], dsf[:, 0:2]), reads=[dsf.b], writes=[g.dst.b])
            S("dve", lambda e, lb=lb: e.tensor_copy(g.wts[:, lb, :], sm[:, 6:8]), reads=[sm.b], writes=[g.wts.b])
            for j in range(2):
                S("pool", lambda e, lb=lb, j=j, hnb_=hnb_: e.indirect_dma_start(
                    out=g.XB.ap, out_offset=bass.IndirectOffsetOnAxis(ap=g.dst[:, lb, j:j + 1], axis=0), in_=hnb_[:], in_offset=None,
                    bounds_check=g.breg, oob_is_err=False),
                  reads=[hnb_.b, g.dst.b], writes=[g.XB.buf()], dma="r_sc")
            if "RT" in g.taps:
                S("dve", lambda e: e.tensor_copy(dsf[:, 2:4], sm[:, 6:8]), reads=[sm.b, dsf.b], writes=[dsf.b])
                S("sp", lambda e, lb=lb: e.dma_start(out=g.RT.ap[lb], in_=dsf[:, 0:4]), reads=[dsf.b], writes=[g.RT.buf(lb)], dma="r_rt")
        barrier(g)


def phase6(g):
    nc, sch, st = g.nc, g.sch, g.st
    S = sch.issue
    CAP = g.CAP
    NSB = CAP // 128
    idb = g.idb
    g.YB = g.dr("YB", [NEXP * CAP, D], F32)
    ew1, ew3, ew2 = g.inp["ew1"].ap, g.inp["ew3"].ap, g.inp["ew2"].ap
    with contextlib.ExitStack() as ls:
        w1s = [T(ls, nc, "e_w1s%d" % i, [128, D], F32) for i in range(2)]
        w3s = [T(ls, nc, "e_w3s%d" % i, [128, D], F32) for i in range(2)]
        w2s = [T(ls, nc, "e_w2s%d" % i, [128, D], F32) for i in range(2)]
        w1b = [T(ls, nc, "e_w1b%d" % i, [128, 16, 128], BF16) for i in range(2)]
        w3b = [T(ls, nc, "e_w3b%d" % i, [128, 16, 128], BF16) for i in range(2)]
        w2b = T(ls, nc, "e_w2b", [128, NFC, D], BF16)
        xet = T(ls, nc, "e_xet", [128, 16, CAP], BF16)
        xr = [T(ls, nc, "e_xr%d" % i, [128, D], BF16) for i in range(2)]
        hid = T(ls, nc, "e_hid", [128, NFC, CAP], BF16)
        sl = T(ls, nc, "e_sl", [128, CAP], F32)
        yst = [T(ls, nc, "e_yst%d" % i, [128, D], F32) for i in range(2)]
        h1 = T(ls, nc, "e_h1", [128, 512], F32, psum=True)
        h3 = T(ls, nc, "e_h3", [128, 512], F32, psum=True)
        ptr = [T(ls, nc, "e_pt%d" % i, [128, 1024], BF16, psum=True) for i in range(2)]
        py = [T(ls, nc, "e_py%d" % i, [128, 512], F32, psum=True) for i in range(4)]
        steps = [(e_, fc) for e_ in range(NEXP) for fc in range(NFC)]

        def loads(i):
            e_, fc = steps[i]
            S("sp", lambda e: e.dma_start(out=w1s[i % 2][:], in_=ew1[e_, fc]), writes=[w1s[i % 2].b], dma="e_w1s%d" % (i % 2))
            S("sp", lambda e: e.dma_start(out=w3s[i % 2][:], in_=ew3[e_, fc]), writes=[w3s[i % 2].b], dma="e_w3s%d" % (i % 2))
            S("sp", lambda e: e.dma_start(out=w2s[i % 2][:], in_=ew2[e_, fc * 128:(fc + 1) * 128, :]), writes=[w2s[i % 2].b], dma="e_w2s%d" % (i % 2))

        loads(0)
        nx = 0
        ny = 0
        for i, (e_, fc) in enumerate(steps):
            if i + 1 < len(steps):
                loads(i + 1)
            if fc == 0:
                for sb in range(NSB):
                    xr_ = xr[nx % 2]
                    S("sp", lambda e, xr_=xr_, sb=sb: e.dma_start(out=xr_[:], in_=g.XB.ap[e_ * CAP + sb * 128:e_ * CAP + (sb + 1) * 128, :]),
                      reads=g.XB.allbufs(), writes=[xr_.b], dma="e_xr%d" % (nx % 2))
                    nx += 1
                    for hq in range(2):
                        pt = ptr[hq]
                        for j in range(8):
                            dc = hq * 8 + j
                            S("pe", lambda e, pt=pt, j=j, dc=dc, xr_=xr_: e.transpose(pt[:, j * 128:(j + 1) * 128], xr_[:, dc * 128:(dc + 1) * 128], idb[:]),
                              reads=[xr_.b, idb.b], writes=[pt.b], pe_acc=(j > 0))
                        if hq == 0:
                            S("act", lambda e, pt=pt, sb=sb: e.copy(xet[:, 0:8, sb * 128:(sb + 1) * 128], pt[:, :].rearrange("p (a b) -> p a b", b=128)),
                              reads=[pt.b], writes=[xet.b])
                        else:
                            S("dve", lambda e, pt=pt, sb=sb: e.tensor_copy(xet[:, 8:16, sb * 128:(sb + 1) * 128], pt[:, :].rearrange("p (a b) -> p a b", b=128)),
                              reads=[pt.b], writes=[xet.b])
            a, b3 = w1b[i % 2], w3b[i % 2]
            S("act", lambda e, a=a: e.copy(a[:].rearrange("p a b -> p (a b)"), w1s[i % 2][:]), reads=[w1s[i % 2].b], writes=[a.b])
            S("dve", lambda e, b3=b3: e.tensor_copy(b3[:].rearrange("p a b -> p (a b)"), w3s[i % 2][:]), reads=[w3s[i % 2].b], writes=[b3.b])
            S("pool", lambda e, fc=fc: e.tensor_copy(w2b[:, fc, :], w2s[i % 2][:]), reads=[w2s[i % 2].b], writes=[w2b.b])
            for dc in range(16):
                S("pe", lambda e, dc=dc, a=a: e.matmul(h1[:, 0:CAP], a[:, dc, :], xet[:, dc, :], start=(dc == 0), stop=(dc == 15)),
                  reads=[a.b, xet.b], writes=[h1.b], pe_acc=(dc > 0))
            for dc in range(16):
                S("pe", lambda e, dc=dc, b3=b3: e.matmul(h3[:, 0:CAP], b3[:, dc, :], xet[:, dc, :], start=(dc == 0), stop=(dc == 15)),
                  reads=[b3.b, xet.b], writes=[h3.b], pe_acc=(dc > 0))
            S("act", lambda e: e.activation(out=sl[:], in_=h1[:, 0:CAP], func=AF.Silu), reads=[h1.b], writes=[sl.b])
            S("dve", lambda e, fc=fc: e.tensor_tensor(out=hid[:, fc, :], in0=h3[:, 0:CAP], in1=sl[:], op=ALU.mult), reads=[h3.b, sl.b], writes=[hid.b])
            if fc == NFC - 1:
                for sb in range(NSB):
                    ys = yst[ny % 2]
                    ny += 1
                    for oc in range(4):
                        for f2 in range(NFC):
                            S("pe", lambda e, oc=oc, f2=f2, sb=sb: e.matmul(py[oc][:, :], hid[:, f2, sb * 128:(sb + 1) * 128], w2b[:, f2, oc * 512:(oc + 1) * 512],
                                                                        start=(f2 == 0), stop=(f2 == NFC - 1)),
                              reads=[hid.b, w2b.b], writes=[py[oc].b], pe_acc=(f2 > 0))
                        if oc % 2 == 0:
                            S("act", lambda e, oc=oc, ys=ys: e.copy(ys[:, oc * 512:(oc + 1) * 512], py[oc][:, :]), reads=[py[oc].b], writes=[ys.b])
                        else:
                            S("dve", lambda e, oc=oc, ys=ys: e.tensor_copy(ys[:, oc * 512:(oc + 1) * 512], py[oc][:, :]), reads=[py[oc].b], writes=[ys.b])
                    S("sp", lambda e, ys=ys, sb=sb: e.dma_start(out=g.YB.ap[e_ * CAP + sb * 128:e_ * CAP + (sb + 1) * 128, :], in_=ys[:]),
                      reads=[ys.b], writes=[g.YB.buf(e_)], dma="e_yst")
        barrier(g)


def phase7(g):
    nc, sch, st = g.nc, g.sch, g.st
    S = sch.issue
    NQB, CAP = g.NQB, g.CAP
    with contextlib.ExitStack() as ls:
        g.rows = {}
        for nm_ in ("gt2", "gf"):
            g.rows[nm_] = T(ls, nc, "row_" + nm_, [128, D], F32)
        load_rows(g, ls, ("gt2", "gf"))
        y = [[T(ls, nc, "f_y%d_%d" % (i, j), [128, D], F32) for j in range(2)] for i in range(2)]
        x1 = [T(ls, nc, "f_x1_%d" % i, [128, D], F32) for i in range(2)]
        mo = T(ls, nc, "f_mo", [128, D], F32)
        junk = T(ls, nc, "f_junk", [128, D], BF16)
        ss = T(ls, nc, "f_ss", [128, 1], F32)
        ob = [T(ls, nc, "f_ob%d" % i, [128, D], F32) for i in range(2)]
        for lb in range(NQB):
            i2 = lb % 2
            x_ = x1[i2]
            S("sp", lambda e, x_=x_, lb=lb: e.dma_start(out=x_[:], in_=g.X1.ap[lb * 128:(lb + 1) * 128, :]), reads=[g.X1.buf(lb)], writes=[x_.b], dma="f_x%d" % i2)
            for j in range(2):
                yj = y[i2][j]
                S("pool", lambda e, yj=yj: e.memset(yj[:], 0.0), writes=[yj.b])
                S("pool", lambda e, yj=yj, lb=lb, j=j: e.indirect_dma_start(
                    out=yj[:], out_offset=None, in_=g.YB.ap, in_offset=bass.IndirectOffsetOnAxis(ap=g.dst[:, lb, j:j + 1], axis=0),
                    bounds_check=g.breg, oob_is_err=False),
                  reads=g.YB.allbufs() + [g.dst.b], writes=[yj.b], dma="f_y%d_%d" % (i2, j))
            S("dve", lambda e, i2=i2, lb=lb: e.tensor_scalar(out=mo[:], in0=y[i2][0][:], scalar1=g.wts[:, lb, 0:1], scalar2=None, op0=ALU.mult),
              reads=[y[i2][0].b, g.wts.b], writes=[mo.b])
            S("dve", lambda e, i2=i2, lb=lb: e.scalar_tensor_tensor(out=mo[:], in0=y[i2][1][:], scalar=g.wts[:, lb, 1:2], in1=mo[:], op0=ALU.mult, op1=ALU.add),
              reads=[y[i2][1].b, g.wts.b, mo.b], writes=[mo.b])
            S("pool", lambda e: e.tensor_tensor(out=mo[:], in0=mo[:], in1=g.rows["gt2"][:], op=ALU.mult), reads=[mo.b, g.rows["gt2"].b], writes=[mo.b])
            S("pool", lambda e, x_=x_: e.tensor_tensor(out=x_[:], in0=x_[:], in1=mo[:], op=ALU.add), reads=[x_.b, mo.b], writes=[x_.b])
            S("act", lambda e, x_=x_: e.activation(out=junk[:], in_=x_[:], func=AF.Square, accum_out=ss[:]), reads=[x_.b], writes=[junk.b, ss.b])
            S("dve", lambda e: e.tensor_scalar(out=ss[:], in0=ss[:], scalar1=1.0 / D, scalar2=1e-6, op0=ALU.mult, op1=ALU.add), reads=[ss.b], writes=[ss.b])
            S("act", lambda e: e.activation(out=ss[:], in_=ss[:], func=AF.Sqrt), reads=[ss.b], writes=[ss.b])
            S("dve", lambda e: e.reciprocal(out=ss[:], in_=ss[:]), reads=[ss.b], writes=[ss.b])
            o_ = ob[i2]
            S("dve", lambda e, x_=x_, o_=o_: e.scalar_tensor_tensor(out=o_[:], in0=x_[:], scalar=ss[:, 0:1], in1=g.rows["gf"][:], op0=ALU.mult, op1=ALU.mult),
              reads=[x_.b, ss.b, g.rows["gf"].b], writes=[o_.b])
            S("sp", lambda e, o_=o_, lb=lb: e.dma_start(out=g.out.ap[lb * 128:(lb + 1) * 128, :], in_=o_[:]), reads=[o_.b], writes=[g.out.buf(lb)], dma="f_out")
        barrier(g)

def bf(a):
    return np.ascontiguousarray(a).astype(ml_dtypes.bfloat16)


def host_consts(S, half, CAP):
    SV = S + 512
    c = {}
    c["c_idf"] = np.eye(128, dtype=np.float32)
    c["c_idb"] = bf(np.eye(128))
    cf = np.zeros((128, 1024), np.float32)
    cb = np.zeros((128, 4096), np.float32)
    if half == 0:
        cf[:, CF_KB0:CF_KB0 + 4] = NEG
        cf[0:32, CF_CPAD] = NEG
    cf[:, CF_ONE] = 1.0
    p = np.arange(128)[:, None]
    q = np.arange(512)[None, :]
    for dg in range(4):
        cb[:, CB_CAUS + dg * 512:CB_CAUS + (dg + 1) * 512] = ((128 * dg + p) < q)
    jj = np.arange(128)[:, None]
    ss = np.arange(128)[None, :]
    cb[:, CB_NEGU:CB_NEGU + 128] = -(jj > ss).astype(np.float32)
    cb[:, CB_NEG1:CB_NEG1 + 128] = -1.0
    NSV = SV // 64
    kq = np.arange(128)
    wm = np.where(kq[None, :] < kq[:, None], 0.0, NEG)
    cb[:, CB_WM4:CB_WM4 + 512] = np.tile(wm, (1, 4))
    cb[0, CB_ONESROW:CB_ONESROW + 128] = 1.0
    if half == 0:
        cb[0, CB_CPADROW:CB_CPADROW + 32] = NEG
    for m in range(17):
        cb[m, CB_WI + m + 128] = 1.0
    cb[:, CB_ONESCOL] = 1.0
    cb[:, CB_ONES:CB_ONES + 128] = 1.0
    if half == 0:
        cf[:, CF_PADNEG:CF_PADNEG + 8] = -1e30
        cf[:, CF_F0 + 8] = 1e4
    else:
        cf[:, CF_F0 + 0] = 1e4
    cf[:, CF_EOFF:CF_EOFF + 64] = (np.arange(64) * CAP)[None, :]
    tt = np.arange(128)
    cb[:, CB_LTRI:CB_LTRI + 128] = (tt[:, None] < tt[None, :])
    c["c_f32"] = cf
    c["c_bf"] = bf(cb)
    k = np.arange(SV)
    ex = ((k[None, :] // 64) % 128 == np.arange(128)[:, None]).astype(np.float32)
    c["c_exp"] = bf(ex)
    c["c_oh"] = bf(onehot_planes())
    return c


def t5_bucket_np(n):
    n = np.maximum(np.asarray(n), 0)
    nf = np.maximum(n, 1).astype(np.float32)
    large = 16 + (np.log(nf / np.float32(16)) / np.float32(np.log(8.0)) * np.float32(16)).astype(np.int32)
    large = np.minimum(large, 31)
    return np.where(n < 16, n, large)


def onehot_planes():
    oh = np.zeros((128, OH_W), np.float32)
    k = np.arange(128)[:, None]
    q = np.arange(128)[None, :]
    d0 = q - k
    s0 = np.zeros((128, 33, 128), np.float32)
    b0 = t5_bucket_np(d0)
    for b in range(32):
        s0[:, b, :] = (d0 >= 0) & (b0 == b)
    s0[:, 32, :] = d0 < 0
    d1 = 128 + q - k
    s1 = np.zeros((128, 32, 128), np.float32)
    b1 = t5_bucket_np(d1)
    for b in range(32):
        s1[:, b, :] = (d1 < 128) & (b1 == b)
    r = np.arange(128)[:, None]
    m = np.arange(17)[None, :]
    dc = r - 16 * m + 129
    sc = np.zeros((128, 33, 17), np.float32)
    bc = t5_bucket_np(dc)
    for b in range(32):
        sc[:, b, :] = (dc >= 0) & (dc < 128) & (bc == b)
    sc[:, 32, :] = dc < 0
    oh[:, OH_S0:OH_S0 + 33 * 128] = s0.reshape(128, -1)
    oh[:, OH_S1:OH_S1 + 32 * 128] = s1.reshape(128, -1)
    oh[:, OH_BC:OH_BC + 33 * 17] = sc.reshape(128, -1)
    return oh


def padded_x(inp, S, b, half):
    xv = np.zeros((S + 512, D), np.float32)
    off = 512 * (1 - half)
    xv[off:off + S] = inp["x"][b]
    return xv


def prep_core_inputs(inp, S, b, half, shared, CAP, nhalf=1):
    m = dict(shared)
    m["cT"] = np.ascontiguousarray(inp["c"][b].reshape(16, 128).T)
    if nhalf == 1:
        m["xv"] = padded_x(inp, S, b, half)
        m.update(host_consts(S, half, CAP))
    else:
        for h in range(2):
            m["xv_h%d" % h] = padded_x(inp, S, b, h)
            hc = host_consts(S, h, CAP)
            m["c_f32_h%d" % h] = hc.pop("c_f32")
            m["c_bf_h%d" % h] = hc.pop("c_bf")
            m.update(hc)
    return m


def prep_shared(inp, names):
    sh = {}
    sh["ada_w"] = inp["ada_w"][0]
    sh["ada_bT"] = np.ascontiguousarray(inp["ada_b"][0].reshape(96, 128).T)
    sh["g1T"] = np.ascontiguousarray(inp["norm1_g"][0].reshape(16, 128).T)
    sh["g2row"] = inp["norm2_g"][0].reshape(1, D)
    sh["gfrow"] = inp["normf_g"].reshape(1, D)
    sh["w_in"] = inp["w_in"][0]
    sh["relb"] = inp["rel_bias"]
    sh["pe_kT"] = np.ascontiguousarray(inp["cmp_pe_k"][0].T)
    sh["pe_vT"] = np.ascontiguousarray(inp["cmp_pe_v"][0].T)
    sh["cw1k"] = inp["cmp_w1_k"][0]
    sh["cw2k"] = inp["cmp_w2_k"][0]
    sh["cw1v"] = inp["cmp_w1_v"][0]
    sh["cw2v"] = inp["cmp_w2_v"][0]
    sh["wba"] = inp["w_branch_a"][0]
    sh["wbb"] = inp["w_branch_b"][0]
    sh["wout"] = inp["w_out"][0]
    sh["wr"] = np.ascontiguousarray(np.concatenate([inp["router_w_grp"][0], inp["router_w_exp"][0]], axis=1))
    sh["br"] = np.concatenate([inp["router_b_grp"][0], inp["router_b_exp"][0]]).reshape(1, 72)
    def relay(w):
        w = w.reshape(NEXP, NDC, 128, NFC, 128)
        return np.ascontiguousarray(w.transpose(0, 3, 2, 1, 4)).reshape(NEXP, NFC, 128, NDC * 128)
    if "ew1" in names:
        sh["ew1"] = relay(inp["expert_w1"][0])
        sh["ew3"] = relay(inp["expert_w3"][0])
        sh["ew2"] = inp["expert_w2"][0]
    return sh


def run(inp, S, B, CAP, taps=(), upto=99, nhalf=1):
    inp = {k: np.asarray(v) for k, v in inp.items()}
    nc = build_program(S, CAP, taps=taps, upto=upto, nhalf=nhalf)
    shared = prep_shared(inp, nc._inp_names)
    in_maps = []
    ncores = 2 * B if nhalf == 1 else B
    for core in range(ncores):
        if nhalf == 1:
            m = prep_core_inputs(inp, S, core // 2, core % 2, shared, CAP)
        else:
            m = prep_core_inputs(inp, S, core, None, shared, CAP, nhalf=2)
        in_maps.append({k: m[k] for k in nc._inp_names})
    res = run_bass_kernel_spmd(nc, in_maps, core_ids=list(range(ncores)))
    return res.results


def assemble(results, S, B, nhalf=1):
    out = np.zeros((B, S, D), np.float32)
    NQT = S // 1024
    for core, r in enumerate(results):
        if nhalf == 1:
            parts = [(core // 2, core % 2, r["out"])]
        else:
            parts = [(core, h, r["out_h%d" % h]) for h in range(2)]
        for (b, half, o) in parts:
            for k in range(NQT):
                gt = 2 * k + half
                out[b, gt * 512:(gt + 1) * 512] = o[k * 512:(k + 1) * 512]
    return out


def kernel(**inputs):
    res = run(inputs, 8192, 4, 512, nhalf=1)
    return assemble(res, 8192, 4, nhalf=1)
```

```python
import contextlib
import numpy as np
import ml_dtypes
import concourse.bass as bass
import concourse.mybir as mybir
from concourse.bass_utils import run_bass_kernel_spmd

F32 = mybir.dt.float32
BF16 = mybir.dt.bfloat16
I32 = mybir.dt.int32
U32 = mybir.dt.uint32
AF = mybir.ActivationFunctionType
ALU = mybir.AluOpType
AX = mybir.AxisListType

D = 2048
NDC = 16
DH = 128
IN_COLS = 9752
NEXP = 64
EH = 1408
NFC = 11
SCALE = 128 ** -0.5
NEG = -30000.0


class Buf:
    __slots__ = ("name", "last_w", "readers")

    def __init__(self, name=""):
        self.name = name
        self.last_w = None
        self.readers = []


class Sched:
    def __init__(self, nc, stack):
        self.nc = nc
        self.stack = stack
        self.engs = {"pe": nc.tensor, "act": nc.scalar, "dve": nc.vector, "pool": nc.gpsimd, "sp": nc.sync}
        self.sems = {}
        self.cnt = {}
        self.seen = {e: {} for e in self.engs}
        for e in ("pe", "act", "dve", "pool"):
            self.sems[e] = stack.enter_context(nc.semaphore("s_" + e))
            self.cnt[e] = 0
        self.dsems = {}
        self.ninst = 0

    def dma_sem(self, key):
        if key not in self.dsems:
            self.dsems[key] = self.stack.enter_context(self.nc.semaphore("d_%s" % (key,)))
            self.cnt[("d", key)] = 0
        return key

    def _semobj(self, k):
        return self.sems[k] if k in self.sems else self.dsems[k[1]]

    def issue(self, e, fn, reads=(), writes=(), dma=None, pe_acc=False):
        eng = self.engs[e]
        need = {}
        deps = []
        for b in reads:
            if b.last_w is not None:
                deps.append(b.last_w)
        for b in writes:
            if b.last_w is not None:
                deps.append(b.last_w)
            deps.extend(b.readers)
        for (k, v) in deps:
            if pe_acc and k == "pe" and e == "pe":
                continue
            if need.get(k, 0) < v:
                need[k] = v
        for k, v in need.items():
            if self.seen[e].get(k, 0) < v:
                eng.wait_ge(self._semobj(k), v)
                self.seen[e][k] = v
        if dma is not None and isinstance(dma, str) and dma[0] == "u" and dma[1:].isdigit():
            self.dma_sem(dma)
            kk0 = ("d", dma)
            if self.cnt[kk0] > 0 and self.seen[e].get(kk0, 0) < self.cnt[kk0]:
                eng.wait_ge(self.dsems[dma], self.cnt[kk0])
                self.seen[e][kk0] = self.cnt[kk0]
        inst = fn(eng)
        self.ninst += 1
        if dma is not None:
            self.dma_sem(dma)
            kk = ("d", dma)
            self.cnt[kk] += 16
            inst.then_inc(self.dsems[dma], 16)
            op = (kk, self.cnt[kk])
        else:
            self.cnt[e] += 1
            inst.then_inc(self.sems[e], 1)
            op = (e, self.cnt[e])
        for b in writes:
            b.last_w = op
            b.readers = []
        for b in reads:
            if b not in writes:
                b.readers.append(op)
                if len(b.readers) > 48:
                    mx = {}
                    for (k, v) in b.readers:
                        if mx.get(k, 0) < v:
                            mx[k] = v
                    b.readers = list(mx.items())
        return op

    def wait_bufs(self, e, bufs):
        eng = self.engs[e]
        for b in bufs:
            for dep in ([b.last_w] if b.last_w is not None else []) + list(b.readers):
                k, v = dep
                if self.seen[e].get(k, 0) < v:
                    eng.wait_ge(self._semobj(k), v)
                    self.seen[e][k] = v


class T:
    n = 0

    def __init__(self, st, nc, name, shape, dtype, psum=False):
        T.n += 1
        nm = "t%d_%s" % (T.n, name)
        if psum:
            self.t = st.enter_context(nc.psum_tensor(nm, shape, dtype))
        else:
            self.t = st.enter_context(nc.sbuf_tensor(nm, shape, dtype))
        self.b = Buf(name)

    def __getitem__(self, idx):
        return self.t[idx]


class DR:
    def __init__(self, nc, name, shape, dtype, kind="Internal"):
        self.h = nc.dram_tensor(name, list(shape), dtype, kind=kind)
        self.ap = self.h.ap()
        self.bufs = {}
        self.name = name

    def buf(self, key=0):
        if key not in self.bufs:
            self.bufs[key] = Buf("%s_%s" % (self.name, key))
        return self.bufs[key]

    def allbufs(self):
        return list(self.bufs.values())


def own_tiles(S, half):
    NT = S // 512
    return [t for t in range(NT) if ((t % 4) in (0, 3)) == (half == 0)]


class Ctx:
    pass


def build_program(S, CAP, taps=(), upto=99, nhalf=1):
    nc = bass.Bass("TRN2", target_bir_lowering=False)
    g = Ctx()
    g.nc = nc
    g.S, g.CAP = S, CAP
    g.SV = S + 512
    g.NTV = g.SV // 512
    g.NKB = g.SV // 128
    g.NQT = (g.NTV - 1) // 2
    g.NQ = g.NQT * 512
    g.NQB = g.NQ // 128
    g.NCV = g.SV // 16 - 1
    g.NSV = g.SV // 64
    g.taps = set(taps)
    g._u = [0]

    def uniq():
        g._u[0] += 1
        return 'u%d' % (g._u[0] % 16)

    g.uniq = uniq
    g.upto = upto

    g.sfx = ""

    def dr(name, shape, dtype, kind=None):
        if kind is None:
            kind = "ExternalOutput" if name in g.taps else "Internal"
        return DR(nc, name + g.sfx, shape, dtype, kind)

    g.dr = dr
    inp = {}

    def ein(name, shape, dtype=F32):
        inp[name] = DR(nc, name, shape, dtype, "ExternalInput")
        return inp[name]

    g.inp = inp
    sfxs = [""] if nhalf == 1 else ["_h0", "_h1"]
    for sf in sfxs:
        ein("xv" + sf, [g.SV, D])
        ein("c_f32" + sf, [128, 1024])
        ein("c_bf" + sf, [128, 4096], BF16)
    ein("cT", [128, 16])
    ein("ada_w", [D, 6 * D])
    ein("ada_bT", [128, 96])
    ein("g1T", [128, 16])
    ein("g2row", [1, D])
    ein("gfrow", [1, D])
    ein("w_in", [D, IN_COLS])
    ein("relb", [32, 8])
    ein("pe_kT", [128, 32])
    ein("pe_vT", [128, 32])
    ein("cw1k", [4096, 256])
    ein("cw2k", [256, 128])
    ein("cw1v", [4096, 256])
    ein("cw2v", [256, 128])
    ein("wba", [1024, D])
    ein("wbb", [1024, D])
    ein("wout", [D, D])
    ein("wr", [D, 72])
    ein("br", [1, 72])
    if upto >= 7:
        ein("ew1", [NEXP, NFC, 128, NDC * 128])
        ein("ew3", [NEXP, NFC, 128, NDC * 128])
        ein("ew2", [NEXP, EH, D])
    ein("c_idf", [128, 128])
    ein("c_idb", [128, 128], BF16)
    ein("c_exp", [128, g.SV], BF16)
    ein("c_oh", [128, OH_W], BF16)
    g.outs = [DR(nc, "out" + sf, [g.NQ, D], F32, "ExternalOutput") for sf in sfxs]
    g.tapdr = []
    names = list(inp.keys())

    with contextlib.ExitStack() as st:
        g.sch = Sched(nc, st)
        for hi, sf in enumerate(sfxs):
            g.sfx = sf
            g.out = g.outs[hi]
            for nm in ("xv", "c_f32", "c_bf"):
                inp[nm] = inp[nm + sf]
            with contextlib.ExitStack() as hst:
                g.st = hst
                phase0(g)
                if upto >= 1:
                    phase1(g)
                if upto >= 3:
                    phase2(g)
                if upto >= 4:
                    phase3(g)
                if upto >= 5:
                    phase4(g)
                if upto >= 6:
                    phase5(g)
                if upto >= 7:
                    phase6(g)
                    phase7(g)
                barrier(g)
        finish(g)
    nc._inp_names = names
    return nc


def finish(g):
    sch = g.sch
    outs = list(g.outs) + [d for d in g.tapdr]
    for d in outs:
        sch.wait_bufs("sp", d.allbufs())


def phase0(g):
    nc, sch, st = g.nc, g.sch, g.st
    S = sch.issue
    g.idf = T(st, nc, "idf", [128, 128], F32)
    g.idb = T(st, nc, "idb", [128, 128], BF16)
    g.cf = T(st, nc, "cf", [128, 1024], F32)
    g.cb = T(st, nc, "cb", [128, 4096], BF16)
    S("sp", lambda e: e.dma_start(out=g.idf[:], in_=g.inp["c_idf"].ap), writes=[g.idf.b], dma=g.uniq())
    S("sp", lambda e: e.dma_start(out=g.idb[:], in_=g.inp["c_idb"].ap), writes=[g.idb.b], dma=g.uniq())
    S("sp", lambda e: e.dma_start(out=g.cf[:], in_=g.inp["c_f32"].ap), writes=[g.cf.b], dma=g.uniq())
    S("sp", lambda e: e.dma_start(out=g.cb[:], in_=g.inp["c_bf"].ap), writes=[g.cb.b], dma=g.uniq())
    g.modT = T(st, nc, "modT", [128, 96], F32)
    g.a1T = T(st, nc, "a1T", [128, 16], F32)
    g.modD = g.dr("modD", [96, 128], F32)
    if "modD" in g.taps:
        g.tapdr.append(g.modD)
    with contextlib.ExitStack() as ls:
        cT = T(ls, nc, "cT", [128, 16], F32)
        cact = T(ls, nc, "cact", [128, 16], F32)
        abT = T(ls, nc, "abT", [128, 96], F32)
        g1T = T(ls, nc, "g1Tt", [128, 16], F32)
        stg = [T(ls, nc, "adaw%d" % i, [128, 16, 512], F32) for i in range(2)]
        mps = T(ls, nc, "modps", [128, 96], F32, psum=True)
        tps = T(ls, nc, "modtp", [128, 128], F32, psum=True)
        mrow = T(ls, nc, "mrow", [96, 128], F32)
        S("sp", lambda e: e.dma_start(out=cT[:], in_=g.inp["cT"].ap), writes=[cT.b], dma=g.uniq())
        S("sp", lambda e: e.dma_start(out=abT[:], in_=g.inp["ada_bT"].ap), writes=[abT.b], dma=g.uniq())
        S("sp", lambda e: e.dma_start(out=g1T[:], in_=g.inp["g1T"].ap), writes=[g1T.b], dma=g.uniq())
        S("act", lambda e: e.activation(out=cact[:], in_=cT[:], func=AF.Silu), reads=[cT.b], writes=[cact.b])
        aw = g.inp["ada_w"].ap
        for slab in range(24):
            sg = stg[slab % 2]
            src = aw[:, slab * 512:(slab + 1) * 512].rearrange("(dc p) j -> p dc j", p=128)
            S("sp", lambda e, sg=sg, src=src: e.dma_start(out=sg[:], in_=src), writes=[sg.b], dma="adaw%d" % (slab % 2))
            for jl in range(4):
                jc = slab * 4 + jl
                for dc in range(16):
                    S("pe", lambda e, sg=sg, jl=jl, jc=jc, dc=dc: e.matmul(
                        mps[:, jc:jc + 1], sg[:, dc, jl * 128:(jl + 1) * 128], cact[:, dc:dc + 1],
                        start=(dc == 0), stop=(dc == 15)),
                      reads=[sg.b, cact.b], writes=[mps.b], pe_acc=True)
        S("dve", lambda e: e.tensor_tensor(out=g.modT[:], in0=mps[:], in1=abT[:], op=ALU.add),
          reads=[mps.b, abT.b], writes=[g.modT.b])
        S("dve", lambda e: e.tensor_scalar(out=g.a1T[:], in0=g.modT[:, 16:32], scalar1=1.0, scalar2=None, op0=ALU.add),
          reads=[g.modT.b], writes=[g.a1T.b])
        S("dve", lambda e: e.tensor_tensor(out=g.a1T[:], in0=g.a1T[:], in1=g1T[:], op=ALU.mult),
          reads=[g.a1T.b, g1T.b], writes=[g.a1T.b])
        S("pe", lambda e: e.transpose(tps[0:96, :], g.modT[:], g.idf[:]), reads=[g.modT.b, g.idf.b], writes=[tps.b])
        S("act", lambda e: e.copy(mrow[:], tps[0:96, :]), reads=[tps.b], writes=[mrow.b])
        S("sp", lambda e: e.dma_start(out=g.modD.ap, in_=mrow[:]), reads=[mrow.b], writes=[g.modD.buf()], dma=g.uniq())


def load_rows(g, st, names):
    nc, sch = g.nc, g.sch
    S = sch.issue
    md = g.modD.ap

    def rowsrc(k):
        return md[k * 16:(k + 1) * 16, :].rearrange("(o a) b -> o (a b)", o=1).partition_broadcast(128)

    idx = {"gt1": 2, "sh2": 3, "a2": 4, "gt2": 5}
    for nm in names:
        if nm == "gf":
            S("sp", lambda e: e.dma_start(out=g.rows["gf"][:], in_=g.inp["gfrow"].ap.partition_broadcast(128)), writes=[g.rows["gf"].b], dma=g.uniq())
        else:
            S("sp", lambda e, nm=nm: e.dma_start(out=g.rows[nm][:], in_=rowsrc(idx[nm])), reads=[g.modD.buf()], writes=[g.rows[nm].b], dma=g.uniq())
    if "a2" in names:
        with contextlib.ExitStack() as ls:
            g2r = T(ls, nc, "g2r", [128, D], F32)
            S("sp", lambda e: e.dma_start(out=g2r[:], in_=g.inp["g2row"].ap.partition_broadcast(128)), writes=[g2r.b], dma=g.uniq())
            S("dve", lambda e: e.tensor_scalar(out=g.rows["a2"][:], in0=g.rows["a2"][:], scalar1=1.0, scalar2=None, op0=ALU.add),
              reads=[g.rows["a2"].b], writes=[g.rows["a2"].b])
            S("dve", lambda e: e.tensor_tensor(out=g.rows["a2"][:], in0=g.rows["a2"][:], in1=g2r[:], op=ALU.mult),
              reads=[g.rows["a2"].b, g2r.b], writes=[g.rows["a2"].b])
            barrier(g)


def barrier(g):
    sch = g.sch
    for e, eng in sch.engs.items():
        for k in list(sch.sems.keys()):
            v = sch.cnt[k]
            if v > 0 and sch.seen[e].get(k, 0) < v:
                eng.wait_ge(sch.sems[k], v)
                sch.seen[e][k] = v
        for key in list(sch.dsems.keys()):
            kk = ("d", key)
            v = sch.cnt[kk]
            if v > 0 and sch.seen[e].get(kk, 0) < v:
                eng.wait_ge(sch.dsems[key], v)
                sch.seen[e][kk] = v


def phase1(g):
    nc, sch, st = g.nc, g.sch, g.st
    S = sch.issue
    SV, NTV, NKB, NQT, NQ, NQB = g.SV, g.NTV, g.NKB, g.NQT, g.NQ, g.NQB
    g.XNT = g.dr("XNT", [NTV, 128, 16 * 512], BF16)
    xv = g.inp["xv"].ap
    sh1 = g.modT
    with contextlib.ExitStack() as ls:
        xt = [T(ls, nc, "xt%d" % i, [128, D], F32) for i in range(2)]
        junk = T(ls, nc, "junk", [128, D], BF16)
        yb = [T(ls, nc, "yb%d" % i, [128, D], BF16) for i in range(2)]
        ss = [T(ls, nc, "ss%d" % i, [128, 1], F32) for i in range(2)]
        xn = [T(ls, nc, "xn%d" % i, [128, 16, 512], BF16) for i in range(2)]
        tp = [T(ls, nc, "tp%d" % i, [128, 2048], BF16, psum=True) for i in range(2)]
        for tb in range(NKB):
            Tt, sub = tb // 4, tb % 4
            x_, y_, s_, p_ = xt[tb % 2], yb[tb % 2], ss[tb % 2], tp[tb % 2]
            xo = xn[Tt % 2]
            S("sp", lambda e, x_=x_, tb=tb: e.dma_start(out=x_[:], in_=xv[tb * 128:(tb + 1) * 128, :]),
              writes=[x_.b], dma="xt%d" % (tb % 2))
            S("act", lambda e, x_=x_, s_=s_: e.activation(out=junk[:], in_=x_[:], func=AF.Square, accum_out=s_[:]),
              reads=[x_.b], writes=[junk.b, s_.b])
            S("dve", lambda e, s_=s_: e.tensor_scalar(out=s_[:], in0=s_[:], scalar1=1.0 / D, scalar2=1e-6, op0=ALU.mult, op1=ALU.add),
              reads=[s_.b], writes=[s_.b])
            S("act", lambda e, s_=s_: e.activation(out=s_[:], in_=s_[:], func=AF.Sqrt), reads=[s_.b], writes=[s_.b])
            S("dve", lambda e, s_=s_: e.reciprocal(out=s_[:], in_=s_[:]), reads=[s_.b], writes=[s_.b])
            S("act", lambda e, x_=x_, y_=y_, s_=s_: e.activation(out=y_[:], in_=x_[:], func=AF.Identity, scale=s_[:, 0:1]),
              reads=[x_.b, s_.b], writes=[y_.b])
            for dc in range(16):
                S("pe", lambda e, y_=y_, p_=p_, dc=dc: e.transpose(p_[:, dc * 128:(dc + 1) * 128], y_[:, dc * 128:(dc + 1) * 128], g.idb[:]),
                  reads=[y_.b, g.idb.b], writes=[p_.b], pe_acc=True)
            for dc in range(16):
                eng = "act" if dc % 2 == 0 else "dve"
                if eng == "act":
                    S("act", lambda e, p_=p_, xo=xo, dc=dc, sub=sub: e.activation(
                        out=xo[:, dc, sub * 128:(sub + 1) * 128], in_=p_[:, dc * 128:(dc + 1) * 128], func=AF.Identity,
                        scale=g.a1T[:, dc:dc + 1], bias=sh1[:, dc:dc + 1]),
                      reads=[p_.b, g.a1T.b, sh1.b], writes=[xo.b])
                else:
                    S("dve", lambda e, p_=p_, xo=xo, dc=dc, sub=sub: e.tensor_scalar(
                        out=xo[:, dc, sub * 128:(sub + 1) * 128], in0=p_[:, dc * 128:(dc + 1) * 128],
                        scalar1=g.a1T[:, dc:dc + 1], scalar2=sh1[:, dc:dc + 1], op0=ALU.mult, op1=ALU.add),
                      reads=[p_.b, g.a1T.b, sh1.b], writes=[xo.b])
            if sub == 3:
                S("sp", lambda e, xo=xo, Tt=Tt: e.dma_start(out=g.XNT.ap[Tt], in_=xo[:].rearrange("p a b -> p (a b)")),
                  reads=[xo.b], writes=[g.XNT.buf(Tt)], dma="xnt_st")
        barrier(g)
    if "XNT" in g.taps:
        g.tapdr.append(g.XNT)
    if g.upto < 2:
        return
    dr = g.dr
    g.QAT = dr("QAT", [8, 128, NQ], BF16)
    g.KAT = dr("KAT", [8, 128, SV], BF16)
    g.VA = dr("VA", [8, 128, NKB, 128], BF16)
    g.QBT = dr("QBT", [8, 128, NQ], BF16)
    g.CPT = dr("CPT", [4, 128, SV], BF16)
    g.KSLT = dr("KSLT", [2, 128, SV], BF16)
    g.VSL = dr("VSL", [2, 128, NKB, 128], BF16)
    g.KSWT = dr("KSWT", [2, 128, SV], BF16)
    g.VSW = dr("VSW", [2, 128, NKB, 128], BF16)
    g.GN = dr("GN", [NQB, 128, 24], F32)
    g.GAT = dr("GAT", [16, 128, NQ], BF16)
    g.GBT = dr("GBT", [16, 128, NQ], BF16)
    for nm in ("QAT", "KAT", "VA", "QBT", "CPT", "KSLT", "VSL", "KSWT", "VSW", "GN", "GAT", "GBT"):
        if nm in g.taps:
            g.tapdr.append(getattr(g, nm))

    def fm_store(dst, i0, own):
        def f(stg, ncc, tk):
            t0 = tk * 512
            return lambda e: e.dma_start(out=dst.ap[i0:i0 + ncc, :, t0:t0 + 512].rearrange("h p t -> p h t"), in_=stg[:, 0:ncc, :])
        return f

    def tmv_store(dst, i0):
        def f(stg, nh, tk):
            return [(lambda e, h=h: e.dma_start(
                out=dst.ap[i0 + h, :, 4 * tk:4 * tk + 4, :],
                in_=stg[:, :, h * 128:(h + 1) * 128])) for h in range(nh)]
        return f

    jobs = []
    for j in range(2):
        jobs.append((j * 512, 512, "fm", True, False, fm_store(g.QAT, 4 * j, True), g.QAT))
    for j in range(2):
        jobs.append((1024 + j * 512, 512, "fm", False, False, fm_store(g.KAT, 4 * j, False), g.KAT))
    for j in range(2):
        jobs.append((2048 + j * 512, 512, "tm", False, False, tmv_store(g.VA, 4 * j), g.VA))
    for j in range(2):
        jobs.append((3072 + j * 512, 512, "fm", True, False, fm_store(g.QBT, 4 * j, True), g.QBT))
    jobs.append((4096, 512, "fm", False, False, fm_store(g.CPT, 0, False), g.CPT))
    jobs.append((4608, 256, "fm", False, False, fm_store(g.KSLT, 0, False), g.KSLT))
    jobs.append((4864, 256, "tm", False, False, tmv_store(g.VSL, 0), g.VSL))
    jobs.append((5120, 256, "fm", False, False, fm_store(g.KSWT, 0, False), g.KSWT))
    jobs.append((5376, 256, "tm", False, False, tmv_store(g.VSW, 0), g.VSW))
    jobs.append((5632, 24, "gn", True, True, None, g.GN))
    for j in range(4):
        jobs.append((5656 + j * 512, 512, "fm", True, True, fm_store(g.GAT, 4 * j, True), g.GAT))
    for j in range(4):
        jobs.append((7704 + j * 512, 512, "fm", True, True, fm_store(g.GBT, 4 * j, True), g.GBT))

    win = g.inp["w_in"].ap
    with contextlib.ExitStack() as ls:
        wst = [T(ls, nc, "wst%d" % i, [128, 16, 512], F32) for i in range(2)]
        wb = [T(ls, nc, "wb%d" % i, [128, 16, 512], BF16) for i in range(2)]
        xn = [T(ls, nc, "xnl%d" % i, [128, 16, 512], BF16) for i in range(2)]
        ostg = [T(ls, nc, "ostg%d" % i, [128, 4, 512], BF16) for i in range(2)]
        gstg = [T(ls, nc, "gstg%d" % i, [128, 4, 24], F32) for i in range(2)]
        ps = [T(ls, nc, "pps%d" % i, [128, 512], F32, psum=True) for i in range(8)]
        nload = 0
        nout = 0
        for ji, (c0, w, kind, own, sig, store, dst) in enumerate(jobs):
            ws_, wb_ = wst[ji % 2], wb[ji % 2]
            src = win[:, c0:c0 + w].rearrange("(dc p) j -> p dc j", p=128)
            S("sp", lambda e, ws_=ws_, src=src, w=w: e.dma_start(out=ws_[:, :, 0:w], in_=src), writes=[ws_.b], dma="wst%d" % (ji % 2))
            for (eng, d0, d1) in (("act", 0, 6), ("dve", 6, 11), ("pool", 11, 16)):
                if eng == "act":
                    S("act", lambda e, ws_=ws_, wb_=wb_, d0=d0, d1=d1, w=w: e.copy(wb_[:, d0:d1, 0:w], ws_[:, d0:d1, 0:w]),
                      reads=[ws_.b], writes=[wb_.b])
                else:
                    S(eng, lambda e, ws_=ws_, wb_=wb_, d0=d0, d1=d1, w=w: e.tensor_copy(wb_[:, d0:d1, 0:w], ws_[:, d0:d1, 0:w]),
                      reads=[ws_.b], writes=[wb_.b])
            tiles = [(k, 2 * k + 1) for k in range(NQT)] if own else [(t, t) for t in range(NTV)]
            for (tk, tv) in tiles:
                xl = xn[nload % 2]
                S("sp", lambda e, xl=xl, tv=tv: e.dma_start(out=xl[:].rearrange("p a b -> p (a b)"), in_=g.XNT.ap[tv]),
                  reads=[g.XNT.buf(tv)], writes=[xl.b], dma="xnl%d" % (nload % 2))
                nload += 1
                pb = [ps[(nout % 2) * 4 + i] for i in range(4)]
                og = ostg[nout % 2]
                gg = gstg[nout % 2]
                nout += 1
                if kind == "fm":
                    ncc = w // 128
                    for cc in range(ncc):
                        for dc in range(16):
                            S("pe", lambda e, cc=cc, dc=dc, xl=xl, wb_=wb_, pb=pb: e.matmul(
                                pb[cc][:, :], wb_[:, dc, cc * 128:(cc + 1) * 128], xl[:, dc, :], start=(dc == 0), stop=(dc == 15)),
                              reads=[wb_.b, xl.b], writes=[pb[cc].b], pe_acc=True)
                    for cc in range(ncc):
                        if sig:
                            S("act", lambda e, cc=cc, og=og, pb=pb: e.activation(out=og[:, cc, :], in_=pb[cc][:, :], func=AF.Sigmoid),
                              reads=[pb[cc].b], writes=[og.b])
                        elif cc % 2 == 0:
                            S("act", lambda e, cc=cc, og=og, pb=pb: e.copy(og[:, cc, :], pb[cc][:, :]), reads=[pb[cc].b], writes=[og.b])
                        else:
                            S("dve", lambda e, cc=cc, og=og, pb=pb: e.tensor_copy(og[:, cc, :], pb[cc][:, :]), reads=[pb[cc].b], writes=[og.b])
                    S("sp", store(og, ncc, tk), reads=[og.b], writes=[dst.buf(tk)], dma="pst")
                elif kind == "tm":
                    for sub in range(4):
                        for dc in range(16):
                            S("pe", lambda e, sub=sub, dc=dc, xl=xl, wb_=wb_, pb=pb, w=w: e.matmul(
                                pb[sub][:, 0:w], xl[:, dc, sub * 128:(sub + 1) * 128], wb_[:, dc, 0:w], start=(dc == 0), stop=(dc == 15)),
                              reads=[wb_.b, xl.b], writes=[pb[sub].b], pe_acc=True)
                    for sub in range(4):
                        if sub % 2 == 0:
                            S("act", lambda e, sub=sub, og=og, pb=pb, w=w: e.copy(og[:, sub, 0:w], pb[sub][:, 0:w]), reads=[pb[sub].b], writes=[og.b])
                        else:
                            S("dve", lambda e, sub=sub, og=og, pb=pb, w=w: e.tensor_copy(og[:, sub, 0:w], pb[sub][:, 0:w]), reads=[pb[sub].b], writes=[og.b])
                    for fn in store(og, w // 128, tk):
                        S("sp", fn, reads=[og.b], writes=[dst.buf(tk)], dma="pst")
                else:
                    for sub in range(4):
                        for dc in range(16):
                            S("pe", lambda e, sub=sub, dc=dc, xl=xl, wb_=wb_, pb=pb, w=w: e.matmul(
                                pb[sub][:, 0:w], xl[:, dc, sub * 128:(sub + 1) * 128], wb_[:, dc, 0:w], start=(dc == 0), stop=(dc == 15)),
                              reads=[wb_.b, xl.b], writes=[pb[sub].b], pe_acc=True)
                    for sub in range(4):
                        S("act", lambda e, sub=sub, gg=gg, pb=pb, w=w: e.activation(out=gg[:, sub, :], in_=pb[sub][:, 0:w], func=AF.Sigmoid),
                          reads=[pb[sub].b], writes=[gg.b])
                    S("sp", lambda e, gg=gg, tk=tk: e.dma_start(out=g.GN.ap[4 * tk:4 * tk + 4].rearrange("s p c -> p s c"), in_=gg[:]),
                      reads=[gg.b], writes=[g.GN.buf(tk)], dma="pst")
        barrier(g)


CF_KB0, CF_ZERO, CF_ONE, CF_CPAD = 0, 4, 5, 8
CB_CAUS, CB_NEGU, CB_NEG1 = 0, 2048, 2176


def phase2(g):
    nc, sch, st = g.nc, g.sch, g.st
    S = sch.issue
    SV, NCV = g.SV, g.NCV
    NCH = (NCV + 127) // 128
    g.NCH = NCH
    g.kcmpT = [T(st, nc, "kcmpT%d" % i, [128, NCH * 128], BF16) for i in range(2)]
    g.vcmp = [T(st, nc, "vcmp%d" % i, [128, NCH, 128], BF16) for i in range(2)]
    nrs = [(n0, min(512, NCV - n0)) for n0 in range(0, NCV, 512)]
    with contextlib.ExitStack() as ls:
        w1s = T(ls, nc, "w1s", [128, 32, 256], F32)
        w1b = T(ls, nc, "w1b", [128, 32, 256], BF16)
        w2s = T(ls, nc, "w2s", [128, 2, 128], F32)
        w2b = T(ls, nc, "w2b", [128, 2, 128], BF16)
        pes = T(ls, nc, "pes", [128, 32], F32)
        peb = T(ls, nc, "peb", [128, 32], BF16)
        cv = T(ls, nc, "cv", [128, 2], F32)
        src = [T(ls, nc, "csrc%d" % i, [128, SV], BF16) for i in range(2)]
        u = T(ls, nc, "cu", [128, 512], F32)
        u2 = T(ls, nc, "cu2", [128, 512], F32)
        gl = [T(ls, nc, "gl%d" % i, [128, NCH * 128], BF16) for i in range(2)]
        hps = [T(ls, nc, "hps%d" % i, [128, 512], F32, psum=True) for i in range(2)]
        cps = T(ls, nc, "cps", [128, 2], F32, psum=True)
        ops = T(ls, nc, "cops", [128, 512], F32, psum=True)
        nsrc = 0
        for kv in range(2):
            w1d = g.inp["cw1k" if kv == 0 else "cw1v"].ap
            w2d = g.inp["cw2k" if kv == 0 else "cw2v"].ap
            ped = g.inp["pe_kT" if kv == 0 else "pe_vT"].ap
            S("sp", lambda e, w1d=w1d: e.dma_start(out=w1s[:], in_=w1d.rearrange("(p d) j -> d p j", d=128)), writes=[w1s.b], dma="cw1")
            S("sp", lambda e, w2d=w2d: e.dma_start(out=w2s[:], in_=w2d.rearrange("(c j) d -> j c d", j=128)), writes=[w2s.b], dma="cw2")
            S("sp", lambda e, ped=ped: e.dma_start(out=pes[:], in_=ped), writes=[pes.b], dma="cpe")
            S("act", lambda e: e.copy(w1b[:, 0:16, :], w1s[:, 0:16, :]), reads=[w1s.b], writes=[w1b.b])
            S("dve", lambda e: e.tensor_copy(w1b[:, 16:32, :], w1s[:, 16:32, :]), reads=[w1s.b], writes=[w1b.b])
            S("dve", lambda e: e.tensor_copy(w2b[:], w2s[:]), reads=[w2s.b], writes=[w2b.b])
            S("dve", lambda e: e.tensor_copy(peb[:], pes[:]), reads=[pes.b], writes=[peb.b])
            for jc in range(2):
                for p in range(32):
                    S("pe", lambda e, jc=jc, p=p: e.matmul(cps[:, jc:jc + 1], w1b[:, p, jc * 128:(jc + 1) * 128], peb[:, p:p + 1],
                                                           start=(p == 0), stop=(p == 31)),
                      reads=[w1b.b, peb.b], writes=[cps.b], pe_acc=True)
            S("dve", lambda e: e.tensor_copy(cv[:], cps[:]), reads=[cps.b], writes=[cv.b])
            for gi in range(2):
                sr = src[nsrc % 2]
                nsrc += 1
                S("sp", lambda e, sr=sr, kv=kv, gi=gi: e.dma_start(out=sr[:], in_=g.CPT.ap[kv * 2 + gi]),
                  reads=g.CPT.allbufs(), writes=[sr.b], dma="csrc%d" % (nsrc % 2))
                srv = sr[:].rearrange("p (n s) -> p n s", s=16)
                for jc in range(2):
                    for (n0, nn) in nrs:
                        hp = hps[(jc + n0 // 512) % 2]
                        for p in range(32):
                            S("pe", lambda e, hp=hp, jc=jc, p=p, n0=n0, nn=nn, srv=srv: e.matmul(
                                hp[:, 0:nn], w1b[:, p, jc * 128:(jc + 1) * 128], srv[:, n0 + p // 16:n0 + p // 16 + nn, p % 16],
                                start=(p == 0), stop=(p == 31)),
                              reads=[w1b.b, sr.b], writes=[hp.b], pe_acc=True)
                        S("dve", lambda e, hp=hp, jc=jc, nn=nn: e.tensor_scalar(out=u[:, 0:nn], in0=hp[:, 0:nn], scalar1=cv[:, jc:jc + 1], scalar2=None, op0=ALU.add),
                          reads=[hp.b, cv.b], writes=[u.b])
                        S("dve", lambda e, nn=nn: e.tensor_tensor(out=u2[:, 0:nn], in0=u[:, 0:nn], in1=u[:, 0:nn], op=ALU.mult), reads=[u.b], writes=[u2.b])
                        S("dve", lambda e, nn=nn: e.tensor_scalar(out=u2[:, 0:nn], in0=u2[:, 0:nn], scalar1=0.044715, scalar2=1.0, op0=ALU.mult, op1=ALU.add),
                          reads=[u2.b], writes=[u2.b])
                        S("dve", lambda e, nn=nn: e.tensor_tensor(out=u2[:, 0:nn], in0=u2[:, 0:nn], in1=u[:, 0:nn], op=ALU.mult), reads=[u.b, u2.b], writes=[u2.b])
                        S("act", lambda e, nn=nn: e.activation(out=u2[:, 0:nn], in_=u2[:, 0:nn], func=AF.Sigmoid, scale=1.5957691216057308),
                          reads=[u2.b], writes=[u2.b])
                        S("dve", lambda e, jc=jc, n0=n0, nn=nn: e.tensor_tensor(out=gl[jc][:, n0:n0 + nn], in0=u2[:, 0:nn], in1=u[:, 0:nn], op=ALU.mult),
                          reads=[u.b, u2.b], writes=[gl[jc].b])
                if kv == 0:
                    for (n0, nn) in nrs:
                        for jc in range(2):
                            S("pe", lambda e, jc=jc, n0=n0, nn=nn: e.matmul(ops[:, 0:nn], w2b[:, jc, :], gl[jc][:, n0:n0 + nn], start=(jc == 0), stop=(jc == 1)),
                              reads=[w2b.b, gl[jc].b], writes=[ops.b], pe_acc=True)
                        S("act", lambda e, gi=gi, n0=n0, nn=nn: e.copy(g.kcmpT[gi][:, n0:n0 + nn], ops[:, 0:nn]), reads=[ops.b], writes=[g.kcmpT[gi].b])
                else:
                    for ncx in range(NCH):
                        kk = min(128, NCV - ncx * 128)
                        for jc in range(2):
                            S("pe", lambda e, jc=jc, ncx=ncx, kk=kk: e.matmul(ops[0:kk, 0:128], gl[jc][:, ncx * 128:ncx * 128 + kk], w2b[:, jc, :],
                                                                            start=(jc == 0), stop=(jc == 1)),
                              reads=[w2b.b, gl[jc].b], writes=[ops.b], pe_acc=True)
                        S("act", lambda e, gi=gi, ncx=ncx, kk=kk: e.copy(g.vcmp[gi][0:kk, ncx, :], ops[0:kk, 0:128]), reads=[ops.b], writes=[g.vcmp[gi].b])
        barrier(g)
    if "KCMP" in g.taps:
        g.KCMP = g.dr("KCMP", [2, 128, NCH * 128], BF16)
        g.VCMP = g.dr("VCMP", [2, 128, NCH, 128], BF16)
        g.tapdr += [g.KCMP, g.VCMP]
        for gi in range(2):
            S("sp", lambda e, gi=gi: e.dma_start(out=g.KCMP.ap[gi], in_=g.kcmpT[gi][:]), reads=[g.kcmpT[gi].b], writes=[g.KCMP.buf(gi)], dma=g.uniq())
            S("sp", lambda e, gi=gi: e.dma_start(out=g.VCMP.ap[gi], in_=g.vcmp[gi][:]), reads=[g.vcmp[gi].b], writes=[g.VCMP.buf(gi)], dma=g.uniq())


def phase3(g):
    nc, sch, st = g.nc, g.sch, g.st
    S = sch.issue
    SV, NKB, NQT, NQ = g.SV, g.NKB, g.NQT, g.NQ
    g.OAT = g.dr("OAT", [8, 128, NQ], BF16)
    if "OAT" in g.taps:
        g.tapdr.append(g.OAT)
    cb, cf = g.cb, g.cf
    with contextlib.ExitStack() as ls:
        kT = T(ls, nc, "sb_kT", [128, 2, SV], BF16)
        vv = T(ls, nc, "sb_v", [128, 2, NKB, 128], BF16)
        qT = T(ls, nc, "sb_qT", [128, 2, NQ], BF16)
        ex = [T(ls, nc, "sb_e%d" % i, [128, 2, 512], F32) for i in range(2)]
        sp = [T(ls, nc, "sb_sp%d" % i, [128, 2, 512], BF16) for i in range(2)]
        t1 = [T(ls, nc, "sb_t%d" % i, [128, 2, 512], F32) for i in range(2)]
        aa = [T(ls, nc, "sb_a%d" % i, [128, 2, 512], BF16) for i in range(2)]
        osb = [T(ls, nc, "sb_o%d" % i, [128, 2, 512], BF16) for i in range(2)]
        zps = [[T(ls, nc, "sb_z%d_%d" % (i, h), [128, 512], F32, psum=True) for h in range(2)] for i in range(2)]
        aps = [T(ls, nc, "sb_ap%d" % h, [128, 512], F32, psum=True) for h in range(2)]
        ops = [T(ls, nc, "sb_op%d" % h, [128, 512], F32, psum=True) for h in range(2)]
        negU = cb[:, CB_NEGU:CB_NEGU + 128]
        negL = cb[:, 3584:3584 + 128]
        ng = 0
        for hp in range(4):
            S("sp", lambda e, hp=hp: e.dma_start(out=kT[:], in_=g.KAT.ap[2 * hp:2 * hp + 2].rearrange("h p t -> p h t")),
              reads=g.KAT.allbufs(), writes=[kT.b], dma="sbk")
            S("sp", lambda e, hp=hp: e.dma_start(out=vv[:], in_=g.VA.ap[2 * hp:2 * hp + 2].rearrange("h p t d -> p h t d")),
              reads=g.VA.allbufs(), writes=[vv.b], dma="sbv")
            S("sp", lambda e, hp=hp: e.dma_start(out=qT[:], in_=g.QAT.ap[2 * hp:2 * hp + 2].rearrange("h p t -> p h t")),
              reads=g.QAT.allbufs(), writes=[qT.b], dma="sbq")
            for k in range(NQT):
                tv = 2 * k + 1
                jts = list(range(4 * tv + 3, -1, -1))
                nj = len(jts)
                base = ng
                ng += nj

                def bufs(ji):
                    i2 = (base + ji) % 2
                    return ex[i2], sp[i2], t1[i2], aa[i2], zps[i2]

                def kbias(jt):
                    return cf[:, CF_KB0 + jt:CF_KB0 + jt + 1] if jt < 4 else cf[:, CF_ZERO:CF_ZERO + 1]

                def stage_a(ji):
                    jt = jts[ji]
                    e_, sp_, t_, a_, z_ = bufs(ji)
                    dg = jt - 4 * tv
                    kb = kbias(jt)
                    for h in range(2):
                        S("pe", lambda e, h=h: e.matmul(z_[h][:, :], kT[:, h, jt * 128:(jt + 1) * 128], qT[:, h, k * 512:(k + 1) * 512], start=True, stop=True),
                          reads=[kT.b, qT.b], writes=[z_[h].b])
                    for h in range(2):
                        S("act", lambda e, h=h: e.activation(out=e_[:, h, :], in_=z_[h][:, :], func=AF.Exp, scale=SCALE, bias=kb),
                          reads=[z_[h].b, cf.b], writes=[e_.b])
                    S("act", lambda e: e.activation(out=sp_[:], in_=e_[:], func=AF.Ln, bias=cf[:, CF_ONE:CF_ONE + 1]),
                      reads=[e_.b, cf.b], writes=[sp_.b])
                    if dg >= 0:
                        for h in range(2):
                            S("pool", lambda e, h=h: e.tensor_tensor(out=sp_[:, h, :], in0=sp_[:, h, :], in1=cb[:, CB_CAUS + dg * 512:CB_CAUS + (dg + 1) * 512], op=ALU.mult),
                              reads=[sp_.b, cb.b], writes=[sp_.b])

                def stage_b(ji):
                    jt = jts[ji]
                    e_, sp_, t_, a_, z_ = bufs(ji)
                    first, last = (ji == 0), (ji == nj - 1)
                    dg = jt - 4 * tv
                    kb = kbias(jt)
                    for h in range(2):
                        S("pe", lambda e, h=h: e.matmul(aps[h][:, :], negU, sp_[:, h, :], start=first, stop=last),
                          reads=[cb.b, sp_.b], writes=[aps[h].b], pe_acc=True)
                    for h in range(2):
                        S("dve", lambda e, h=h: e.scalar_tensor_tensor(out=t_[:, h, :], in0=z_[h][:, :], scalar=SCALE, in1=sp_[:, h, :], op0=ALU.mult, op1=ALU.subtract),
                          reads=[z_[h].b, sp_.b], writes=[t_.b])
                        S("dve", lambda e, h=h: e.tensor_tensor(out=t_[:, h, :], in0=aps[h][:, :], in1=t_[:, h, :], op=ALU.add),
                          reads=[aps[h].b, t_.b], writes=[t_.b])
                    if not last:
                        for h in range(2):
                            S("pe", lambda e, h=h: e.matmul(aps[h][:, :], negL, sp_[:, h, :], start=False, stop=False),
                              reads=[cb.b, sp_.b], writes=[aps[h].b], pe_acc=True)
                    S("act", lambda e: e.activation(out=a_[:], in_=t_[:], func=AF.Exp, bias=kb), reads=[t_.b, cf.b], writes=[a_.b])
                    if dg >= 0:
                        for h in range(2):
                            S("pool", lambda e, h=h: e.tensor_tensor(out=a_[:, h, :], in0=a_[:, h, :], in1=cb[:, CB_CAUS + dg * 512:CB_CAUS + (dg + 1) * 512], op=ALU.mult),
                              reads=[a_.b, cb.b], writes=[a_.b])
                    for h in range(2):
                        S("pe", lambda e, h=h: e.matmul(ops[h][:, :], vv[:, h, jt, :], a_[:, h, :], start=first, stop=last),
                          reads=[vv.b, a_.b], writes=[ops[h].b], pe_acc=not first)

                stage_a(0)
                for ji in range(nj):
                    if ji + 1 < nj:
                        stage_a(ji + 1)
                    stage_b(ji)
                o_ = osb[(hp * NQT + k) % 2]
                S("act", lambda e, o_=o_: e.copy(o_[:, 0, :], ops[0][:, :]), reads=[ops[0].b], writes=[o_.b])
                S("dve", lambda e, o_=o_: e.tensor_copy(o_[:, 1, :], ops[1][:, :]), reads=[ops[1].b], writes=[o_.b])
                S("sp", lambda e, o_=o_, hp=hp, k=k: e.dma_start(out=g.OAT.ap[2 * hp:2 * hp + 2, :, k * 512:(k + 1) * 512].rearrange("h p t -> p h t"), in_=o_[:]),
                  reads=[o_.b], writes=[g.OAT.buf((hp, k))], dma="sbo")
        barrier(g)


CB_WM4, CB_ONESROW, CB_CPADROW, CB_WI, CB_ONESCOL, CB_ONES = 2304, 2816, 2944, 2976, 3264, 3328
CF_PADNEG, CF_F0 = 16, 176
OH_S0, OH_S1, OH_BC, OH_W = 0, 33 * 128, 33 * 128 + 32 * 128, 33 * 128 + 32 * 128 + 33 * 17


def phase4(g):
    nc, sch, st = g.nc, g.sch, g.st
    S = sch.issue
    SV, NKB, NQT, NQ, NQB, NCV, NSV, NCH = g.SV, g.NKB, g.NQT, g.NQ, g.NQB, g.NCV, g.NSV, g.NCH
    NCVP = NSV * 4
    KA = min(128, NSV)
    KB = NSV - KA
    g.OBT = g.dr("OBT", [8, 128, NQ], BF16)
    if "OBT" in g.taps:
        g.tapdr.append(g.OBT)
    cb, cf, idb = g.cb, g.cf, g.idb
    with contextlib.ExitStack() as ls:
        BS0 = T(ls, nc, "BS0", [128, 8, 128], BF16)
        BS1 = T(ls, nc, "BS1", [128, 8, 128], BF16)
        BCq = T(ls, nc, "BCq", [128, 8, 17], BF16)
        BCT = T(ls, nc, "BCT", [17, 8, 128], BF16)
        bmax = T(ls, nc, "bmax", [128, 1], F32)
        exp_c = T(ls, nc, "expc", [128, SV], BF16)
        S("sp", lambda e: e.dma_start(out=exp_c[:], in_=g.inp["c_exp"].ap), writes=[exp_c.b], dma=g.uniq())
        with contextlib.ExitStack() as l2:
            oh = T(l2, nc, "oh", [128, OH_W], BF16)
            tv = T(l2, nc, "tv", [128, 256], F32)
            td = T(l2, nc, "td", [128, 256], F32)
            acc0 = T(l2, nc, "acc0", [128, 128], F32)
            acc1 = T(l2, nc, "acc1", [128, 128], F32)
            acc2 = T(l2, nc, "acc2", [128, 17], F32)
            tps = T(l2, nc, "bctps", [128, 128], F32, psum=True)
            S("sp", lambda e: e.dma_start(out=oh[:], in_=g.inp["c_oh"].ap), writes=[oh.b], dma=g.uniq())
            S("sp", lambda e: e.dma_start(out=tv[:], in_=g.inp["relb"].ap.rearrange("(o b) h -> o (b h)", o=1).partition_broadcast(128)),
              writes=[tv.b], dma=g.uniq())
            tv3 = tv[:].rearrange("p (b h) -> p b h", h=8)
            td3 = td[:].rearrange("p (b h) -> p b h", h=8)
            for h in range(8):
                S("dve", lambda e, h=h: e.tensor_scalar(out=td3[:, :, h], in0=tv3[:, :, h], scalar1=tv[:, 248 + h:249 + h], scalar2=None, op0=ALU.subtract),
                  reads=[tv.b], writes=[td.b])
            S("dve", lambda e: e.tensor_reduce(out=bmax[:], in_=td[:], axis=AX.X, op=ALU.max, apply_absolute_value=True), reads=[td.b], writes=[bmax.b])
            S("dve", lambda e: e.tensor_scalar(out=td[:], in0=td[:], scalar1=1.0 / SCALE, scalar2=None, op0=ALU.mult), reads=[td.b], writes=[td.b])
            ohs0 = oh[:, OH_S0:OH_S0 + 33 * 128].rearrange("p (b q) -> p b q", q=128)
            ohs1 = oh[:, OH_S1:OH_S1 + 32 * 128].rearrange("p (b q) -> p b q", q=128)
            ohbc = oh[:, OH_BC:OH_BC + 33 * 17].rearrange("p (b q) -> p b q", q=17)
            for h in range(8):
                S("dve", lambda e: e.tensor_scalar(out=acc0[:], in0=ohs0[:, 32, :], scalar1=NEG, scalar2=None, op0=ALU.mult), reads=[oh.b], writes=[acc0.b])
                S("pool", lambda e: e.tensor_scalar(out=acc2[:], in0=ohbc[:, 32, :], scalar1=NEG, scalar2=None, op0=ALU.mult), reads=[oh.b], writes=[acc2.b])
                for b in range(31):
                    sc_ = td[:, b * 8 + h:b * 8 + h + 1]
                    S("dve", lambda e, b=b, sc_=sc_: e.scalar_tensor_tensor(out=acc0[:], in0=ohs0[:, b, :], scalar=sc_, in1=acc0[:], op0=ALU.mult, op1=ALU.add),
                      reads=[oh.b, td.b, acc0.b], writes=[acc0.b])
                    if b == 0:
                        S("dve", lambda e, b=b, sc_=sc_: e.tensor_scalar(out=acc1[:], in0=ohs1[:, b, :], scalar1=sc_, scalar2=None, op0=ALU.mult),
                          reads=[oh.b, td.b], writes=[acc1.b])
                    else:
                        S("dve", lambda e, b=b, sc_=sc_: e.scalar_tensor_tensor(out=acc1[:], in0=ohs1[:, b, :], scalar=sc_, in1=acc1[:], op0=ALU.mult, op1=ALU.add),
                          reads=[oh.b, td.b, acc1.b], writes=[acc1.b])
                    S("dve", lambda e, b=b, sc_=sc_: e.scalar_tensor_tensor(out=acc2[:], in0=ohbc[:, b, :], scalar=sc_, in1=acc2[:], op0=ALU.mult, op1=ALU.add),
                      reads=[oh.b, td.b, acc2.b], writes=[acc2.b])
                S("act", lambda e, h=h: e.copy(BS0[:, h, :], acc0[:]), reads=[acc0.b], writes=[BS0.b])
                S("act", lambda e, h=h: e.copy(BS1[:, h, :], acc1[:]), reads=[acc1.b], writes=[BS1.b])
                S("act", lambda e, h=h: e.copy(BCq[:, h, :], acc2[:]), reads=[acc2.b], writes=[BCq.b])
                S("pe", lambda e: e.transpose(tps[0:17, :], acc2[:], g.idf[:]), reads=[acc2.b, g.idf.b], writes=[tps.b])
                S("act", lambda e, h=h: e.copy(BCT[:, h, :], tps[0:17, :]), reads=[tps.b], writes=[BCT.b])
            barrier(g)
        kslT = T(ls, nc, "kslT", [128, SV], BF16)
        vsl = T(ls, nc, "vsl", [128, NKB, 128], BF16)
        kswT = T(ls, nc, "kswT", [128, SV], BF16)
        vsw = T(ls, nc, "vsw", [128, NKB, 128], BF16)
        qT = T(ls, nc, "nqT", [128, 4, NQ], BF16)
        sq = T(ls, nc, "nsq", [128, 2048], BF16)
        mx = T(ls, nc, "nmx", [128, 4], F32)
        nmk = T(ls, nc, "nmk", [128, 8], F32)
        ec = T(ls, nc, "n_ec", [128, 4, NCVP], F32)
        zc = T(ls, nc, "n_zc", [128, 4], F32)
        pc = T(ls, nc, "n_pc", [128, NCVP], F32)
        imp = T(ls, nc, "n_imp", [128, NSV], F32)
        sc2 = T(ls, nc, "n_sc2", [128, NSV], F32)
        m8 = T(ls, nc, "n_m8", [128, 16], F32)
        nm = T(ls, nc, "n_nm", [128, NSV], BF16)
        nmTA = T(ls, nc, "n_nmTA", [128, 4, 128], BF16)
        nmTB = T(ls, nc, "n_nmTB", [8, 4, 128], BF16)
        pT = [T(ls, nc, "n_pT%d" % i, [128, 512], BF16) for i in range(2)]
        gn = T(ls, nc, "n_gn", [128, 24], F32)
        zz = T(ls, nc, "n_zz", [128, 12], F32)
        coef = T(ls, nc, "n_coef", [128, 12], F32)
        otmp = T(ls, nc, "n_otmp", [128, 128], F32)
        ob = T(ls, nc, "n_ob", [128, 4, 128], BF16)
        obT = [T(ls, nc, "n_obT%d" % i, [128, 4, 128], BF16) for i in range(2)]
        scq = T(ls, nc, "n_scq", [128, 512], F32, psum=True)
        trp = T(ls, nc, "n_trp", [128, 1024], BF16, psum=True)
        stp = [T(ls, nc, "n_st%d" % i, [128, 512], F32, psum=True) for i in range(2)]
        opb = [T(ls, nc, "n_o%d" % i, [128, 512], F32, psum=True) for i in range(3)]
        zpb = T(ls, nc, "n_z", [128, 512], F32, psum=True)
        ones_m = cb[:, CB_ONES:CB_ONES + 128]
        onescol = cb[:, CB_ONESCOL:CB_ONESCOL + 1]
        nst = [0]

        def maxnorm(src_fn, total, col, srcbuf):
            for c0 in range(0, total, 512):
                w = min(512, total - c0)
                S("dve", lambda e, c0=c0, w=w: e.tensor_tensor(out=sq[:, 0:w], in0=src_fn(c0, w), in1=src_fn(c0, w), op=ALU.mult), reads=[srcbuf], writes=[sq.b])
                S("pe", lambda e, w=w: e.matmul(scq[:, 0:w], ones_m, sq[:, 0:w], start=True, stop=True), reads=[cb.b, sq.b], writes=[scq.b])
                S("dve", lambda e, w=w: e.tensor_reduce(out=mx[:, 2:3], in_=scq[:, 0:w], axis=AX.X, op=ALU.max), reads=[scq.b], writes=[mx.b])
                S("dve", lambda e, col=col: e.tensor_tensor(out=mx[:, col:col + 1], in0=mx[:, col:col + 1], in1=mx[:, 2:3], op=ALU.max), reads=[mx.b], writes=[mx.b])

        for gi in range(2):
            S("sp", lambda e, gi=gi: e.dma_start(out=kslT[:], in_=g.KSLT.ap[gi]), reads=g.KSLT.allbufs(), writes=[kslT.b], dma="n_k1")
            S("sp", lambda e, gi=gi: e.dma_start(out=vsl[:], in_=g.VSL.ap[gi]), reads=g.VSL.allbufs(), writes=[vsl.b], dma="n_v1")
            S("sp", lambda e, gi=gi: e.dma_start(out=kswT[:], in_=g.KSWT.ap[gi]), reads=g.KSWT.allbufs(), writes=[kswT.b], dma="n_k2")
            S("sp", lambda e, gi=gi: e.dma_start(out=vsw[:], in_=g.VSW.ap[gi]), reads=g.VSW.allbufs(), writes=[vsw.b], dma="n_v2")
            S("sp", lambda e, gi=gi: e.dma_start(out=qT[:], in_=g.QBT.ap[4 * gi:4 * gi + 4].rearrange("h p t -> p h t")),
              reads=g.QBT.allbufs(), writes=[qT.b], dma="n_q")
            S("dve", lambda e: e.memset(mx[:], 0.0), writes=[mx.b])
            S("dve", lambda e: e.memset(pc[:], 0.0), writes=[pc.b])
            for h in range(4):
                maxnorm(lambda c0, w, h=h: qT[:, h, c0:c0 + w], NQ, 0, qT.b)
            maxnorm(lambda c0, w: kslT[:, c0:c0 + w], SV, 1, kslT.b)
            maxnorm(lambda c0, w: kswT[:, c0:c0 + w], SV, 1, kswT.b)
            maxnorm(lambda c0, w, gi=gi: g.kcmpT[gi][:, c0:c0 + w], NCV, 1, g.kcmpT[gi].b)
            S("dve", lambda e: e.tensor_tensor(out=mx[:, 2:3], in0=mx[:, 0:1], in1=mx[:, 1:2], op=ALU.mult), reads=[mx.b], writes=[mx.b])
            S("act", lambda e: e.activation(out=mx[:, 2:3], in_=mx[:, 2:3], func=AF.Sqrt), reads=[mx.b], writes=[mx.b])
            S("dve", lambda e: e.scalar_tensor_tensor(out=mx[:, 3:4], in0=mx[:, 2:3], scalar=-SCALE * 1.02, in1=bmax[:], op0=ALU.mult, op1=ALU.subtract),
              reads=[mx.b, bmax.b], writes=[mx.b])
            for j in range(4):
                S("dve", lambda e, j=j: e.tensor_tensor(out=nmk[:, j:j + 1], in0=mx[:, 3:4], in1=cf[:, CF_KB0 + j:CF_KB0 + j + 1], op=ALU.add),
                  reads=[mx.b, cf.b], writes=[nmk.b])
            S("dve", lambda e: e.tensor_copy(nmk[:, 4:5], mx[:, 3:4]), reads=[mx.b], writes=[nmk.b])
            S("dve", lambda e: e.tensor_tensor(out=nmk[:, 5:6], in0=mx[:, 3:4], in1=cf[:, CF_CPAD:CF_CPAD + 1], op=ALU.add), reads=[mx.b, cf.b], writes=[nmk.b])
            negM = nmk[:, 4:5]

            for k in range(NQT):
                for c in range(4):
                    iv = 4 * (2 * k + 1) + c
                    lb = 4 * k + c
                    q4 = qT[:, :, lb * 128:(lb + 1) * 128]
                    ncols = min(8 * iv + 7, NCV)
                    n0 = 8 * iv - 10
                    S("sp", lambda e, lb=lb: e.dma_start(out=gn[:], in_=g.GN.ap[lb]), reads=g.GN.allbufs(), writes=[gn.b], dma="n_gn")
                    for h in range(4):
                        S("pe", lambda e, h=h, lb=lb, ncols=ncols: e.matmul(scq[:, 0:ncols], qT[:, h, lb * 128:(lb + 1) * 128], g.kcmpT[gi][:, 0:ncols], start=True, stop=False),
                          reads=[qT.b, g.kcmpT[gi].b], writes=[scq.b])
                        S("pe", lambda e, h=h, n0=n0: e.matmul(scq[:, n0:n0 + 17], idb[:], BCq[:, 4 * gi + h, :], start=False, stop=False),
                          reads=[idb.b, BCq.b], writes=[scq.b], pe_acc=True)
                        S("pe", lambda e: e.matmul(scq[:, 0:32], cb[0:1, CB_ONESROW:CB_ONESROW + 128], cb[0:1, CB_CPADROW:CB_CPADROW + 32], start=False, stop=True),
                          reads=[cb.b], writes=[scq.b], pe_acc=True)
                        S("act", lambda e, h=h, ncols=ncols: e.activation(out=ec[:, h, 0:ncols], in_=scq[:, 0:ncols], func=AF.Exp, scale=SCALE, bias=negM, accum_out=zc[:, h:h + 1]),
                          reads=[scq.b, nmk.b], writes=[ec.b, zc.b])
                    S("dve", lambda e: e.tensor_scalar(out=zc[:], in0=zc[:], scalar1=1e-30, scalar2=None, op0=ALU.max), reads=[zc.b], writes=[zc.b])
                    S("dve", lambda e: e.reciprocal(out=zc[:], in_=zc[:]), reads=[zc.b], writes=[zc.b])
                    S("dve", lambda e, ncols=ncols: e.tensor_scalar(out=pc[:, 0:ncols], in0=ec[:, 0, 0:ncols], scalar1=zc[:, 0:1], scalar2=None, op0=ALU.mult),
                      reads=[ec.b, zc.b], writes=[pc.b])
                    for h in range(1, 4):
                        S("dve", lambda e, h=h, ncols=ncols: e.scalar_tensor_tensor(out=pc[:, 0:ncols], in0=ec[:, h, 0:ncols], scalar=zc[:, h:h + 1], in1=pc[:, 0:ncols],
                                                                                 op0=ALU.mult, op1=ALU.add),
                          reads=[ec.b, zc.b, pc.b], writes=[pc.b])
                    pcv = pc[:].rearrange("p (j f) -> p j f", f=4)
                    S("dve", lambda e: e.tensor_reduce(out=imp[:], in_=pcv, axis=AX.X, op=ALU.add), reads=[pc.b], writes=[imp.b])
                    S("dve", lambda e: e.scalar_tensor_tensor(out=imp[:], in0=pcv[:, :, 3], scalar=-0.5, in1=imp[:], op0=ALU.mult, op1=ALU.add),
                      reads=[pc.b, imp.b], writes=[imp.b])
                    S("dve", lambda e: e.scalar_tensor_tensor(out=imp[:, 1:NSV], in0=pcv[:, 0:NSV - 1, 3], scalar=0.5, in1=imp[:, 1:NSV], op0=ALU.mult, op1=ALU.add),
                      reads=[pc.b, imp.b], writes=[imp.b])
                    S("dve", lambda e: e.tensor_tensor(out=imp[:], in0=imp[:], in1=cf[:, CF_PADNEG:CF_PADNEG + NSV], op=ALU.add), reads=[imp.b, cf.b], writes=[imp.b])
                    S("dve", lambda e: e.tensor_tensor(out=imp[:], in0=imp[:], in1=cf[:, CF_F0:CF_F0 + NSV], op=ALU.max), reads=[imp.b, cf.b], writes=[imp.b])
                    if 2 * iv + 2 < NSV:
                        S("dve", lambda e, iv=iv: e.memset(imp[:, 2 * iv + 2:NSV], -1e30), writes=[imp.b])
                    S("dve", lambda e, iv=iv: e.memset(imp[0:64, 2 * iv + 1:2 * iv + 2], -1e30), writes=[imp.b])
                    S("dve", lambda e, iv=iv: e.memset(imp[64:128, 2 * iv + 1:2 * iv + 2], 1e4), writes=[imp.b])
                    S("dve", lambda e, iv=iv: e.memset(imp[:, 2 * iv:2 * iv + 1], 1e4), writes=[imp.b])
                    S("dve", lambda e, iv=iv: e.memset(imp[0:64, 2 * iv - 1:2 * iv], 1e4), writes=[imp.b])
                    S("dve", lambda e: e.max(out=m8[:, 0:8], in_=imp[:]), reads=[imp.b], writes=[m8.b])
                    S("dve", lambda e: e.match_replace(out=sc2[:], in_to_replace=m8[:, 0:8], in_values=imp[:], imm_value=-3e38), reads=[imp.b, m8.b], writes=[sc2.b])
                    S("dve", lambda e: e.max(out=m8[:, 8:16], in_=sc2[:]), reads=[sc2.b], writes=[m8.b])
                    S("dve", lambda e: e.tensor_scalar(out=nm[:], in0=imp[:], scalar1=m8[:, 15:16], scalar2=NEG, op0=ALU.is_lt, op1=ALU.mult),
                      reads=[imp.b, m8.b], writes=[nm.b])
                    trv = trp[:, 0:512].bitcast(F32) if False else None
                    S("pe", lambda e: e.matmul(scq[0:KA, 0:128], nm[:, 0:KA], idb[:], start=True, stop=True), reads=[nm.b, idb.b], writes=[scq.b])
                    for h in range(4):
                        eng = "act" if h % 2 == 0 else "dve"
                        if eng == "act":
                            S("act", lambda e, h=h: e.copy(nmTA[0:KA, h, :], scq[0:KA, 0:128]), reads=[scq.b], writes=[nmTA.b])
                        else:
                            S("dve", lambda e, h=h: e.tensor_copy(nmTA[0:KA, h, :], scq[0:KA, 0:128]), reads=[scq.b], writes=[nmTA.b])
                    if KB > 0:
                        S("pe", lambda e: e.matmul(scq[0:KB, 128:256], nm[:, KA:KA + KB], idb[:], start=True, stop=True), reads=[nm.b, idb.b], writes=[scq.b])
                        for h in range(4):
                            S("dve", lambda e, h=h: e.tensor_copy(nmTB[0:KB, h, :], scq[0:KB, 128:256]), reads=[scq.b], writes=[nmTB.b])

                    tl = []

                    def run_tile(br, first, kk, mms, bias_ap, vrhs, vbuf):
                        tl.append((br, first, kk, mms, bias_ap, vrhs, vbuf, nst[0] % 2))
                        nst[0] += 1

                    def tile_a(t):
                        br, first, kk, mms, bias_ap, vrhs, vbuf, bi = t
                        st_, p_ = stp[bi], pT[bi]
                        for mi, (lhsT, rhs, rbufs) in enumerate(mms):
                            S("pe", lambda e, lhsT=lhsT, rhs=rhs, mi=mi: e.matmul(st_[0:kk, :], lhsT, rhs, start=(mi == 0), stop=(mi == len(mms) - 1)),
                              reads=rbufs, writes=[st_.b], pe_acc=(mi > 0))
                        S("act", lambda e: e.activation(out=p_[0:kk, :], in_=st_[0:kk, :], func=AF.Exp, scale=SCALE, bias=bias_ap),
                          reads=[st_.b, nmk.b], writes=[p_.b])

                    def tile_b(t):
                        br, first, kk, mms, bias_ap, vrhs, vbuf, bi = t
                        p_ = pT[bi]
                        for h in range(4):
                            S("pe", lambda e, h=h: e.matmul(opb[br][:, h * 128:(h + 1) * 128], p_[0:kk, h * 128:(h + 1) * 128], vrhs, start=(first and h == 0), stop=False),
                              reads=[p_.b, vbuf], writes=[opb[br].b], pe_acc=not (first and h == 0))
                        for h in range(4):
                            S("pe", lambda e, h=h: e.matmul(zpb[:, br * 4 + h:br * 4 + h + 1], p_[0:kk, h * 128:(h + 1) * 128], cb[0:kk, CB_ONESCOL:CB_ONESCOL + 1],
                                                          start=(first and h == 0 and br == 0), stop=False),
                              reads=[p_.b, cb.b], writes=[zpb.b], pe_acc=not (first and h == 0 and br == 0))

                    nchunks = (ncols + 127) // 128
                    for ncx in range(nchunks):
                        kk = min(128, ncols - 128 * ncx)
                        mms = [(g.kcmpT[gi][:, ncx * 128:ncx * 128 + kk], q4, [g.kcmpT[gi].b, qT.b])]
                        dlt = n0 - 128 * ncx
                        if dlt > -17 and dlt < kk:
                            mms.append((cb[0:17, CB_WI + 128 - dlt:CB_WI + 128 - dlt + kk], BCT[:, 4 * gi:4 * gi + 4, :], [cb.b, BCT.b]))
                        run_tile(0, ncx == 0, kk, mms, nmk[0:kk, 5:6] if ncx == 0 else nmk[0:kk, 4:5], g.vcmp[gi][0:kk, ncx, :], g.vcmp[gi].b)
                    for jt in range(iv + 1):
                        mms = [(kslT[:, jt * 128:(jt + 1) * 128], q4, [kslT.b, qT.b])]
                        if jt < 64 or KB == 0:
                            mms.append((exp_c[0:KA, jt * 128:(jt + 1) * 128], nmTA[0:KA, :, :], [exp_c.b, nmTA.b]))
                        else:
                            mms.append((exp_c[0:KB, jt * 128:(jt + 1) * 128], nmTB[0:KB, :, :], [exp_c.b, nmTB.b]))
                        if jt == iv:
                            mms.append((idb[:], BS0[:, 4 * gi:4 * gi + 4, :], [idb.b, BS0.b]))
                        elif jt == iv - 1:
                            mms.append((idb[:], BS1[:, 4 * gi:4 * gi + 4, :], [idb.b, BS1.b]))
                        run_tile(1, jt == 0, 128, mms, nmk[:, jt:jt + 1] if jt < 4 else nmk[:, 4:5], vsl[:, jt, :], vsl.b)
                    for jt in range(iv - 4, iv + 1):
                        mms = [(kswT[:, jt * 128:(jt + 1) * 128], q4, [kswT.b, qT.b])]
                        if jt == iv:
                            mms.append((idb[:], BS0[:, 4 * gi:4 * gi + 4, :], [idb.b, BS0.b]))
                        elif jt == iv - 1:
                            mms.append((idb[:], BS1[:, 4 * gi:4 * gi + 4, :], [idb.b, BS1.b]))
                        elif jt == iv - 4:
                            mms.append((idb[:], cb[:, CB_WM4:CB_WM4 + 512], [idb.b, cb.b]))
                        run_tile(2, jt == iv - 4, 128, mms, nmk[:, jt:jt + 1] if jt < 4 else nmk[:, 4:5], vsw[:, jt, :], vsw.b)
                    tile_a(tl[0])
                    for ti in range(len(tl)):
                        if ti + 1 < len(tl):
                            tile_a(tl[ti + 1])
                        tile_b(tl[ti])
                    S("dve", lambda e: e.tensor_scalar(out=zz[:], in0=zpb[:, 0:12], scalar1=1e-30, scalar2=None, op0=ALU.max), reads=[zpb.b], writes=[zz.b])
                    S("dve", lambda e: e.reciprocal(out=zz[:], in_=zz[:]), reads=[zz.b], writes=[zz.b])
                    S("dve", lambda e: e.tensor_tensor(out=coef[:].rearrange("p (b h) -> p b h", h=4), in0=zz[:].rearrange("p (b h) -> p b h", h=4),
                                                      in1=gn[:, gi * 12:(gi + 1) * 12].rearrange("p (h b) -> p b h", b=3), op=ALU.mult),
                      reads=[zz.b, gn.b], writes=[coef.b])
                    for h in range(4):
                        S("dve", lambda e, h=h: e.tensor_scalar(out=otmp[:], in0=opb[0][:, h * 128:(h + 1) * 128], scalar1=coef[:, h:h + 1], scalar2=None, op0=ALU.mult),
                          reads=[opb[0].b, coef.b], writes=[otmp.b])
                        S("dve", lambda e, h=h: e.scalar_tensor_tensor(out=otmp[:], in0=opb[1][:, h * 128:(h + 1) * 128], scalar=coef[:, 4 + h:5 + h], in1=otmp[:], op0=ALU.mult, op1=ALU.add),
                          reads=[opb[1].b, coef.b, otmp.b], writes=[otmp.b])
                        S("dve", lambda e, h=h: e.scalar_tensor_tensor(out=ob[:, h, :], in0=opb[2][:, h * 128:(h + 1) * 128], scalar=coef[:, 8 + h:9 + h], in1=otmp[:], op0=ALU.mult, op1=ALU.add),
                          reads=[opb[2].b, coef.b, otmp.b], writes=[ob.b])
                    for h in range(4):
                        S("pe", lambda e, h=h: e.transpose(trp[:, h * 128:(h + 1) * 128], ob[:, h, :], idb[:]), reads=[ob.b, idb.b], writes=[trp.b], pe_acc=(h > 0))
                    oT_ = obT[(gi * NQB + lb) % 2]
                    S("act", lambda e, oT_=oT_: e.copy(oT_[:].rearrange("p h q -> p (h q)"), trp[:, 0:512]), reads=[trp.b], writes=[oT_.b])
                    S("sp", lambda e, oT_=oT_, lb=lb: e.dma_start(out=g.OBT.ap[4 * gi:4 * gi + 4, :, lb * 128:(lb + 1) * 128].rearrange("h p t -> p h t"), in_=oT_[:]),
                      reads=[oT_.b], writes=[g.OBT.buf((gi, lb))], dma="n_o")
        barrier(g)


CF_EOFF = 336
CB_LTRI = 3456
CB_NEGL = 3584


def phase5(g):
    nc, sch, st = g.nc, g.sch, g.st
    S = sch.issue
    NQT, NQ, NQB, CAP = g.NQT, g.NQ, g.NQB, g.CAP
    cb, cf, idb, idf = g.cb, g.cf, g.idb, g.idf
    g.MT = g.dr("MT", [16, 128, NQ], BF16)
    g.X1 = g.dr("X1", [NQ, D], F32)
    g.XB = g.dr("XB", [NEXP * CAP, D], BF16)
    g.RT = g.dr("RT", [NQB, 128, 4], F32)
    for nm_ in ("MT", "X1", "RT"):
        if nm_ in g.taps:
            g.tapdr.append(getattr(g, nm_))
    g.breg = nc.gpsimd.to_reg(NEXP * CAP - 1)
    g.wts = T(st, nc, "wts", [128, NQB, 2], F32)
    g.dst = T(st, nc, "dst", [128, NQB, 2], I32)
    with contextlib.ExitStack() as ls:
        wab = T(ls, nc, "wab", [128, 8, D], BF16)
        wbb = T(ls, nc, "wbb", [128, 8, D], BF16)
        with contextlib.ExitStack() as l2:
            stg = [T(l2, nc, "wstg%d" % i, [128, 8, 512], F32) for i in range(2)]
            n = 0
            for (wd, wt) in ((g.inp["wba"].ap, wab), (g.inp["wbb"].ap, wbb)):
                for cs in range(4):
                    sg = stg[n % 2]
                    S("sp", lambda e, sg=sg, wd=wd, cs=cs: e.dma_start(out=sg[:], in_=wd[:, cs * 512:(cs + 1) * 512].rearrange("(c p) j -> p c j", p=128)),
                      writes=[sg.b], dma="wstg%d" % (n % 2))
                    S("act", lambda e, sg=sg, wt=wt, cs=cs: e.copy(wt[:, 0:4, cs * 512:(cs + 1) * 512], sg[:, 0:4, :]), reads=[sg.b], writes=[wt.b])
                    S("dve", lambda e, sg=sg, wt=wt, cs=cs: e.tensor_copy(wt[:, 4:8, cs * 512:(cs + 1) * 512], sg[:, 4:8, :]), reads=[sg.b], writes=[wt.b])
                    n += 1
            barrier(g)
        oa = [T(ls, nc, "m_oa%d" % i, [128, 8, 512], BF16) for i in range(2)]
        obt = [T(ls, nc, "m_ob%d" % i, [128, 8, 512], BF16) for i in range(2)]
        ga = [T(ls, nc, "m_ga%d" % i, [128, 4, 512], BF16) for i in range(2)]
        gb = [T(ls, nc, "m_gb%d" % i, [128, 4, 512], BF16) for i in range(2)]
        mo = [T(ls, nc, "m_mo%d" % i, [128, 4, 512], BF16) for i in range(2)]
        ta = [T(ls, nc, "m_ta%d" % i, [128, 512], F32) for i in range(2)]
        tb_ = [T(ls, nc, "m_tb%d" % i, [128, 512], F32) for i in range(2)]
        psA = [T(ls, nc, "m_pa%d" % i, [128, 512], F32, psum=True) for i in range(2)]
        psB = [T(ls, nc, "m_pb%d" % i, [128, 512], F32, psum=True) for i in range(2)]
        nq = 0
        for k in range(NQT):
            oa_, ob_ = oa[k % 2], obt[k % 2]
            S("sp", lambda e, oa_=oa_, k=k: e.dma_start(out=oa_[:], in_=g.OAT.ap[:, :, k * 512:(k + 1) * 512].rearrange("h p t -> p h t")),
              reads=g.OAT.allbufs(), writes=[oa_.b], dma="m_oa%d" % (k % 2))
            S("sp", lambda e, ob_=ob_, k=k: e.dma_start(out=ob_[:], in_=g.OBT.ap[:, :, k * 512:(k + 1) * 512].rearrange("h p t -> p h t")),
              reads=g.OBT.allbufs(), writes=[ob_.b], dma="m_ob%d" % (k % 2))
            for dq in range(4):
                ga_, gb_, mo_ = ga[nq % 2], gb[nq % 2], mo[nq % 2]
                S("sp", lambda e, ga_=ga_, k=k, dq=dq: e.dma_start(out=ga_[:], in_=g.GAT.ap[4 * dq:4 * dq + 4, :, k * 512:(k + 1) * 512].rearrange("c p t -> p c t")),
                  reads=g.GAT.allbufs(), writes=[ga_.b], dma="m_ga%d" % (nq % 2))
                S("sp", lambda e, gb_=gb_, k=k, dq=dq: e.dma_start(out=gb_[:], in_=g.GBT.ap[4 * dq:4 * dq + 4, :, k * 512:(k + 1) * 512].rearrange("c p t -> p c t")),
                  reads=g.GBT.allbufs(), writes=[gb_.b], dma="m_gb%d" % (nq % 2))
                nq += 1
                for dl in range(4):
                    dc = 4 * dq + dl
                    pa, pb = psA[dc % 2], psB[dc % 2]
                    ta_, tb2 = ta[dc % 2], tb_[dc % 2]
                    for hc in range(8):
                        S("pe", lambda e, pa=pa, hc=hc, dc=dc, oa_=oa_: e.matmul(pa[:, :], wab[:, hc, dc * 128:(dc + 1) * 128], oa_[:, hc, :], start=(hc == 0), stop=(hc == 7)),
                          reads=[wab.b, oa_.b], writes=[pa.b], pe_acc=True)
                    for hc in range(8):
                        S("pe", lambda e, pb=pb, hc=hc, dc=dc, ob_=ob_: e.matmul(pb[:, :], wbb[:, hc, dc * 128:(dc + 1) * 128], ob_[:, hc, :], start=(hc == 0), stop=(hc == 7)),
                          reads=[wbb.b, ob_.b], writes=[pb.b], pe_acc=True)
                    S("dve", lambda e, pa=pa, ta_=ta_, ga_=ga_, dl=dl: e.tensor_tensor(out=ta_[:], in0=pa[:, :], in1=ga_[:, dl, :], op=ALU.mult), reads=[pa.b, ga_.b], writes=[ta_.b])
                    S("dve", lambda e, pb=pb, tb2=tb2, gb_=gb_, dl=dl: e.tensor_tensor(out=tb2[:], in0=pb[:, :], in1=gb_[:, dl, :], op=ALU.mult), reads=[pb.b, gb_.b], writes=[tb2.b])
                    S("pool", lambda e, ta_=ta_, tb2=tb2, mo_=mo_, dl=dl: e.tensor_tensor(out=mo_[:, dl, :], in0=ta_[:], in1=tb2[:], op=ALU.add), reads=[ta_.b, tb2.b], writes=[mo_.b])
                S("sp", lambda e, mo_=mo_, k=k, dq=dq: e.dma_start(out=g.MT.ap[4 * dq:4 * dq + 4, :, k * 512:(k + 1) * 512].rearrange("c p t -> p c t"), in_=mo_[:]),
                  reads=[mo_.b], writes=[g.MT.buf((k, dq))], dma="m_st")
        barrier(g)
    with contextlib.ExitStack() as ls:
        wob = T(ls, nc, "wob", [128, 16, D], BF16)
        g.rows = {}
        for nm_ in ("gt1", "a2", "sh2"):
            g.rows[nm_] = T(ls, nc, "row_" + nm_, [128, D], F32)
        load_rows(g, ls, ("gt1", "a2", "sh2"))
        with contextlib.ExitStack() as l2:
            stg = [T(l2, nc, "wostg%d" % i, [128, 16, 512], F32) for i in range(2)]
            for cs in range(4):
                sg = stg[cs % 2]
                S("sp", lambda e, sg=sg, cs=cs: e.dma_start(out=sg[:], in_=g.inp["wout"].ap[:, cs * 512:(cs + 1) * 512].rearrange("(c p) j -> p c j", p=128)),
                  writes=[sg.b], dma="wostg%d" % (cs % 2))
                S("act", lambda e, sg=sg, cs=cs: e.copy(wob[:, 0:8, cs * 512:(cs + 1) * 512], sg[:, 0:8, :]), reads=[sg.b], writes=[wob.b])
                S("dve", lambda e, sg=sg, cs=cs: e.tensor_copy(wob[:, 8:16, cs * 512:(cs + 1) * 512], sg[:, 8:16, :]), reads=[sg.b], writes=[wob.b])
            barrier(g)
        wr = T(ls, nc, "wr", [128, 16, 72], F32)
        brr = T(ls, nc, "brr", [128, 72], F32)
        S("sp", lambda e: e.dma_start(out=wr[:], in_=g.inp["wr"].ap.rearrange("(c p) j -> p c j", p=128)), writes=[wr.b], dma=g.uniq())
        S("sp", lambda e: e.dma_start(out=brr[:], in_=g.inp["br"].ap.partition_broadcast(128)), writes=[brr.b], dma=g.uniq())
        mt = [T(ls, nc, "r_mt%d" % i, [128, 16, 128], BF16) for i in range(2)]
        xb = [T(ls, nc, "r_xb%d" % i, [128, D], F32) for i in range(2)]
        hn = T(ls, nc, "r_hn", [128, D], F32)
        hnb = [T(ls, nc, "r_hnb%d" % i, [128, D], BF16) for i in range(2)]
        hnT = T(ls, nc, "r_hnT", [128, 16, 128], F32)
        junk = T(ls, nc, "r_junk", [128, D], BF16)
        ss = T(ls, nc, "r_ss", [128, 1], F32)
        lg = T(ls, nc, "r_lg", [128, 72], F32)
        sm = T(ls, nc, "r_sm", [128, 16], F32)
        oh8 = T(ls, nc, "r_oh8", [128, 8], F32)
        lem = T(ls, nc, "r_lem", [128, 64], F32)
        top8 = T(ls, nc, "r_top8", [128, 8], F32)
        A1 = T(ls, nc, "r_A1", [128, 64], F32)
        A2 = T(ls, nc, "r_A2", [128, 64], F32)
        Ab = T(ls, nc, "r_Ab", [128, 64], BF16)
        Acum = T(ls, nc, "r_Acum", [128, 64], BF16)
        t64 = T(ls, nc, "r_t64", [128, 64], F32)
        j64 = T(ls, nc, "r_j64", [128, 64], F32)
        dsf = T(ls, nc, "r_dsf", [128, 8], F32)
        psy = [T(ls, nc, "r_py%d" % i, [128, 512], F32, psum=True) for i in range(4)]
        pst = [T(ls, nc, "r_pt%d" % i, [128, 512], F32, psum=True) for i in range(2)]
        psl = T(ls, nc, "r_pl", [128, 512], F32, psum=True)
        S("dve", lambda e: e.memset(Acum[:], 0.0), writes=[Acum.b])
        ltri = cb[:, CB_LTRI:CB_LTRI + 128]
        ones_m = cb[:, CB_ONES:CB_ONES + 128]
        xv = g.inp["xv"].ap
        for lb in range(NQB):
            k, c = lb // 4, lb % 4
            r0 = (2 * k + 1) * 512 + c * 128
            mt_, xb_, hnb_ = mt[lb % 2], xb[lb % 2], hnb[lb % 2]
            S("sp", lambda e, mt_=mt_, lb=lb: e.dma_start(out=mt_[:], in_=g.MT.ap[:, :, lb * 128:(lb + 1) * 128].rearrange("c p t -> p c t")),
              reads=g.MT.allbufs(), writes=[mt_.b], dma="r_mt%d" % (lb % 2))
            S("sp", lambda e, xb_=xb_, r0=r0: e.dma_start(out=xb_[:], in_=xv[r0:r0 + 128, :]), writes=[xb_.b], dma="r_xb%d" % (lb % 2))
            for oc in range(4):
                for dc in range(16):
                    S("pe", lambda e, oc=oc, dc=dc, mt_=mt_: e.matmul(psy[oc][:, :], mt_[:, dc, :], wob[:, dc, oc * 512:(oc + 1) * 512], start=(dc == 0), stop=(dc == 15)),
                      reads=[mt_.b, wob.b], writes=[psy[oc].b], pe_acc=True)
                S("dve", lambda e, oc=oc: e.tensor_tensor(out=hn[:, oc * 512:(oc + 1) * 512], in0=psy[oc][:, :], in1=g.rows["gt1"][:, oc * 512:(oc + 1) * 512], op=ALU.mult),
                  reads=[psy[oc].b, g.rows["gt1"].b], writes=[hn.b])
            S("pool", lambda e, xb_=xb_: e.tensor_tensor(out=xb_[:], in0=xb_[:], in1=hn[:], op=ALU.add), reads=[xb_.b, hn.b], writes=[xb_.b])
            S("sp", lambda e, xb_=xb_, lb=lb: e.dma_start(out=g.X1.ap[lb * 128:(lb + 1) * 128, :], in_=xb_[:]), reads=[xb_.b], writes=[g.X1.buf(lb)], dma="r_x1")
            S("act", lambda e, xb_=xb_: e.activation(out=junk[:], in_=xb_[:], func=AF.Square, accum_out=ss[:]), reads=[xb_.b], writes=[junk.b, ss.b])
            S("dve", lambda e: e.tensor_scalar(out=ss[:], in0=ss[:], scalar1=1.0 / D, scalar2=1e-6, op0=ALU.mult, op1=ALU.add), reads=[ss.b], writes=[ss.b])
            S("act", lambda e: e.activation(out=ss[:], in_=ss[:], func=AF.Sqrt), reads=[ss.b], writes=[ss.b])
            S("dve", lambda e: e.reciprocal(out=ss[:], in_=ss[:]), reads=[ss.b], writes=[ss.b])
            S("dve", lambda e, xb_=xb_: e.scalar_tensor_tensor(out=hn[:], in0=xb_[:], scalar=ss[:, 0:1], in1=g.rows["a2"][:], op0=ALU.mult, op1=ALU.mult),
              reads=[xb_.b, ss.b, g.rows["a2"].b], writes=[hn.b])
            S("pool", lambda e: e.tensor_tensor(out=hn[:], in0=hn[:], in1=g.rows["sh2"][:], op=ALU.add), reads=[hn.b, g.rows["sh2"].b], writes=[hn.b])
            S("act", lambda e, hnb_=hnb_: e.copy(hnb_[:], hn[:]), reads=[hn.b], writes=[hnb_.b])
            for q4 in range(4):
                pt = pst[q4 % 2]
                for j in range(4):
                    dc = q4 * 4 + j
                    S("pe", lambda e, pt=pt, j=j, dc=dc: e.transpose(pt[:, j * 128:(j + 1) * 128], hn[:, dc * 128:(dc + 1) * 128], idf[:]),
                      reads=[hn.b, idf.b], writes=[pt.b], pe_acc=(j > 0))
                if q4 % 2 == 0:
                    S("act", lambda e, pt=pt, q4=q4: e.copy(hnT[:, q4 * 4:(q4 + 1) * 4, :].rearrange("p a b -> p (a b)"), pt[:, :]), reads=[pt.b], writes=[hnT.b])
                else:
                    S("dve", lambda e, pt=pt, q4=q4: e.tensor_copy(hnT[:, q4 * 4:(q4 + 1) * 4, :].rearrange("p a b -> p (a b)"), pt[:, :]), reads=[pt.b], writes=[hnT.b])
            for dc in range(16):
                S("pe", lambda e, dc=dc: e.matmul(psl[:, 0:72], hnT[:, dc, :], wr[:, dc, :], start=(dc == 0), stop=(dc == 15)),
                  reads=[hnT.b, wr.b], writes=[psl.b], pe_acc=(dc > 0))
            S("dve", lambda e: e.tensor_tensor(out=lg[:], in0=psl[:, 0:72], in1=brr[:], op=ALU.add), reads=[psl.b, brr.b], writes=[lg.b])
            S("dve", lambda e: e.tensor_reduce(out=sm[:, 0:1], in_=lg[:, 0:8], axis=AX.X, op=ALU.max), reads=[lg.b], writes=[sm.b])
            S("dve", lambda e: e.tensor_scalar(out=sm[:, 1:2], in0=sm[:, 0:1], scalar1=-1.0, scalar2=None, op0=ALU.mult), reads=[sm.b], writes=[sm.b])
            S("dve", lambda e: e.tensor_scalar(out=oh8[:], in0=lg[:, 0:8], scalar1=sm[:, 0:1], scalar2=None, op0=ALU.is_equal), reads=[lg.b, sm.b], writes=[oh8.b])
            S("act", lambda e: e.activation(out=j64[:, 0:8], in_=lg[:, 0:8], func=AF.Exp, bias=sm[:, 1:2], accum_out=sm[:, 2:3]), reads=[lg.b, sm.b], writes=[j64.b, sm.b])
            S("dve", lambda e: e.tensor_scalar(out=oh8[:], in0=oh8[:], scalar1=-1.0, scalar2=1e30, op0=ALU.add, op1=ALU.mult), reads=[oh8.b], writes=[oh8.b])
            for gq in range(8):
                S("dve", lambda e, gq=gq: e.tensor_scalar(out=lem[:, gq * 8:(gq + 1) * 8], in0=lg[:, 8 + gq * 8:16 + gq * 8], scalar1=oh8[:, gq:gq + 1], scalar2=None, op0=ALU.add),
                  reads=[lg.b, oh8.b], writes=[lem.b])
            S("dve", lambda e: e.max(out=top8[:], in_=lem[:]), reads=[lem.b], writes=[top8.b])
            S("dve", lambda e: e.tensor_scalar(out=A1[:], in0=lem[:], scalar1=top8[:, 0:1], scalar2=None, op0=ALU.is_equal), reads=[lem.b, top8.b], writes=[A1.b])
            S("dve", lambda e: e.tensor_scalar(out=A2[:], in0=lem[:], scalar1=top8[:, 1:2], scalar2=None, op0=ALU.is_equal), reads=[lem.b, top8.b], writes=[A2.b])
            S("dve", lambda e: e.tensor_tensor(out=Ab[:], in0=A1[:], in1=A2[:], op=ALU.add), reads=[A1.b, A2.b], writes=[Ab.b])
            S("dve", lambda e: e.tensor_scalar(out=sm[:, 3:4], in0=top8[:, 0:1], scalar1=-1.0, scalar2=None, op0=ALU.mult), reads=[top8.b], writes=[sm.b])
            S("act", lambda e: e.activation(out=sm[:, 4:5], in_=top8[:, 1:2], func=AF.Exp, bias=sm[:, 3:4]), reads=[top8.b, sm.b], writes=[sm.b])
            S("dve", lambda e: e.scalar_tensor_tensor(out=sm[:, 5:6], in0=sm[:, 4:5], scalar=1.0, in1=sm[:, 2:3], op0=ALU.add, op1=ALU.mult), reads=[sm.b], writes=[sm.b])
            S("dve", lambda e: e.reciprocal(out=sm[:, 6:7], in_=sm[:, 5:6]), reads=[sm.b], writes=[sm.b])
            S("dve", lambda e: e.tensor_tensor(out=sm[:, 7:8], in0=sm[:, 6:7], in1=sm[:, 4:5], op=ALU.mult), reads=[sm.b], writes=[sm.b])
            S("pe", lambda e, lb=lb: e.matmul(psl[:, 128:192], ltri, Ab[:], start=True, stop=(lb == 0)), reads=[cb.b, Ab.b], writes=[psl.b])
            if lb > 0:
                S("pe", lambda e: e.matmul(psl[:, 128:192], ones_m, Acum[:], start=False, stop=True), reads=[cb.b, Acum.b], writes=[psl.b], pe_acc=True)
            S("dve", lambda e: e.tensor_tensor(out=t64[:], in0=psl[:, 128:192], in1=cf[:, CF_EOFF:CF_EOFF + 64], op=ALU.add), reads=[psl.b, cf.b], writes=[t64.b])
            for j, Aj in enumerate((A1, A2)):
                S("dve", lambda e, Aj=Aj, j=j: e.scalar_tensor_tensor(out=j64[:], in0=t64[:], scalar=1.0, in1=Aj[:], op0=ALU.mult, op1=ALU.mult, accum_out=dsf[:, j:j + 1]),
                  reads=[t64.b, Aj.b], writes=[j64.b, dsf.b])
                S("dve", lambda e, Aj=Aj, j=j: e.scalar_tensor_tensor(out=j64[:], in0=psl[:, 128:192], scalar=1.0, in1=Aj[:], op0=ALU.mult, op1=ALU.mult, accum_out=dsf[:, 2 + j:3 + j]),
                  reads=[psl.b, Aj.b], writes=[j64.b, dsf.b])
                S("dve", lambda e, j=j: e.tensor_scalar(out=dsf[:, 4 + j:5 + j], in0=dsf[:, 2 + j:3 + j], scalar1=CAP - 0.5, scalar2=1e9, op0=ALU.is_ge, op1=ALU.mult),
                  reads=[dsf.b], writes=[dsf.b])
                S("dve", lambda e, j=j: e.tensor_tensor(out=dsf[:, j:j + 1], in0=dsf[:, j:j + 1], in1=dsf[:, 4 + j:5 + j], op=ALU.add), reads=[dsf.b], writes=[dsf.b])
            S("dve", lambda e: e.tensor_tensor(out=Acum[:], in0=Acum[:], in1=Ab[:], op=ALU.add), reads=[Acum.b, Ab.b], writes=[Acum.b])
            S("dve", lambda e, lb=lb: e.tensor_copy(g.dst[:, lb, :], dsf[:, 0:2]), reads=[dsf.b], writes=[g.dst.b])
            S("dve", lambda e, lb=lb: e.tensor_copy(g.wts[:, lb, :], sm[:, 6:8]), reads=[sm.b], writes=[g.wts.b])
            for j in range(2):
                S("pool", lambda e, lb=lb, j=j, hnb_=hnb_: e.indirect_dma_start(
                    out=g.XB.ap, out_offset=bass.IndirectOffsetOnAxis(ap=g.dst[:, lb, j:j + 1], axis=0), in_=hnb_[:], in_offset=None,
                    bounds_check=g.breg, oob_is_err=False),
                  reads=[hnb_.b, g.dst.b], writes=[g.XB.buf()], dma="r_sc")
            if "RT" in g.taps:
                S("dve", lambda e: e.tensor_copy(dsf[:, 2:4], sm[:, 6:8]), reads=[sm.b, dsf.b], writes=[dsf.b])
                S("sp", lambda e, lb=lb: e.dma_start(out=g.RT.ap[lb], in_=dsf[:, 0:4]), reads=[dsf.b], writes=[g.RT.buf(lb)], dma="r_rt")
        barrier(g)


def phase6(g):
    nc, sch, st = g.nc, g.sch, g.st
    S = sch.issue
    CAP = g.CAP
    NSB = CAP // 128
    idb = g.idb
    g.YB = g.dr("YB", [NEXP * CAP, D], F32)
    ew1, ew3, ew2 = g.inp["ew1"].ap, g.inp["ew3"].ap, g.inp["ew2"].ap
    with contextlib.ExitStack() as ls:
        w1s = [T(ls, nc, "e_w1s%d" % i, [128, D], F32) for i in range(2)]
        w3s = [T(ls, nc, "e_w3s%d" % i, [128, D], F32) for i in range(2)]
        w2s = [T(ls, nc, "e_w2s%d" % i, [128, D], F32) for i in range(2)]
        w1b = [T(ls, nc, "e_w1b%d" % i, [128, 16, 128], BF16) for i in range(2)]
        w3b = [T(ls, nc, "e_w3b%d" % i, [128, 16, 128], BF16) for i in range(2)]
        w2b = T(ls, nc, "e_w2b", [128, NFC, D], BF16)
        xet = T(ls, nc, "e_xet", [128, 16, CAP], BF16)
        xr = [T(ls, nc, "e_xr%d" % i, [128, D], BF16) for i in range(2)]
        hid = T(ls, nc, "e_hid", [128, NFC, CAP], BF16)
        sl = T(ls, nc, "e_sl", [128, CAP], F32)
        yst = [T(ls, nc, "e_yst%d" % i, [128, D], F32) for i in range(2)]
        h1 = T(ls, nc, "e_h1", [128, 512], F32, psum=True)
        h3 = T(ls, nc, "e_h3", [128, 512], F32, psum=True)
        ptr = [T(ls, nc, "e_pt%d" % i, [128, 1024], BF16, psum=True) for i in range(2)]
        py = [T(ls, nc, "e_py%d" % i, [128, 512], F32, psum=True) for i in range(4)]
        steps = [(e_, fc) for e_ in range(NEXP) for fc in range(NFC)]

        def loads(i):
            e_, fc = steps[i]
            S("sp", lambda e: e.dma_start(out=w1s[i % 2][:], in_=ew1[e_, fc]), writes=[w1s[i % 2].b], dma="e_w1s%d" % (i % 2))
            S("sp", lambda e: e.dma_start(out=w3s[i % 2][:], in_=ew3[e_, fc]), writes=[w3s[i % 2].b], dma="e_w3s%d" % (i % 2))
            S("sp", lambda e: e.dma_start(out=w2s[i % 2][:], in_=ew2[e_, fc * 128:(fc + 1) * 128, :]), writes=[w2s[i % 2].b], dma="e_w2s%d" % (i % 2))

        loads(0)
        nx = 0
        ny = 0
        for i, (e_, fc) in enumerate(steps):
            if i + 1 < len(steps):
                loads(i + 1)
            if fc == 0:
                for sb in range(NSB):
                    xr_ = xr[nx % 2]
                    S("sp", lambda e, xr_=xr_, sb=sb: e.dma_start(out=xr_[:], in_=g.XB.ap[e_ * CAP + sb * 128:e_ * CAP + (sb + 1) * 128, :]),
                      reads=g.XB.allbufs(), writes=[xr_.b], dma="e_xr%d" % (nx % 2))
                    nx += 1
                    for hq in range(2):
                        pt = ptr[hq]
                        for j in range(8):
                            dc = hq * 8 + j
                            S("pe", lambda e, pt=pt, j=j, dc=dc, xr_=xr_: e.transpose(pt[:, j * 128:(j + 1) * 128], xr_[:, dc * 128:(dc + 1) * 128], idb[:]),
                              reads=[xr_.b, idb.b], writes=[pt.b], pe_acc=(j > 0))
                        if hq == 0:
                            S("act", lambda e, pt=pt, sb=sb: e.copy(xet[:, 0:8, sb * 128:(sb + 1) * 128], pt[:, :].rearrange("p (a b) -> p a b", b=128)),
                              reads=[pt.b], writes=[xet.b])
                        else:
                            S("dve", lambda e, pt=pt, sb=sb: e.tensor_copy(xet[:, 8:16, sb * 128:(sb + 1) * 128], pt[:, :].rearrange("p (a b) -> p a b", b=128)),
                              reads=[pt.b], writes=[xet.b])
            a, b3 = w1b[i % 2], w3b[i % 2]
            S("act", lambda e, a=a: e.copy(a[:].rearrange("p a b -> p (a b)"), w1s[i % 2][:]), reads=[w1s[i % 2].b], writes=[a.b])
            S("dve", lambda e, b3=b3: e.tensor_copy(b3[:].rearrange("p a b -> p (a b)"), w3s[i % 2][:]), reads=[w3s[i % 2].b], writes=[b3.b])
            S("pool", lambda e, fc=fc: e.tensor_copy(w2b[:, fc, :], w2s[i % 2][:]), reads=[w2s[i % 2].b], writes=[w2b.b])
            for dc in range(16):
                S("pe", lambda e, dc=dc, a=a: e.matmul(h1[:, 0:CAP], a[:, dc, :], xet[:, dc, :], start=(dc == 0), stop=(dc == 15)),
                  reads=[a.b, xet.b], writes=[h1.b], pe_acc=(dc > 0))
            for dc in range(16):
                S("pe", lambda e, dc=dc, b3=b3: e.matmul(h3[:, 0:CAP], b3[:, dc, :], xet[:, dc, :], start=(dc == 0), stop=(dc == 15)),
                  reads=[b3.b, xet.b], writes=[h3.b], pe_acc=(dc > 0))
            S("act", lambda e: e.activation(out=sl[:], in_=h1[:, 0:CAP], func=AF.Silu), reads=[h1.b], writes=[sl.b])
            S("dve", lambda e, fc=fc: e.tensor_tensor(out=hid[:, fc, :], in0=h3[:, 0:CAP], in1=sl[:], op=ALU.mult), reads=[h3.b, sl.b], writes=[hid.b])
            if fc == NFC - 1:
                for sb in range(NSB):
                    ys = yst[ny % 2]
                    ny += 1
                    for oc in range(4):
                        for f2 in range(NFC):
                            S("pe", lambda e, oc=oc, f2=f2, sb=sb: e.matmul(py[oc][:, :], hid[:, f2, sb * 128:(sb + 1) * 128], w2b[:, f2, oc * 512:(oc + 1) * 512],
                                                                        start=(f2 == 0), stop=(f2 == NFC - 1)),
                              reads=[hid.b, w2b.b], writes=[py[oc].b], pe_acc=(f2 > 0))
                        if oc % 2 == 0:
                            S("act", lambda e, oc=oc, ys=ys: e.copy(ys[:, oc * 512:(oc + 1) * 512], py[oc][:, :]), reads=[py[oc].b], writes=[ys.b])
                        else:
                            S("dve", lambda e, oc=oc, ys=ys: e.tensor_copy(ys[:, oc * 512:(oc + 1) * 512], py[oc][:, :]), reads=[py[oc].b], writes=[ys.b])
                    S("sp", lambda e, ys=ys, sb=sb: e.dma_start(out=g.YB.ap[e_ * CAP + sb * 128:e_ * CAP + (sb + 1) * 128, :], in_=ys[:]),
                      reads=[ys.b], writes=[g.YB.buf(e_)], dma="e_yst")
        barrier(g)


def phase7(g):
    nc, sch, st = g.nc, g.sch, g.st
    S = sch.issue
    NQB, CAP = g.NQB, g.CAP
    with contextlib.ExitStack() as ls:
        g.rows = {}
        for nm_ in ("gt2", "gf"):
            g.rows[nm_] = T(ls, nc, "row_" + nm_, [128, D], F32)
        load_rows(g, ls, ("gt2", "gf"))
        y = [[T(ls, nc, "f_y%d_%d" % (i, j), [128, D], F32) for j in range(2)] for i in range(2)]
        x1 = [T(ls, nc, "f_x1_%d" % i, [128, D], F32) for i in range(2)]
        mo = T(ls, nc, "f_mo", [128, D], F32)
        junk = T(ls, nc, "f_junk", [128, D], BF16)
        ss = T(ls, nc, "f_ss", [128, 1], F32)
        ob = [T(ls, nc, "f_ob%d" % i, [128, D], F32) for i in range(2)]
        for lb in range(NQB):
            i2 = lb % 2
            x_ = x1[i2]
            S("sp", lambda e, x_=x_, lb=lb: e.dma_start(out=x_[:], in_=g.X1.ap[lb * 128:(lb + 1) * 128, :]), reads=[g.X1.buf(lb)], writes=[x_.b], dma="f_x%d" % i2)
            for j in range(2):
                yj = y[i2][j]
                S("pool", lambda e, yj=yj: e.memset(yj[:], 0.0), writes=[yj.b])
                S("pool", lambda e, yj=yj, lb=lb, j=j: e.indirect_dma_start(
                    out=yj[:], out_offset=None, in_=g.YB.ap, in_offset=bass.IndirectOffsetOnAxis(ap=g.dst[:, lb, j:j + 1], axis=0),
                    bounds_check=g.breg, oob_is_err=False),
                  reads=g.YB.allbufs() + [g.dst.b], writes=[yj.b], dma="f_y%d_%d" % (i2, j))
            S("dve", lambda e, i2=i2, lb=lb: e.tensor_scalar(out=mo[:], in0=y[i2][0][:], scalar1=g.wts[:, lb, 0:1], scalar2=None, op0=ALU.mult),
              reads=[y[i2][0].b, g.wts.b], writes=[mo.b])
            S("dve", lambda e, i2=i2, lb=lb: e.scalar_tensor_tensor(out=mo[:], in0=y[i2][1][:], scalar=g.wts[:, lb, 1:2], in1=mo[:], op0=ALU.mult, op1=ALU.add),
              reads=[y[i2][1].b, g.wts.b, mo.b], writes=[mo.b])
            S("pool", lambda e: e.tensor_tensor(out=mo[:], in0=mo[:], in1=g.rows["gt2"][:], op=ALU.mult), reads=[mo.b, g.rows["gt2"].b], writes=[mo.b])
            S("pool", lambda e, x_=x_: e.tensor_tensor(out=x_[:], in0=x_[:], in1=mo[:], op=ALU.add), reads=[x_.b, mo.b], writes=[x_.b])
            S("act", lambda e, x_=x_: e.activation(out=junk[:], in_=x_[:], func=AF.Square, accum_out=ss[:]), reads=[x_.b], writes=[junk.b, ss.b])
            S("dve", lambda e: e.tensor_scalar(out=ss[:], in0=ss[:], scalar1=1.0 / D, scalar2=1e-6, op0=ALU.mult, op1=ALU.add), reads=[ss.b], writes=[ss.b])
            S("act", lambda e: e.activation(out=ss[:], in_=ss[:], func=AF.Sqrt), reads=[ss.b], writes=[ss.b])
            S("dve", lambda e: e.reciprocal(out=ss[:], in_=ss[:]), reads=[ss.b], writes=[ss.b])
            o_ = ob[i2]
            S("dve", lambda e, x_=x_, o_=o_: e.scalar_tensor_tensor(out=o_[:], in0=x_[:], scalar=ss[:, 0:1], in1=g.rows["gf"][:], op0=ALU.mult, op1=ALU.mult),
              reads=[x_.b, ss.b, g.rows["gf"].b], writes=[o_.b])
            S("sp", lambda e, o_=o_, lb=lb: e.dma_start(out=g.out.ap[lb * 128:(lb + 1) * 128, :], in_=o_[:]), reads=[o_.b], writes=[g.out.buf(lb)], dma="f_out")
        barrier(g)

def bf(a):
    return np.ascontiguousarray(a).astype(ml_dtypes.bfloat16)


def host_consts(S, half, CAP):
    SV = S + 512
    c = {}
    c["c_idf"] = np.eye(128, dtype=np.float32)
    c["c_idb"] = bf(np.eye(128))
    cf = np.zeros((128, 1024), np.float32)
    cb = np.zeros((128, 4096), np.float32)
    if half == 0:
        cf[:, CF_KB0:CF_KB0 + 4] = NEG
        cf[0:32, CF_CPAD] = NEG
    cf[:, CF_ONE] = 1.0
    p = np.arange(128)[:, None]
    q = np.arange(512)[None, :]
    for dg in range(4):
        cb[:, CB_CAUS + dg * 512:CB_CAUS + (dg + 1) * 512] = ((128 * dg + p) < q)
    jj = np.arange(128)[:, None]
    ss = np.arange(128)[None, :]
    cb[:, CB_NEGU:CB_NEGU + 128] = -(jj > ss).astype(np.float32)
    cb[:, CB_NEG1:CB_NEG1 + 128] = -1.0
    cb[:, 3584:3584 + 128] = -(jj <= ss).astype(np.float32)
    NSV = SV // 64
    kq = np.arange(128)
    wm = np.where(kq[None, :] < kq[:, None], 0.0, NEG)
    cb[:, CB_WM4:CB_WM4 + 512] = np.tile(wm, (1, 4))
    cb[0, CB_ONESROW:CB_ONESROW + 128] = 1.0
    if half == 0:
        cb[0, CB_CPADROW:CB_CPADROW + 32] = NEG
    for m in range(17):
        cb[m, CB_WI + m + 128] = 1.0
    cb[:, CB_ONESCOL] = 1.0
    cb[:, CB_ONES:CB_ONES + 128] = 1.0
    if half == 0:
        cf[:, CF_PADNEG:CF_PADNEG + 8] = -1e30
        cf[:, CF_F0 + 8] = 1e4
    else:
        cf[:, CF_F0 + 0] = 1e4
    cf[:, CF_EOFF:CF_EOFF + 64] = (np.arange(64) * CAP)[None, :]
    tt = np.arange(128)
    cb[:, CB_LTRI:CB_LTRI + 128] = (tt[:, None] < tt[None, :])
    c["c_f32"] = cf
    c["c_bf"] = bf(cb)
    k = np.arange(SV)
    ex = ((k[None, :] // 64) % 128 == np.arange(128)[:, None]).astype(np.float32)
    c["c_exp"] = bf(ex)
    c["c_oh"] = bf(onehot_planes())
    return c


def t5_bucket_np(n):
    n = np.maximum(np.asarray(n), 0)
    nf = np.maximum(n, 1).astype(np.float32)
    large = 16 + (np.log(nf / np.float32(16)) / np.float32(np.log(8.0)) * np.float32(16)).astype(np.int32)
    large = np.minimum(large, 31)
    return np.where(n < 16, n, large)


def onehot_planes():
    oh = np.zeros((128, OH_W), np.float32)
    k = np.arange(128)[:, None]
    q = np.arange(128)[None, :]
    d0 = q - k
    s0 = np.zeros((128, 33, 128), np.float32)
    b0 = t5_bucket_np(d0)
    for b in range(32):
        s0[:, b, :] = (d0 >= 0) & (b0 == b)
    s0[:, 32, :] = d0 < 0
    d1 = 128 + q - k
    s1 = np.zeros((128, 32, 128), np.float32)
    b1 = t5_bucket_np(d1)
    for b in range(32):
        s1[:, b, :] = (d1 < 128) & (b1 == b)
    r = np.arange(128)[:, None]
    m = np.arange(17)[None, :]
    dc = r - 16 * m + 129
    sc = np.zeros((128, 33, 17), np.float32)
    bc = t5_bucket_np(dc)
    for b in range(32):
        sc[:, b, :] = (dc >= 0) & (dc < 128) & (bc == b)
    sc[:, 32, :] = dc < 0
    oh[:, OH_S0:OH_S0 + 33 * 128] = s0.reshape(128, -1)
    oh[:, OH_S1:OH_S1 + 32 * 128] = s1.reshape(128, -1)
    oh[:, OH_BC:OH_BC + 33 * 17] = sc.reshape(128, -1)
    return oh


def padded_x(inp, S, b, half):
    xv = np.zeros((S + 512, D), np.float32)
    off = 512 * (1 - half)
    xv[off:off + S] = inp["x"][b]
    return xv


def prep_core_inputs(inp, S, b, half, shared, CAP, nhalf=1):
    m = dict(shared)
    m["cT"] = np.ascontiguousarray(inp["c"][b].reshape(16, 128).T)
    if nhalf == 1:
        m["xv"] = padded_x(inp, S, b, half)
        m.update(host_consts(S, half, CAP))
    else:
        for h in range(2):
            m["xv_h%d" % h] = padded_x(inp, S, b, h)
            hc = host_consts(S, h, CAP)
            m["c_f32_h%d" % h] = hc.pop("c_f32")
            m["c_bf_h%d" % h] = hc.pop("c_bf")
            m.update(hc)
    return m


def prep_shared(inp, names):
    sh = {}
    sh["ada_w"] = inp["ada_w"][0]
    sh["ada_bT"] = np.ascontiguousarray(inp["ada_b"][0].reshape(96, 128).T)
    sh["g1T"] = np.ascontiguousarray(inp["norm1_g"][0].reshape(16, 128).T)
    sh["g2row"] = inp["norm2_g"][0].reshape(1, D)
    sh["gfrow"] = inp["normf_g"].reshape(1, D)
    sh["w_in"] = inp["w_in"][0]
    sh["relb"] = inp["rel_bias"]
    sh["pe_kT"] = np.ascontiguousarray(inp["cmp_pe_k"][0].T)
    sh["pe_vT"] = np.ascontiguousarray(inp["cmp_pe_v"][0].T)
    sh["cw1k"] = inp["cmp_w1_k"][0]
    sh["cw2k"] = inp["cmp_w2_k"][0]
    sh["cw1v"] = inp["cmp_w1_v"][0]
    sh["cw2v"] = inp["cmp_w2_v"][0]
    sh["wba"] = inp["w_branch_a"][0]
    sh["wbb"] = inp["w_branch_b"][0]
    sh["wout"] = inp["w_out"][0]
    sh["wr"] = np.ascontiguousarray(np.concatenate([inp["router_w_grp"][0], inp["router_w_exp"][0]], axis=1))
    sh["br"] = np.concatenate([inp["router_b_grp"][0], inp["router_b_exp"][0]]).reshape(1, 72)
    def relay(w):
        w = w.reshape(NEXP, NDC, 128, NFC, 128)
        return np.ascontiguousarray(w.transpose(0, 3, 2, 1, 4)).reshape(NEXP, NFC, 128, NDC * 128)
    if "ew1" in names:
        sh["ew1"] = relay(inp["expert_w1"][0])
        sh["ew3"] = relay(inp["expert_w3"][0])
        sh["ew2"] = inp["expert_w2"][0]
    return sh


def run(inp, S, B, CAP, taps=(), upto=99, nhalf=1):
    inp = {k: np.asarray(v) for k, v in inp.items()}
    nc = build_program(S, CAP, taps=taps, upto=upto, nhalf=nhalf)
    shared = prep_shared(inp, nc._inp_names)
    in_maps = []
    ncores = 2 * B if nhalf == 1 else B
    for core in range(ncores):
        if nhalf == 1:
            m = prep_core_inputs(inp, S, core // 2, core % 2, shared, CAP)
        else:
            m = prep_core_inputs(inp, S, core, None, shared, CAP, nhalf=2)
        in_maps.append({k: m[k] for k in nc._inp_names})
    res = run_bass_kernel_spmd(nc, in_maps, core_ids=list(range(ncores)))
    return res.results


def assemble(results, S, B, nhalf=1):
    out = np.zeros((B, S, D), np.float32)
    NQT = S // 1024
    for core, r in enumerate(results):
        if nhalf == 1:
            parts = [(core // 2, core % 2, r["out"])]
        else:
            parts = [(core, h, r["out_h%d" % h]) for h in range(2)]
        for (b, half, o) in parts:
            for k in range(NQT):
                gt = 2 * k + half
                out[b, gt * 512:(gt + 1) * 512] = o[k * 512:(k + 1) * 512]
    return out


def kernel(**inputs):
    res = run(inputs, 8192, 4, 512, nhalf=1)
    return assemble(res, 8192, 4, nhalf=1)
```

```python
import contextlib
import numpy as np
import ml_dtypes
import concourse.bass as bass
import concourse.mybir as mybir
from concourse.bass_utils import run_bass_kernel_spmd

F32 = mybir.dt.float32
BF16 = mybir.dt.bfloat16
I32 = mybir.dt.int32
U32 = mybir.dt.uint32
AF = mybir.ActivationFunctionType
ALU = mybir.AluOpType
AX = mybir.AxisListType

D = 2048
NDC = 16
DH = 128
IN_COLS = 9752
NEXP = 64
EH = 1408
NFC = 11
SCALE = 128 ** -0.5
NEG = -30000.0


class Buf:
    __slots__ = ("name", "last_w", "readers")

    def __init__(self, name=""):
        self.name = name
        self.last_w = None
        self.readers = []


class Sched:
    def __init__(self, nc, stack):
        self.nc = nc
        self.stack = stack
        self.engs = {"pe": nc.tensor, "act": nc.scalar, "dve": nc.vector, "pool": nc.gpsimd, "sp": nc.sync}
        self.sems = {}
        self.cnt = {}
        self.seen = {e: {} for e in self.engs}
        for e in ("pe", "act", "dve", "pool"):
            self.sems[e] = stack.enter_context(nc.semaphore("s_" + e))
            self.cnt[e] = 0
        self.dsems = {}
        self.ninst = 0

    def dma_sem(self, key):
        if key not in self.dsems:
            self.dsems[key] = self.stack.enter_context(self.nc.semaphore("d_%s" % (key,)))
            self.cnt[("d", key)] = 0
        return key

    def _semobj(self, k):
        return self.sems[k] if k in self.sems else self.dsems[k[1]]

    def issue(self, e, fn, reads=(), writes=(), dma=None, pe_acc=False):
        eng = self.engs[e]
        need = {}
        deps = []
        for b in reads:
            if b.last_w is not None:
                deps.append(b.last_w)
        for b in writes:
            if b.last_w is not None:
                deps.append(b.last_w)
            deps.extend(b.readers)
        for (k, v) in deps:
            if pe_acc and k == "pe" and e == "pe":
                continue
            if need.get(k, 0) < v:
                need[k] = v
        for k, v in need.items():
            if self.seen[e].get(k, 0) < v:
                eng.wait_ge(self._semobj(k), v)
                self.seen[e][k] = v
        if dma is not None and isinstance(dma, str) and dma[0] == "u" and dma[1:].isdigit():
            self.dma_sem(dma)
            kk0 = ("d", dma)
            if self.cnt[kk0] > 0 and self.seen[e].get(kk0, 0) < self.cnt[kk0]:
                eng.wait_ge(self.dsems[dma], self.cnt[kk0])
                self.seen[e][kk0] = self.cnt[kk0]
        inst = fn(eng)
        self.ninst += 1
        if dma is not None:
            self.dma_sem(dma)
            kk = ("d", dma)
            self.cnt[kk] += 16
            inst.then_inc(self.dsems[dma], 16)
            op = (kk, self.cnt[kk])
        else:
            self.cnt[e] += 1
            inst.then_inc(self.sems[e], 1)
            op = (e, self.cnt[e])
        for b in writes:
            b.last_w = op
            b.readers = []
        for b in reads:
            if b not in writes:
                b.readers.append(op)
                if len(b.readers) > 48:
                    mx = {}
                    for (k, v) in b.readers:
                        if mx.get(k, 0) < v:
                            mx[k] = v
                    b.readers = list(mx.items())
        return op

    def wait_bufs(self, e, bufs):
        eng = self.engs[e]
        for b in bufs:
            for dep in ([b.last_w] if b.last_w is not None else []) + list(b.readers):
                k, v = dep
                if self.seen[e].get(k, 0) < v:
                    eng.wait_ge(self._semobj(k), v)
                    self.seen[e][k] = v


class T:
    n = 0

    def __init__(self, st, nc, name, shape, dtype, psum=False):
        T.n += 1
        nm = "t%d_%s" % (T.n, name)
        if psum:
            self.t = st.enter_context(nc.psum_tensor(nm, shape, dtype))
        else:
            self.t = st.enter_context(nc.sbuf_tensor(nm, shape, dtype))
        self.b = Buf(name)

    def __getitem__(self, idx):
        return self.t[idx]


class DR:
    def __init__(self, nc, name, shape, dtype, kind="Internal"):
        self.h = nc.dram_tensor(name, list(shape), dtype, kind=kind)
        self.ap = self.h.ap()
        self.bufs = {}
        self.name = name

    def buf(self, key=0):
        if key not in self.bufs:
            self.bufs[key] = Buf("%s_%s" % (self.name, key))
        return self.bufs[key]

    def allbufs(self):
        return list(self.bufs.values())


def own_tiles(S, half):
    NT = S // 512
    return [t for t in range(NT) if ((t % 4) in (0, 3)) == (half == 0)]


class Ctx:
    pass


def build_program(S, CAP, taps=(), upto=99, nhalf=1):
    nc = bass.Bass("TRN2", target_bir_lowering=False)
    g = Ctx()
    g.nc = nc
    g.S, g.CAP = S, CAP
    g.SV = S + 512
    g.NTV = g.SV // 512
    g.NKB = g.SV // 128
    g.NQT = (g.NTV - 1) // 2
    g.NQ = g.NQT * 512
    g.NQB = g.NQ // 128
    g.NCV = g.SV // 16 - 1
    g.NSV = g.SV // 64
    g.taps = set(taps)
    g._u = [0]

    def uniq():
        g._u[0] += 1
        return 'u%d' % (g._u[0] % 16)

    g.uniq = uniq
    g.upto = upto

    g.sfx = ""

    def dr(name, shape, dtype, kind=None):
        if kind is None:
            kind = "ExternalOutput" if name in g.taps else "Internal"
        return DR(nc, name + g.sfx, shape, dtype, kind)

    g.dr = dr
    inp = {}

    def ein(name, shape, dtype=F32):
        inp[name] = DR(nc, name, shape, dtype, "ExternalInput")
        return inp[name]

    g.inp = inp
    sfxs = [""] if nhalf == 1 else ["_h0", "_h1"]
    for sf in sfxs:
        ein("xv" + sf, [g.SV, D])
        ein("c_f32" + sf, [128, 1024])
        ein("c_bf" + sf, [128, 4096], BF16)
    ein("cT", [128, 16])
    ein("ada_w", [D, 6 * D])
    ein("ada_bT", [128, 96])
    ein("g1T", [128, 16])
    ein("g2row", [1, D])
    ein("gfrow", [1, D])
    ein("w_in", [D, IN_COLS])
    ein("relb", [32, 8])
    ein("pe_kT", [128, 32])
    ein("pe_vT", [128, 32])
    ein("cw1k", [4096, 256])
    ein("cw2k", [256, 128])
    ein("cw1v", [4096, 256])
    ein("cw2v", [256, 128])
    ein("wba", [1024, D])
    ein("wbb", [1024, D])
    ein("wout", [D, D])
    ein("wr", [D, 72])
    ein("br", [1, 72])
    if upto >= 7:
        ein("ew1", [NEXP, NFC, 128, NDC * 128])
        ein("ew3", [NEXP, NFC, 128, NDC * 128])
        ein("ew2", [NEXP, EH, D])
    ein("c_idf", [128, 128])
    ein("c_idb", [128, 128], BF16)
    ein("c_exp", [128, g.SV], BF16)
    ein("c_oh", [128, OH_W], BF16)
    g.outs = [DR(nc, "out" + sf, [g.NQ, D], F32, "ExternalOutput") for sf in sfxs]
    g.tapdr = []
    names = list(inp.keys())

    with contextlib.ExitStack() as st:
        g.sch = Sched(nc, st)
        for hi, sf in enumerate(sfxs):
            g.sfx = sf
            g.out = g.outs[hi]
            for nm in ("xv", "c_f32", "c_bf"):
                inp[nm] = inp[nm + sf]
            with contextlib.ExitStack() as hst:
                g.st = hst
                phase0(g)
                if upto >= 1:
                    phase1(g)
                if upto >= 3:
                    phase2(g)
                if upto >= 4:
                    phase3(g)
                if upto >= 5:
                    phase4(g)
                if upto >= 6:
                    phase5(g)
                if upto >= 7:
                    phase6(g)
                    phase7(g)
                barrier(g)
        finish(g)
    nc._inp_names = names
    return nc


def finish(g):
    sch = g.sch
    outs = list(g.outs) + [d for d in g.tapdr]
    for d in outs:
        sch.wait_bufs("sp", d.allbufs())


def phase0(g):
    nc, sch, st = g.nc, g.sch, g.st
    S = sch.issue
    g.idf = T(st, nc, "idf", [128, 128], F32)
    g.idb = T(st, nc, "idb", [128, 128], BF16)
    g.cf = T(st, nc, "cf", [128, 1024], F32)
    g.cb = T(st, nc, "cb", [128, 4096], BF16)
    S("sp", lambda e: e.dma_start(out=g.idf[:], in_=g.inp["c_idf"].ap), writes=[g.idf.b], dma=g.uniq())
    S("sp", lambda e: e.dma_start(out=g.idb[:], in_=g.inp["c_idb"].ap), writes=[g.idb.b], dma=g.uniq())
    S("sp", lambda e: e.dma_start(out=g.cf[:], in_=g.inp["c_f32"].ap), writes=[g.cf.b], dma=g.uniq())
    S("sp", lambda e: e.dma_start(out=g.cb[:], in_=g.inp["c_bf"].ap), writes=[g.cb.b], dma=g.uniq())
    g.modT = T(st, nc, "modT", [128, 96], F32)
    g.a1T = T(st, nc, "a1T", [128, 16], F32)
    g.modD = g.dr("modD", [96, 128], F32)
    if "modD" in g.taps:
        g.tapdr.append(g.modD)
    with contextlib.ExitStack() as ls:
        cT = T(ls, nc, "cT", [128, 16], F32)
        cact = T(ls, nc, "cact", [128, 16], F32)
        abT = T(ls, nc, "abT", [128, 96], F32)
        g1T = T(ls, nc, "g1Tt", [128, 16], F32)
        stg = [T(ls, nc, "adaw%d" % i, [128, 16, 512], F32) for i in range(2)]
        mps = T(ls, nc, "modps", [128, 96], F32, psum=True)
        tps = T(ls, nc, "modtp", [128, 128], F32, psum=True)
        mrow = T(ls, nc, "mrow", [96, 128], F32)
        S("sp", lambda e: e.dma_start(out=cT[:], in_=g.inp["cT"].ap), writes=[cT.b], dma=g.uniq())
        S("sp", lambda e: e.dma_start(out=abT[:], in_=g.inp["ada_bT"].ap), writes=[abT.b], dma=g.uniq())
        S("sp", lambda e: e.dma_start(out=g1T[:], in_=g.inp["g1T"].ap), writes=[g1T.b], dma=g.uniq())
        S("act", lambda e: e.activation(out=cact[:], in_=cT[:], func=AF.Silu), reads=[cT.b], writes=[cact.b])
        aw = g.inp["ada_w"].ap
        for slab in range(24):
            sg = stg[slab % 2]
            src = aw[:, slab * 512:(slab + 1) * 512].rearrange("(dc p) j -> p dc j", p=128)
            S("sp", lambda e, sg=sg, src=src: e.dma_start(out=sg[:], in_=src), writes=[sg.b], dma="adaw%d" % (slab % 2))
            for jl in range(4):
                jc = slab * 4 + jl
                for dc in range(16):
                    S("pe", lambda e, sg=sg, jl=jl, jc=jc, dc=dc: e.matmul(
                        mps[:, jc:jc + 1], sg[:, dc, jl * 128:(jl + 1) * 128], cact[:, dc:dc + 1],
                        start=(dc == 0), stop=(dc == 15)),
                      reads=[sg.b, cact.b], writes=[mps.b], pe_acc=True)
        S("dve", lambda e: e.tensor_tensor(out=g.modT[:], in0=mps[:], in1=abT[:], op=ALU.add),
          reads=[mps.b, abT.b], writes=[g.modT.b])
        S("dve", lambda e: e.tensor_scalar(out=g.a1T[:], in0=g.modT[:, 16:32], scalar1=1.0, scalar2=None, op0=ALU.add),
          reads=[g.modT.b], writes=[g.a1T.b])
        S("dve", lambda e: e.tensor_tensor(out=g.a1T[:], in0=g.a1T[:], in1=g1T[:], op=ALU.mult),
          reads=[g.a1T.b, g1T.b], writes=[g.a1T.b])
        S("pe", lambda e: e.transpose(tps[0:96, :], g.modT[:], g.idf[:]), reads=[g.modT.b, g.idf.b], writes=[tps.b])
        S("act", lambda e: e.copy(mrow[:], tps[0:96, :]), reads=[tps.b], writes=[mrow.b])
        S("sp", lambda e: e.dma_start(out=g.modD.ap, in_=mrow[:]), reads=[mrow.b], writes=[g.modD.buf()], dma=g.uniq())


def load_rows(g, st, names):
    nc, sch = g.nc, g.sch
    S = sch.issue
    md = g.modD.ap

    def rowsrc(k):
        return md[k * 16:(k + 1) * 16, :].rearrange("(o a) b -> o (a b)", o=1).partition_broadcast(128)

    idx = {"gt1": 2, "sh2": 3, "a2": 4, "gt2": 5}
    for nm in names:
        if nm == "gf":
            S("sp", lambda e: e.dma_start(out=g.rows["gf"][:], in_=g.inp["gfrow"].ap.partition_broadcast(128)), writes=[g.rows["gf"].b], dma=g.uniq())
        else:
            S("sp", lambda e, nm=nm: e.dma_start(out=g.rows[nm][:], in_=rowsrc(idx[nm])), reads=[g.modD.buf()], writes=[g.rows[nm].b], dma=g.uniq())
    if "a2" in names:
        with contextlib.ExitStack() as ls:
            g2r = T(ls, nc, "g2r", [128, D], F32)
            S("sp", lambda e: e.dma_start(out=g2r[:], in_=g.inp["g2row"].ap.partition_broadcast(128)), writes=[g2r.b], dma=g.uniq())
            S("dve", lambda e: e.tensor_scalar(out=g.rows["a2"][:], in0=g.rows["a2"][:], scalar1=1.0, scalar2=None, op0=ALU.add),
              reads=[g.rows["a2"].b], writes=[g.rows["a2"].b])
            S("dve", lambda e: e.tensor_tensor(out=g.rows["a2"][:], in0=g.rows["a2"][:], in1=g2r[:], op=ALU.mult),
              reads=[g.rows["a2"].b, g2r.b], writes=[g.rows["a2"].b])
            barrier(g)


def barrier(g):
    sch = g.sch
    for e, eng in sch.engs.items():
        for k in list(sch.sems.keys()):
            v = sch.cnt[k]
            if v > 0 and sch.seen[e].get(k, 0) < v:
                eng.wait_ge(sch.sems[k], v)
                sch.seen[e][k] = v
        for key in list(sch.dsems.keys()):
            kk = ("d", key)
            v = sch.cnt[kk]
            if v > 0 and sch.seen[e].get(kk, 0) < v:
                eng.wait_ge(sch.dsems[key], v)
                sch.seen[e][kk] = v


def phase1(g):
    nc, sch, st = g.nc, g.sch, g.st
    S = sch.issue
    SV, NTV, NKB, NQT, NQ, NQB = g.SV, g.NTV, g.NKB, g.NQT, g.NQ, g.NQB
    g.XNT = g.dr("XNT", [NTV, 128, 16 * 512], BF16)
    xv = g.inp["xv"].ap
    sh1 = g.modT
    with contextlib.ExitStack() as ls:
        xt = [T(ls, nc, "xt%d" % i, [128, D], F32) for i in range(2)]
        junk = T(ls, nc, "junk", [128, D], BF16)
        yb = [T(ls, nc, "yb%d" % i, [128, D], BF16) for i in range(2)]
        ss = [T(ls, nc, "ss%d" % i, [128, 1], F32) for i in range(2)]
        xn = [T(ls, nc, "xn%d" % i, [128, 16, 512], BF16) for i in range(2)]
        tp = [T(ls, nc, "tp%d" % i, [128, 2048], BF16, psum=True) for i in range(2)]
        for tb in range(NKB):
            Tt, sub = tb // 4, tb % 4
            x_, y_, s_, p_ = xt[tb % 2], yb[tb % 2], ss[tb % 2], tp[tb % 2]
            xo = xn[Tt % 2]
            S("sp", lambda e, x_=x_, tb=tb: e.dma_start(out=x_[:], in_=xv[tb * 128:(tb + 1) * 128, :]),
              writes=[x_.b], dma="xt%d" % (tb % 2))
            S("act", lambda e, x_=x_, s_=s_: e.activation(out=junk[:], in_=x_[:], func=AF.Square, accum_out=s_[:]),
              reads=[x_.b], writes=[junk.b, s_.b])
            S("dve", lambda e, s_=s_: e.tensor_scalar(out=s_[:], in0=s_[:], scalar1=1.0 / D, scalar2=1e-6, op0=ALU.mult, op1=ALU.add),
              reads=[s_.b], writes=[s_.b])
            S("act", lambda e, s_=s_: e.activation(out=s_[:], in_=s_[:], func=AF.Sqrt), reads=[s_.b], writes=[s_.b])
            S("dve", lambda e, s_=s_: e.reciprocal(out=s_[:], in_=s_[:]), reads=[s_.b], writes=[s_.b])
            S("act", lambda e, x_=x_, y_=y_, s_=s_: e.activation(out=y_[:], in_=x_[:], func=AF.Identity, scale=s_[:, 0:1]),
              reads=[x_.b, s_.b], writes=[y_.b])
            for dc in range(16):
                S("pe", lambda e, y_=y_, p_=p_, dc=dc: e.transpose(p_[:, dc * 128:(dc + 1) * 128], y_[:, dc * 128:(dc + 1) * 128], g.idb[:]),
                  reads=[y_.b, g.idb.b], writes=[p_.b], pe_acc=True)
            for dc in range(16):
                eng = "act" if dc % 2 == 0 else "dve"
                if eng == "act":
                    S("act", lambda e, p_=p_, xo=xo, dc=dc, sub=sub: e.activation(
                        out=xo[:, dc, sub * 128:(sub + 1) * 128], in_=p_[:, dc * 128:(dc + 1) * 128], func=AF.Identity,
                        scale=g.a1T[:, dc:dc + 1], bias=sh1[:, dc:dc + 1]),
                      reads=[p_.b, g.a1T.b, sh1.b], writes=[xo.b])
                else:
                    S("dve", lambda e, p_=p_, xo=xo, dc=dc, sub=sub: e.tensor_scalar(
                        out=xo[:, dc, sub * 128:(sub + 1) * 128], in0=p_[:, dc * 128:(dc + 1) * 128],
                        scalar1=g.a1T[:, dc:dc + 1], scalar2=sh1[:, dc:dc + 1], op0=ALU.mult, op1=ALU.add),
                      reads=[p_.b, g.a1T.b, sh1.b], writes=[xo.b])
            if sub == 3:
                S("sp", lambda e, xo=xo, Tt=Tt: e.dma_start(out=g.XNT.ap[Tt], in_=xo[:].rearrange("p a b -> p (a b)")),
                  reads=[xo.b], writes=[g.XNT.buf(Tt)], dma="xnt_st")
        barrier(g)
    if "XNT" in g.taps:
        g.tapdr.append(g.XNT)
    if g.upto < 2:
        return
    dr = g.dr
    g.QAT = dr("QAT", [8, 128, NQ], BF16)
    g.KAT = dr("KAT", [8, 128, SV], BF16)
    g.VA = dr("VA", [8, 128, NKB, 128], BF16)
    g.QBT = dr("QBT", [8, 128, NQ], BF16)
    g.CPT = dr("CPT", [4, 128, SV], BF16)
    g.KSLT = dr("KSLT", [2, 128, SV], BF16)
    g.VSL = dr("VSL", [2, 128, NKB, 128], BF16)
    g.KSWT = dr("KSWT", [2, 128, SV], BF16)
    g.VSW = dr("VSW", [2, 128, NKB, 128], BF16)
    g.GN = dr("GN", [NQB, 128, 24], F32)
    g.GAT = dr("GAT", [16, 128, NQ], BF16)
    g.GBT = dr("GBT", [16, 128, NQ], BF16)
    for nm in ("QAT", "KAT", "VA", "QBT", "CPT", "KSLT", "VSL", "KSWT", "VSW", "GN", "GAT", "GBT"):
        if nm in g.taps:
            g.tapdr.append(getattr(g, nm))

    def fm_store(dst, i0, own):
        def f(stg, ncc, tk):
            t0 = tk * 512
            return lambda e: e.dma_start(out=dst.ap[i0:i0 + ncc, :, t0:t0 + 512].rearrange("h p t -> p h t"), in_=stg[:, 0:ncc, :])
        return f

    def tmv_store(dst, i0):
        def f(stg, nh, tk):
            return [(lambda e, h=h: e.dma_start(
                out=dst.ap[i0 + h, :, 4 * tk:4 * tk + 4, :],
                in_=stg[:, :, h * 128:(h + 1) * 128])) for h in range(nh)]
        return f

    jobs = []
    for j in range(2):
        jobs.append((j * 512, 512, "fm", True, False, fm_store(g.QAT, 4 * j, True), g.QAT))
    for j in range(2):
        jobs.append((1024 + j * 512, 512, "fm", False, False, fm_store(g.KAT, 4 * j, False), g.KAT))
    for j in range(2):
        jobs.append((2048 + j * 512, 512, "tm", False, False, tmv_store(g.VA, 4 * j), g.VA))
    for j in range(2):
        jobs.append((3072 + j * 512, 512, "fm", True, False, fm_store(g.QBT, 4 * j, True), g.QBT))
    jobs.append((4096, 512, "fm", False, False, fm_store(g.CPT, 0, False), g.CPT))
    jobs.append((4608, 256, "fm", False, False, fm_store(g.KSLT, 0, False), g.KSLT))
    jobs.append((4864, 256, "tm", False, False, tmv_store(g.VSL, 0), g.VSL))
    jobs.append((5120, 256, "fm", False, False, fm_store(g.KSWT, 0, False), g.KSWT))
    jobs.append((5376, 256, "tm", False, False, tmv_store(g.VSW, 0), g.VSW))
    jobs.append((5632, 24, "gn", True, True, None, g.GN))
    for j in range(4):
        jobs.append((5656 + j * 512, 512, "fm", True, True, fm_store(g.GAT, 4 * j, True), g.GAT))
    for j in range(4):
        jobs.append((7704 + j * 512, 512, "fm", True, True, fm_store(g.GBT, 4 * j, True), g.GBT))

    win = g.inp["w_in"].ap
    with contextlib.ExitStack() as ls:
        wst = [T(ls, nc, "wst%d" % i, [128, 16, 512], F32) for i in range(2)]
        wb = [T(ls, nc, "wb%d" % i, [128, 16, 512], BF16) for i in range(4)]
        xn = [T(ls, nc, "xnl%d" % i, [128, 16, 512], BF16) for i in range(2)]
        ostg = [T(ls, nc, "ostg%d" % i, [128, 4, 512], BF16) for i in range(2)]
        gstg = [T(ls, nc, "gstg%d" % i, [128, 4, 24], F32) for i in range(2)]
        ps = [T(ls, nc, "pps%d" % i, [128, 512], F32, psum=True) for i in range(8)]
        nload = 0
        nout = 0
        nslab = 0
        prev_tiles = 0
        pairs = []
        for flag in (False, True):
            js = [j for j in jobs if j[3] == flag]
            for i in range(0, len(js), 2):
                pairs.append(js[i:i + 2])
        for pi, pair in enumerate(pairs):
            wbs = []
            for jj, (c0, w, kind, own, sig, store, dst) in enumerate(pair):
                ws_ = wst[nslab % 2]
                wb_ = wb[(pi % 2) * 2 + jj]
                wbs.append(wb_)
                src = win[:, c0:c0 + w].rearrange("(dc p) j -> p dc j", p=128)
                S("sp", lambda e, ws_=ws_, src=src, w=w: e.dma_start(out=ws_[:, :, 0:w], in_=src), writes=[ws_.b], dma="wst%d" % (nslab % 2))
                nslab += 1
                for (eng, d0, d1) in (("act", 0, 6), ("dve", 6, 13), ("pool", 13, 16)):
                    if eng == "act":
                        S("act", lambda e, ws_=ws_, wb_=wb_, d0=d0, d1=d1, w=w: e.copy(wb_[:, d0:d1, 0:w], ws_[:, d0:d1, 0:w]),
                          reads=[ws_.b], writes=[wb_.b])
                    else:
                        S(eng, lambda e, ws_=ws_, wb_=wb_, d0=d0, d1=d1, w=w: e.tensor_copy(wb_[:, d0:d1, 0:w], ws_[:, d0:d1, 0:w]),
                          reads=[ws_.b], writes=[wb_.b])
            own = pair[0][3]
            tiles = [(k, 2 * k + 1) for k in range(NQT)] if own else [(t, t) for t in range(NTV)]
            nload += prev_tiles
            prev_tiles = len(tiles)
            def xload(ti):
                tv = tiles[ti][1]
                xl = xn[(nload + ti) % 2]
                S("sp", lambda e, xl=xl, tv=tv: e.dma_start(out=xl[:].rearrange("p a b -> p (a b)"), in_=g.XNT.ap[tv]),
                  reads=[g.XNT.buf(tv)], writes=[xl.b], dma="xnl%d" % ((nload + ti) % 2))

            xload(0)
            for ti, (tk, tv) in enumerate(tiles):
                xl = xn[(nload + ti) % 2]
                if ti + 1 < len(tiles):
                    xload(ti + 1)
                for jj, (c0, w, kind, own_, sig, store, dst) in enumerate(pair):
                  wb_ = wbs[jj]
                  pb = [ps[(nout % 2) * 4 + i] for i in range(4)]
                  og = ostg[nout % 2]
                  gg = gstg[nout % 2]
                  nout += 1
                  if kind == "fm":
                      ncc = w // 128
                      for cc in range(ncc):
                          for dc in range(16):
                              S("pe", lambda e, cc=cc, dc=dc, xl=xl, wb_=wb_, pb=pb: e.matmul(
                                  pb[cc][:, :], wb_[:, dc, cc * 128:(cc + 1) * 128], xl[:, dc, :], start=(dc == 0), stop=(dc == 15)),
                                reads=[wb_.b, xl.b], writes=[pb[cc].b], pe_acc=True)
                      for cc in range(ncc):
                          if sig:
                              S("act", lambda e, cc=cc, og=og, pb=pb: e.activation(out=og[:, cc, :], in_=pb[cc][:, :], func=AF.Sigmoid),
                                reads=[pb[cc].b], writes=[og.b])
                          elif cc % 2 == 0:
                              S("act", lambda e, cc=cc, og=og, pb=pb: e.copy(og[:, cc, :], pb[cc][:, :]), reads=[pb[cc].b], writes=[og.b])
                          else:
                              S("dve", lambda e, cc=cc, og=og, pb=pb: e.tensor_copy(og[:, cc, :], pb[cc][:, :]), reads=[pb[cc].b], writes=[og.b])
                      S("sp", store(og, ncc, tk), reads=[og.b], writes=[dst.buf(tk)], dma="pst")
                  elif kind == "tm":
                      for sub in range(4):
                          for dc in range(16):
                              S("pe", lambda e, sub=sub, dc=dc, xl=xl, wb_=wb_, pb=pb, w=w: e.matmul(
                                  pb[sub][:, 0:w], xl[:, dc, sub * 128:(sub + 1) * 128], wb_[:, dc, 0:w], start=(dc == 0), stop=(dc == 15)),
                                reads=[wb_.b, xl.b], writes=[pb[sub].b], pe_acc=True)
                      for sub in range(4):
                          if sub % 2 == 0:
                              S("act", lambda e, sub=sub, og=og, pb=pb, w=w: e.copy(og[:, sub, 0:w], pb[sub][:, 0:w]), reads=[pb[sub].b], writes=[og.b])
                          else:
                              S("dve", lambda e, sub=sub, og=og, pb=pb, w=w: e.tensor_copy(og[:, sub, 0:w], pb[sub][:, 0:w]), reads=[pb[sub].b], writes=[og.b])
                      for fn in store(og, w // 128, tk):
                          S("sp", fn, reads=[og.b], writes=[dst.buf(tk)], dma="pst")
                  else:
                      for sub in range(4):
                          for dc in range(16):
                              S("pe", lambda e, sub=sub, dc=dc, xl=xl, wb_=wb_, pb=pb, w=w: e.matmul(
                                  pb[sub][:, 0:w], xl[:, dc, sub * 128:(sub + 1) * 128], wb_[:, dc, 0:w], start=(dc == 0), stop=(dc == 15)),
                                reads=[wb_.b, xl.b], writes=[pb[sub].b], pe_acc=True)
                      for sub in range(4):
                          S("act", lambda e, sub=sub, gg=gg, pb=pb, w=w: e.activation(out=gg[:, sub, :], in_=pb[sub][:, 0:w], func=AF.Sigmoid),
                            reads=[pb[sub].b], writes=[gg.b])
                      S("sp", lambda e, gg=gg, tk=tk: e.dma_start(out=g.GN.ap[4 * tk:4 * tk + 4].rearrange("s p c -> p s c"), in_=gg[:]),
                        reads=[gg.b], writes=[g.GN.buf(tk)], dma="pst")
        barrier(g)


CF_KB0, CF_ZERO, CF_ONE, CF_CPAD = 0, 4, 5, 8
CB_CAUS, CB_NEGU, CB_NEG1 = 0, 2048, 2176


def phase2(g):
    nc, sch, st = g.nc, g.sch, g.st
    S = sch.issue
    SV, NCV = g.SV, g.NCV
    NCH = (NCV + 127) // 128
    g.NCH = NCH
    g.kcmpT = [T(st, nc, "kcmpT%d" % i, [128, NCH * 128], BF16) for i in range(2)]
    g.vcmp = [T(st, nc, "vcmp%d" % i, [128, NCH, 128], BF16) for i in range(2)]
    nrs = [(n0, min(512, NCV - n0)) for n0 in range(0, NCV, 512)]
    with contextlib.ExitStack() as ls:
        w1s = T(ls, nc, "w1s", [128, 32, 256], F32)
        w1b = T(ls, nc, "w1b", [128, 32, 256], BF16)
        w2s = T(ls, nc, "w2s", [128, 2, 128], F32)
        w2b = T(ls, nc, "w2b", [128, 2, 128], BF16)
        pes = T(ls, nc, "pes", [128, 32], F32)
        peb = T(ls, nc, "peb", [128, 32], BF16)
        cv = T(ls, nc, "cv", [128, 2], F32)
        src = [T(ls, nc, "csrc%d" % i, [128, SV], BF16) for i in range(2)]
        u = T(ls, nc, "cu", [128, 512], F32)
        u2 = T(ls, nc, "cu2", [128, 512], F32)
        gl = [T(ls, nc, "gl%d" % i, [128, NCH * 128], BF16) for i in range(2)]
        hps = [T(ls, nc, "hps%d" % i, [128, 512], F32, psum=True) for i in range(2)]
        cps = T(ls, nc, "cps", [128, 2], F32, psum=True)
        ops = T(ls, nc, "cops", [128, 512], F32, psum=True)
        nsrc = 0
        for kv in range(2):
            w1d = g.inp["cw1k" if kv == 0 else "cw1v"].ap
            w2d = g.inp["cw2k" if kv == 0 else "cw2v"].ap
            ped = g.inp["pe_kT" if kv == 0 else "pe_vT"].ap
            S("sp", lambda e, w1d=w1d: e.dma_start(out=w1s[:], in_=w1d.rearrange("(p d) j -> d p j", d=128)), writes=[w1s.b], dma="cw1")
            S("sp", lambda e, w2d=w2d: e.dma_start(out=w2s[:], in_=w2d.rearrange("(c j) d -> j c d", j=128)), writes=[w2s.b], dma="cw2")
            S("sp", lambda e, ped=ped: e.dma_start(out=pes[:], in_=ped), writes=[pes.b], dma="cpe")
            S("act", lambda e: e.copy(w1b[:, 0:16, :], w1s[:, 0:16, :]), reads=[w1s.b], writes=[w1b.b])
            S("dve", lambda e: e.tensor_copy(w1b[:, 16:32, :], w1s[:, 16:32, :]), reads=[w1s.b], writes=[w1b.b])
            S("dve", lambda e: e.tensor_copy(w2b[:], w2s[:]), reads=[w2s.b], writes=[w2b.b])
            S("dve", lambda e: e.tensor_copy(peb[:], pes[:]), reads=[pes.b], writes=[peb.b])
            for jc in range(2):
                for p in range(32):
                    S("pe", lambda e, jc=jc, p=p: e.matmul(cps[:, jc:jc + 1], w1b[:, p, jc * 128:(jc + 1) * 128], peb[:, p:p + 1],
                                                           start=(p == 0), stop=(p == 31)),
                      reads=[w1b.b, peb.b], writes=[cps.b], pe_acc=True)
            S("dve", lambda e: e.tensor_copy(cv[:], cps[:]), reads=[cps.b], writes=[cv.b])
            for gi in range(2):
                sr = src[nsrc % 2]
                nsrc += 1
                S("sp", lambda e, sr=sr, kv=kv, gi=gi: e.dma_start(out=sr[:], in_=g.CPT.ap[kv * 2 + gi]),
                  reads=g.CPT.allbufs(), writes=[sr.b], dma="csrc%d" % (nsrc % 2))
                srv = sr[:].rearrange("p (n s) -> p n s", s=16)
                for jc in range(2):
                    for (n0, nn) in nrs:
                        hp = hps[(jc + n0 // 512) % 2]
                        for p in range(32):
                            S("pe", lambda e, hp=hp, jc=jc, p=p, n0=n0, nn=nn, srv=srv: e.matmul(
                                hp[:, 0:nn], w1b[:, p, jc * 128:(jc + 1) * 128], srv[:, n0 + p // 16:n0 + p // 16 + nn, p % 16],
                                start=(p == 0), stop=(p == 31)),
                              reads=[w1b.b, sr.b], writes=[hp.b], pe_acc=True)
                        S("dve", lambda e, hp=hp, jc=jc, nn=nn: e.tensor_scalar(out=u[:, 0:nn], in0=hp[:, 0:nn], scalar1=cv[:, jc:jc + 1], scalar2=None, op0=ALU.add),
                          reads=[hp.b, cv.b], writes=[u.b])
                        S("dve", lambda e, nn=nn: e.tensor_tensor(out=u2[:, 0:nn], in0=u[:, 0:nn], in1=u[:, 0:nn], op=ALU.mult), reads=[u.b], writes=[u2.b])
                        S("dve", lambda e, nn=nn: e.tensor_scalar(out=u2[:, 0:nn], in0=u2[:, 0:nn], scalar1=0.044715, scalar2=1.0, op0=ALU.mult, op1=ALU.add),
                          reads=[u2.b], writes=[u2.b])
                        S("dve", lambda e, nn=nn: e.tensor_tensor(out=u2[:, 0:nn], in0=u2[:, 0:nn], in1=u[:, 0:nn], op=ALU.mult), reads=[u.b, u2.b], writes=[u2.b])
                        S("act", lambda e, nn=nn: e.activation(out=u2[:, 0:nn], in_=u2[:, 0:nn], func=AF.Sigmoid, scale=1.5957691216057308),
                          reads=[u2.b], writes=[u2.b])
                        S("dve", lambda e, jc=jc, n0=n0, nn=nn: e.tensor_tensor(out=gl[jc][:, n0:n0 + nn], in0=u2[:, 0:nn], in1=u[:, 0:nn], op=ALU.mult),
                          reads=[u.b, u2.b], writes=[gl[jc].b])
                if kv == 0:
                    for (n0, nn) in nrs:
                        for jc in range(2):
                            S("pe", lambda e, jc=jc, n0=n0, nn=nn: e.matmul(ops[:, 0:nn], w2b[:, jc, :], gl[jc][:, n0:n0 + nn], start=(jc == 0), stop=(jc == 1)),
                              reads=[w2b.b, gl[jc].b], writes=[ops.b], pe_acc=True)
                        S("act", lambda e, gi=gi, n0=n0, nn=nn: e.copy(g.kcmpT[gi][:, n0:n0 + nn], ops[:, 0:nn]), reads=[ops.b], writes=[g.kcmpT[gi].b])
                else:
                    for ncx in range(NCH):
                        kk = min(128, NCV - ncx * 128)
                        for jc in range(2):
                            S("pe", lambda e, jc=jc, ncx=ncx, kk=kk: e.matmul(ops[0:kk, 0:128], gl[jc][:, ncx * 128:ncx * 128 + kk], w2b[:, jc, :],
                                                                            start=(jc == 0), stop=(jc == 1)),
                              reads=[w2b.b, gl[jc].b], writes=[ops.b], pe_acc=True)
                        S("act", lambda e, gi=gi, ncx=ncx, kk=kk: e.copy(g.vcmp[gi][0:kk, ncx, :], ops[0:kk, 0:128]), reads=[ops.b], writes=[g.vcmp[gi].b])
        barrier(g)
    if "KCMP" in g.taps:
        g.KCMP = g.dr("KCMP", [2, 128, NCH * 128], BF16)
        g.VCMP = g.dr("VCMP", [2, 128, NCH, 128], BF16)
        g.tapdr += [g.KCMP, g.VCMP]
        for gi in range(2):
            S("sp", lambda e, gi=gi: e.dma_start(out=g.KCMP.ap[gi], in_=g.kcmpT[gi][:]), reads=[g.kcmpT[gi].b], writes=[g.KCMP.buf(gi)], dma=g.uniq())
            S("sp", lambda e, gi=gi: e.dma_start(out=g.VCMP.ap[gi], in_=g.vcmp[gi][:]), reads=[g.vcmp[gi].b], writes=[g.VCMP.buf(gi)], dma=g.uniq())


def phase3(g):
    nc, sch, st = g.nc, g.sch, g.st
    S = sch.issue
    SV, NKB, NQT, NQ = g.SV, g.NKB, g.NQT, g.NQ
    g.OAT = g.dr("OAT", [8, 128, NQ], BF16)
    if "OAT" in g.taps:
        g.tapdr.append(g.OAT)
    cb, cf = g.cb, g.cf
    with contextlib.ExitStack() as ls:
        kT = T(ls, nc, "sb_kT", [128, 2, SV], BF16)
        vv = T(ls, nc, "sb_v", [128, 2, NKB, 128], BF16)
        qT = T(ls, nc, "sb_qT", [128, 2, NQ], BF16)
        ex = [T(ls, nc, "sb_e%d" % i, [128, 2, 512], F32) for i in range(2)]
        sp = [T(ls, nc, "sb_sp%d" % i, [128, 2, 512], BF16) for i in range(2)]
        t1 = [T(ls, nc, "sb_t%d" % i, [128, 2, 512], F32) for i in range(2)]
        aa = [T(ls, nc, "sb_a%d" % i, [128, 2, 512], BF16) for i in range(2)]
        osb = [T(ls, nc, "sb_o%d" % i, [128, 2, 512], BF16) for i in range(2)]
        zps = [[T(ls, nc, "sb_z%d_%d" % (i, h), [128, 512], F32, psum=True) for h in range(2)] for i in range(2)]
        aps = [T(ls, nc, "sb_ap%d" % h, [128, 512], F32, psum=True) for h in range(2)]
        ops = [T(ls, nc, "sb_op%d" % h, [128, 512], F32, psum=True) for h in range(2)]
        negU = cb[:, CB_NEGU:CB_NEGU + 128]
        negL = cb[:, 3584:3584 + 128]
        ng = 0
        for hp in range(4):
            S("sp", lambda e, hp=hp: e.dma_start(out=kT[:], in_=g.KAT.ap[2 * hp:2 * hp + 2].rearrange("h p t -> p h t")),
              reads=g.KAT.allbufs(), writes=[kT.b], dma="sbk")
            S("sp", lambda e, hp=hp: e.dma_start(out=vv[:], in_=g.VA.ap[2 * hp:2 * hp + 2].rearrange("h p t d -> p h t d")),
              reads=g.VA.allbufs(), writes=[vv.b], dma="sbv")
            S("sp", lambda e, hp=hp: e.dma_start(out=qT[:], in_=g.QAT.ap[2 * hp:2 * hp + 2].rearrange("h p t -> p h t")),
              reads=g.QAT.allbufs(), writes=[qT.b], dma="sbq")
            for k in range(NQT):
                tv = 2 * k + 1
                jts = list(range(4 * tv + 3, -1, -1))
                nj = len(jts)
                base = ng
                ng += nj

                def bufs(ji):
                    i2 = (base + ji) % 2
                    return ex[i2], sp[i2], t1[i2], aa[i2], zps[i2]

                def kbias(jt):
                    return cf[:, CF_KB0 + jt:CF_KB0 + jt + 1] if jt < 4 else cf[:, CF_ZERO:CF_ZERO + 1]

                def stage_a(ji):
                    jt = jts[ji]
                    e_, sp_, t_, a_, z_ = bufs(ji)
                    dg = jt - 4 * tv
                    kb = kbias(jt)
                    for h in range(2):
                        S("pe", lambda e, h=h: e.matmul(z_[h][:, :], kT[:, h, jt * 128:(jt + 1) * 128], qT[:, h, k * 512:(k + 1) * 512], start=True, stop=True),
                          reads=[kT.b, qT.b], writes=[z_[h].b])
                    for h in range(2):
                        S("act", lambda e, h=h: e.activation(out=e_[:, h, :], in_=z_[h][:, :], func=AF.Exp, scale=SCALE, bias=kb),
                          reads=[z_[h].b, cf.b], writes=[e_.b])
                    S("act", lambda e: e.activation(out=sp_[:], in_=e_[:], func=AF.Ln, bias=cf[:, CF_ONE:CF_ONE + 1]),
                      reads=[e_.b, cf.b], writes=[sp_.b])
                    if dg >= 0:
                        for h in range(2):
                            S("pool", lambda e, h=h: e.tensor_tensor(out=sp_[:, h, :], in0=sp_[:, h, :], in1=cb[:, CB_CAUS + dg * 512:CB_CAUS + (dg + 1) * 512], op=ALU.mult),
                              reads=[sp_.b, cb.b], writes=[sp_.b])

                def stage_b(ji):
                    jt = jts[ji]
                    e_, sp_, t_, a_, z_ = bufs(ji)
                    first, last = (ji == 0), (ji == nj - 1)
                    dg = jt - 4 * tv
                    kb = kbias(jt)
                    for h in range(2):
                        S("pe", lambda e, h=h: e.matmul(aps[h][:, :], negU, sp_[:, h, :], start=first, stop=last),
                          reads=[cb.b, sp_.b], writes=[aps[h].b], pe_acc=True)
                    for h in range(2):
                        S("dve", lambda e, h=h: e.scalar_tensor_tensor(out=t_[:, h, :], in0=z_[h][:, :], scalar=SCALE, in1=sp_[:, h, :], op0=ALU.mult, op1=ALU.subtract),
                          reads=[z_[h].b, sp_.b], writes=[t_.b])
                        S("dve", lambda e, h=h: e.tensor_tensor(out=t_[:, h, :], in0=aps[h][:, :], in1=t_[:, h, :], op=ALU.add),
                          reads=[aps[h].b, t_.b], writes=[t_.b])
                    if not last:
                        for h in range(2):
                            S("pe", lambda e, h=h: e.matmul(aps[h][:, :], negL, sp_[:, h, :], start=False, stop=False),
                              reads=[cb.b, sp_.b], writes=[aps[h].b], pe_acc=True)
                    S("act", lambda e: e.activation(out=a_[:], in_=t_[:], func=AF.Exp, bias=kb), reads=[t_.b, cf.b], writes=[a_.b])
                    if dg >= 0:
                        for h in range(2):
                            S("pool", lambda e, h=h: e.tensor_tensor(out=a_[:, h, :], in0=a_[:, h, :], in1=cb[:, CB_CAUS + dg * 512:CB_CAUS + (dg + 1) * 512], op=ALU.mult),
                              reads=[a_.b, cb.b], writes=[a_.b])
                    for h in range(2):
                        S("pe", lambda e, h=h: e.matmul(ops[h][:, :], vv[:, h, jt, :], a_[:, h, :], start=first, stop=last),
                          reads=[vv.b, a_.b], writes=[ops[h].b], pe_acc=not first)

                stage_a(0)
                for ji in range(nj):
                    if ji + 1 < nj:
                        stage_a(ji + 1)
                    stage_b(ji)
                o_ = osb[(hp * NQT + k) % 2]
                S("act", lambda e, o_=o_: e.copy(o_[:, 0, :], ops[0][:, :]), reads=[ops[0].b], writes=[o_.b])
                S("dve", lambda e, o_=o_: e.tensor_copy(o_[:, 1, :], ops[1][:, :]), reads=[ops[1].b], writes=[o_.b])
                S("sp", lambda e, o_=o_, hp=hp, k=k: e.dma_start(out=g.OAT.ap[2 * hp:2 * hp + 2, :, k * 512:(k + 1) * 512].rearrange("h p t -> p h t"), in_=o_[:]),
                  reads=[o_.b], writes=[g.OAT.buf((hp, k))], dma="sbo")
        barrier(g)


CB_WM4, CB_ONESROW, CB_CPADROW, CB_WI, CB_ONESCOL, CB_ONES = 2304, 2816, 2944, 2976, 3264, 3328
CF_PADNEG, CF_F0 = 16, 176
OH_S0, OH_S1, OH_BC, OH_W = 0, 33 * 128, 33 * 128 + 32 * 128, 33 * 128 + 32 * 128 + 33 * 17


def phase4(g):
    nc, sch, st = g.nc, g.sch, g.st
    S = sch.issue
    SV, NKB, NQT, NQ, NQB, NCV, NSV, NCH = g.SV, g.NKB, g.NQT, g.NQ, g.NQB, g.NCV, g.NSV, g.NCH
    NCVP = NSV * 4
    KA = min(128, NSV)
    KB = NSV - KA
    g.OBT = g.dr("OBT", [8, 128, NQ], BF16)
    if "OBT" in g.taps:
        g.tapdr.append(g.OBT)
    cb, cf, idb = g.cb, g.cf, g.idb
    with contextlib.ExitStack() as ls:
        BS0 = T(ls, nc, "BS0", [128, 8, 128], BF16)
        BS1 = T(ls, nc, "BS1", [128, 8, 128], BF16)
        BCq = T(ls, nc, "BCq", [128, 8, 17], BF16)
        BCT = T(ls, nc, "BCT", [17, 8, 128], BF16)
        bmax = T(ls, nc, "bmax", [128, 1], F32)
        exp_c = T(ls, nc, "expc", [128, SV], BF16)
        S("sp", lambda e: e.dma_start(out=exp_c[:], in_=g.inp["c_exp"].ap), writes=[exp_c.b], dma=g.uniq())
        with contextlib.ExitStack() as l2:
            oh = T(l2, nc, "oh", [128, OH_W], BF16)
            tv = T(l2, nc, "tv", [128, 256], F32)
            td = T(l2, nc, "td", [128, 256], F32)
            acc0 = T(l2, nc, "acc0", [128, 128], F32)
            acc1 = T(l2, nc, "acc1", [128, 128], F32)
            acc2 = T(l2, nc, "acc2", [128, 17], F32)
            tps = T(l2, nc, "bctps", [128, 128], F32, psum=True)
            S("sp", lambda e: e.dma_start(out=oh[:], in_=g.inp["c_oh"].ap), writes=[oh.b], dma=g.uniq())
            S("sp", lambda e: e.dma_start(out=tv[:], in_=g.inp["relb"].ap.rearrange("(o b) h -> o (b h)", o=1).partition_broadcast(128)),
              writes=[tv.b], dma=g.uniq())
            tv3 = tv[:].rearrange("p (b h) -> p b h", h=8)
            td3 = td[:].rearrange("p (b h) -> p b h", h=8)
            for h in range(8):
                S("dve", lambda e, h=h: e.tensor_scalar(out=td3[:, :, h], in0=tv3[:, :, h], scalar1=tv[:, 248 + h:249 + h], scalar2=None, op0=ALU.subtract),
                  reads=[tv.b], writes=[td.b])
            S("dve", lambda e: e.tensor_reduce(out=bmax[:], in_=td[:], axis=AX.X, op=ALU.max, apply_absolute_value=True), reads=[td.b], writes=[bmax.b])
            S("dve", lambda e: e.tensor_scalar(out=td[:], in0=td[:], scalar1=1.0 / SCALE, scalar2=None, op0=ALU.mult), reads=[td.b], writes=[td.b])
            ohs0 = oh[:, OH_S0:OH_S0 + 33 * 128].rearrange("p (b q) -> p b q", q=128)
            ohs1 = oh[:, OH_S1:OH_S1 + 32 * 128].rearrange("p (b q) -> p b q", q=128)
            ohbc = oh[:, OH_BC:OH_BC + 33 * 17].rearrange("p (b q) -> p b q", q=17)
            for h in range(8):
                S("dve", lambda e: e.tensor_scalar(out=acc0[:], in0=ohs0[:, 32, :], scalar1=NEG, scalar2=None, op0=ALU.mult), reads=[oh.b], writes=[acc0.b])
                S("pool", lambda e: e.tensor_scalar(out=acc2[:], in0=ohbc[:, 32, :], scalar1=NEG, scalar2=None, op0=ALU.mult), reads=[oh.b], writes=[acc2.b])
                for b in range(31):
                    sc_ = td[:, b * 8 + h:b * 8 + h + 1]
                    S("dve", lambda e, b=b, sc_=sc_: e.scalar_tensor_tensor(out=acc0[:], in0=ohs0[:, b, :], scalar=sc_, in1=acc0[:], op0=ALU.mult, op1=ALU.add),
                      reads=[oh.b, td.b, acc0.b], writes=[acc0.b])
                    if b == 0:
                        S("dve", lambda e, b=b, sc_=sc_: e.tensor_scalar(out=acc1[:], in0=ohs1[:, b, :], scalar1=sc_, scalar2=None, op0=ALU.mult),
                          reads=[oh.b, td.b], writes=[acc1.b])
                    else:
                        S("dve", lambda e, b=b, sc_=sc_: e.scalar_tensor_tensor(out=acc1[:], in0=ohs1[:, b, :], scalar=sc_, in1=acc1[:], op0=ALU.mult, op1=ALU.add),
                          reads=[oh.b, td.b, acc1.b], writes=[acc1.b])
                    S("dve", lambda e, b=b, sc_=sc_: e.scalar_tensor_tensor(out=acc2[:], in0=ohbc[:, b, :], scalar=sc_, in1=acc2[:], op0=ALU.mult, op1=ALU.add),
                      reads=[oh.b, td.b, acc2.b], writes=[acc2.b])
                S("act", lambda e, h=h: e.copy(BS0[:, h, :], acc0[:]), reads=[acc0.b], writes=[BS0.b])
                S("act", lambda e, h=h: e.copy(BS1[:, h, :], acc1[:]), reads=[acc1.b], writes=[BS1.b])
                S("act", lambda e, h=h: e.copy(BCq[:, h, :], acc2[:]), reads=[acc2.b], writes=[BCq.b])
                S("pe", lambda e: e.transpose(tps[0:17, :], acc2[:], g.idf[:]), reads=[acc2.b, g.idf.b], writes=[tps.b])
                S("act", lambda e, h=h: e.copy(BCT[:, h, :], tps[0:17, :]), reads=[tps.b], writes=[BCT.b])
            barrier(g)
        kslT = T(ls, nc, "kslT", [128, SV], BF16)
        vsl = T(ls, nc, "vsl", [128, NKB, 128], BF16)
        kswT = T(ls, nc, "kswT", [128, SV], BF16)
        vsw = T(ls, nc, "vsw", [128, NKB, 128], BF16)
        qT = T(ls, nc, "nqT", [128, 4, NQ], BF16)
        sq = T(ls, nc, "nsq", [128, 2048], BF16)
        mx = T(ls, nc, "nmx", [128, 4], F32)
        nmk = T(ls, nc, "nmk", [128, 8], F32)
        ec = T(ls, nc, "n_ec", [128, 4, NCVP], F32)
        zc = T(ls, nc, "n_zc", [128, 4], F32)
        pc = T(ls, nc, "n_pc", [128, NCVP], F32)
        imp = T(ls, nc, "n_imp", [128, NSV], F32)
        sc2 = T(ls, nc, "n_sc2", [128, NSV], F32)
        m8 = T(ls, nc, "n_m8", [128, 16], F32)
        nm = T(ls, nc, "n_nm", [128, NSV], BF16)
        nmTA = T(ls, nc, "n_nmTA", [128, 4, 128], BF16)
        nmTB = T(ls, nc, "n_nmTB", [8, 4, 128], BF16)
        pT = [T(ls, nc, "n_pT%d" % i, [128, 512], BF16) for i in range(2)]
        gn = T(ls, nc, "n_gn", [128, 24], F32)
        zz = T(ls, nc, "n_zz", [128, 12], F32)
        coef = T(ls, nc, "n_coef", [128, 12], F32)
        otmp = T(ls, nc, "n_otmp", [128, 128], F32)
        ob = T(ls, nc, "n_ob", [128, 4, 128], BF16)
        obT = [T(ls, nc, "n_obT%d" % i, [128, 4, 128], BF16) for i in range(2)]
        scq = T(ls, nc, "n_scq", [128, 512], F32, psum=True)
        trp = T(ls, nc, "n_trp", [128, 1024], BF16, psum=True)
        stp = [T(ls, nc, "n_st%d" % i, [128, 512], F32, psum=True) for i in range(2)]
        opb = [T(ls, nc, "n_o%d" % i, [128, 512], F32, psum=True) for i in range(3)]
        zpb = T(ls, nc, "n_z", [128, 512], F32, psum=True)
        ones_m = cb[:, CB_ONES:CB_ONES + 128]
        onescol = cb[:, CB_ONESCOL:CB_ONESCOL + 1]
        nst = [0]

        def maxnorm(src_fn, total, col, srcbuf):
            for c0 in range(0, total, 512):
                w = min(512, total - c0)
                S("dve", lambda e, c0=c0, w=w: e.tensor_tensor(out=sq[:, 0:w], in0=src_fn(c0, w), in1=src_fn(c0, w), op=ALU.mult), reads=[srcbuf], writes=[sq.b])
                S("pe", lambda e, w=w: e.matmul(scq[:, 0:w], ones_m, sq[:, 0:w], start=True, stop=True), reads=[cb.b, sq.b], writes=[scq.b])
                S("dve", lambda e, w=w: e.tensor_reduce(out=mx[:, 2:3], in_=scq[:, 0:w], axis=AX.X, op=ALU.max), reads=[scq.b], writes=[mx.b])
                S("dve", lambda e, col=col: e.tensor_tensor(out=mx[:, col:col + 1], in0=mx[:, col:col + 1], in1=mx[:, 2:3], op=ALU.max), reads=[mx.b], writes=[mx.b])

        for gi in range(2):
            S("sp", lambda e, gi=gi: e.dma_start(out=kslT[:], in_=g.KSLT.ap[gi]), reads=g.KSLT.allbufs(), writes=[kslT.b], dma="n_k1")
            S("sp", lambda e, gi=gi: e.dma_start(out=vsl[:], in_=g.VSL.ap[gi]), reads=g.VSL.allbufs(), writes=[vsl.b], dma="n_v1")
            S("sp", lambda e, gi=gi: e.dma_start(out=kswT[:], in_=g.KSWT.ap[gi]), reads=g.KSWT.allbufs(), writes=[kswT.b], dma="n_k2")
            S("sp", lambda e, gi=gi: e.dma_start(out=vsw[:], in_=g.VSW.ap[gi]), reads=g.VSW.allbufs(), writes=[vsw.b], dma="n_v2")
            S("sp", lambda e, gi=gi: e.dma_start(out=qT[:], in_=g.QBT.ap[4 * gi:4 * gi + 4].rearrange("h p t -> p h t")),
              reads=g.QBT.allbufs(), writes=[qT.b], dma="n_q")
            S("dve", lambda e: e.memset(mx[:], 0.0), writes=[mx.b])
            S("dve", lambda e: e.memset(pc[:], 0.0), writes=[pc.b])
            for h in range(4):
                maxnorm(lambda c0, w, h=h: qT[:, h, c0:c0 + w], NQ, 0, qT.b)
            maxnorm(lambda c0, w: kslT[:, c0:c0 + w], SV, 1, kslT.b)
            maxnorm(lambda c0, w: kswT[:, c0:c0 + w], SV, 1, kswT.b)
            maxnorm(lambda c0, w, gi=gi: g.kcmpT[gi][:, c0:c0 + w], NCV, 1, g.kcmpT[gi].b)
            S("dve", lambda e: e.tensor_tensor(out=mx[:, 2:3], in0=mx[:, 0:1], in1=mx[:, 1:2], op=ALU.mult), reads=[mx.b], writes=[mx.b])
            S("act", lambda e: e.activation(out=mx[:, 2:3], in_=mx[:, 2:3], func=AF.Sqrt), reads=[mx.b], writes=[mx.b])
            S("dve", lambda e: e.scalar_tensor_tensor(out=mx[:, 3:4], in0=mx[:, 2:3], scalar=-SCALE * 1.02, in1=bmax[:], op0=ALU.mult, op1=ALU.subtract),
              reads=[mx.b, bmax.b], writes=[mx.b])
            for j in range(4):
                S("dve", lambda e, j=j: e.tensor_tensor(out=nmk[:, j:j + 1], in0=mx[:, 3:4], in1=cf[:, CF_KB0 + j:CF_KB0 + j + 1], op=ALU.add),
                  reads=[mx.b, cf.b], writes=[nmk.b])
            S("dve", lambda e: e.tensor_copy(nmk[:, 4:5], mx[:, 3:4]), reads=[mx.b], writes=[nmk.b])
            S("dve", lambda e: e.tensor_tensor(out=nmk[:, 5:6], in0=mx[:, 3:4], in1=cf[:, CF_CPAD:CF_CPAD + 1], op=ALU.add), reads=[mx.b, cf.b], writes=[nmk.b])
            negM = nmk[:, 4:5]

            for k in range(NQT):
                for c in range(4):
                    iv = 4 * (2 * k + 1) + c
                    lb = 4 * k + c
                    q4 = qT[:, :, lb * 128:(lb + 1) * 128]
                    ncols = min(8 * iv + 7, NCV)
                    n0 = 8 * iv - 10
                    S("sp", lambda e, lb=lb: e.dma_start(out=gn[:], in_=g.GN.ap[lb]), reads=g.GN.allbufs(), writes=[gn.b], dma="n_gn")
                    for h in range(4):
                        S("pe", lambda e, h=h, lb=lb, ncols=ncols: e.matmul(scq[:, 0:ncols], qT[:, h, lb * 128:(lb + 1) * 128], g.kcmpT[gi][:, 0:ncols], start=True, stop=False),
                          reads=[qT.b, g.kcmpT[gi].b], writes=[scq.b])
                        S("pe", lambda e, h=h, n0=n0: e.matmul(scq[:, n0:n0 + 17], idb[:], BCq[:, 4 * gi + h, :], start=False, stop=False),
                          reads=[idb.b, BCq.b], writes=[scq.b], pe_acc=True)
                        S("pe", lambda e: e.matmul(scq[:, 0:32], cb[0:1, CB_ONESROW:CB_ONESROW + 128], cb[0:1, CB_CPADROW:CB_CPADROW + 32], start=False, stop=True),
                          reads=[cb.b], writes=[scq.b], pe_acc=True)
                        S("act", lambda e, h=h, ncols=ncols: e.activation(out=ec[:, h, 0:ncols], in_=scq[:, 0:ncols], func=AF.Exp, scale=SCALE, bias=negM, accum_out=zc[:, h:h + 1]),
                          reads=[scq.b, nmk.b], writes=[ec.b, zc.b])
                    S("dve", lambda e: e.tensor_scalar(out=zc[:], in0=zc[:], scalar1=1e-30, scalar2=None, op0=ALU.max), reads=[zc.b], writes=[zc.b])
                    S("dve", lambda e: e.reciprocal(out=zc[:], in_=zc[:]), reads=[zc.b], writes=[zc.b])
                    S("dve", lambda e, ncols=ncols: e.tensor_scalar(out=pc[:, 0:ncols], in0=ec[:, 0, 0:ncols], scalar1=zc[:, 0:1], scalar2=None, op0=ALU.mult),
                      reads=[ec.b, zc.b], writes=[pc.b])
                    for h in range(1, 4):
                        S("dve", lambda e, h=h, ncols=ncols: e.scalar_tensor_tensor(out=pc[:, 0:ncols], in0=ec[:, h, 0:ncols], scalar=zc[:, h:h + 1], in1=pc[:, 0:ncols],
                                                                                 op0=ALU.mult, op1=ALU.add),
                          reads=[ec.b, zc.b, pc.b], writes=[pc.b])
                    pcv = pc[:].rearrange("p (j f) -> p j f", f=4)
                    S("dve", lambda e: e.tensor_reduce(out=imp[:], in_=pcv, axis=AX.X, op=ALU.add), reads=[pc.b], writes=[imp.b])
                    S("dve", lambda e: e.scalar_tensor_tensor(out=imp[:], in0=pcv[:, :, 3], scalar=-0.5, in1=imp[:], op0=ALU.mult, op1=ALU.add),
                      reads=[pc.b, imp.b], writes=[imp.b])
                    S("dve", lambda e: e.scalar_tensor_tensor(out=imp[:, 1:NSV], in0=pcv[:, 0:NSV - 1, 3], scalar=0.5, in1=imp[:, 1:NSV], op0=ALU.mult, op1=ALU.add),
                      reads=[pc.b, imp.b], writes=[imp.b])
                    S("dve", lambda e: e.tensor_tensor(out=imp[:], in0=imp[:], in1=cf[:, CF_PADNEG:CF_PADNEG + NSV], op=ALU.add), reads=[imp.b, cf.b], writes=[imp.b])
                    S("dve", lambda e: e.tensor_tensor(out=imp[:], in0=imp[:], in1=cf[:, CF_F0:CF_F0 + NSV], op=ALU.max), reads=[imp.b, cf.b], writes=[imp.b])
                    if 2 * iv + 2 < NSV:
                        S("dve", lambda e, iv=iv: e.memset(imp[:, 2 * iv + 2:NSV], -1e30), writes=[imp.b])
                    S("dve", lambda e, iv=iv: e.memset(imp[0:64, 2 * iv + 1:2 * iv + 2], -1e30), writes=[imp.b])
                    S("dve", lambda e, iv=iv: e.memset(imp[64:128, 2 * iv + 1:2 * iv + 2], 1e4), writes=[imp.b])
                    S("dve", lambda e, iv=iv: e.memset(imp[:, 2 * iv:2 * iv + 1], 1e4), writes=[imp.b])
                    S("dve", lambda e, iv=iv: e.memset(imp[0:64, 2 * iv - 1:2 * iv], 1e4), writes=[imp.b])
                    S("dve", lambda e: e.max(out=m8[:, 0:8], in_=imp[:]), reads=[imp.b], writes=[m8.b])
                    S("dve", lambda e: e.match_replace(out=sc2[:], in_to_replace=m8[:, 0:8], in_values=imp[:], imm_value=-3e38), reads=[imp.b, m8.b], writes=[sc2.b])
                    S("dve", lambda e: e.max(out=m8[:, 8:16], in_=sc2[:]), reads=[sc2.b], writes=[m8.b])
                    S("dve", lambda e: e.tensor_scalar(out=nm[:], in0=imp[:], scalar1=m8[:, 15:16], scalar2=NEG, op0=ALU.is_lt, op1=ALU.mult),
                      reads=[imp.b, m8.b], writes=[nm.b])
                    trv = trp[:, 0:512].bitcast(F32) if False else None
                    S("pe", lambda e: e.matmul(scq[0:KA, 0:128], nm[:, 0:KA], idb[:], start=True, stop=True), reads=[nm.b, idb.b], writes=[scq.b])
                    for h in range(4):
                        eng = "act" if h % 2 == 0 else "dve"
                        if eng == "act":
                            S("act", lambda e, h=h: e.copy(nmTA[0:KA, h, :], scq[0:KA, 0:128]), reads=[scq.b], writes=[nmTA.b])
                        else:
                            S("dve", lambda e, h=h: e.tensor_copy(nmTA[0:KA, h, :], scq[0:KA, 0:128]), reads=[scq.b], writes=[nmTA.b])
                    if KB > 0:
                        S("pe", lambda e: e.matmul(scq[0:KB, 128:256], nm[:, KA:KA + KB], idb[:], start=True, stop=True), reads=[nm.b, idb.b], writes=[scq.b])
                        for h in range(4):
                            S("dve", lambda e, h=h: e.tensor_copy(nmTB[0:KB, h, :], scq[0:KB, 128:256]), reads=[scq.b], writes=[nmTB.b])

                    tl = []

                    def run_tile(br, first, kk, mms, bias_ap, vrhs, vbuf):
                        tl.append((br, first, kk, mms, bias_ap, vrhs, vbuf, nst[0] % 2))
                        nst[0] += 1

                    def tile_a(t):
                        br, first, kk, mms, bias_ap, vrhs, vbuf, bi = t
                        st_, p_ = stp[bi], pT[bi]
                        for mi, (lhsT, rhs, rbufs) in enumerate(mms):
                            S("pe", lambda e, lhsT=lhsT, rhs=rhs, mi=mi: e.matmul(st_[0:kk, :], lhsT, rhs, start=(mi == 0), stop=(mi == len(mms) - 1)),
                              reads=rbufs, writes=[st_.b], pe_acc=(mi > 0))
                        S("act", lambda e: e.activation(out=p_[0:kk, :], in_=st_[0:kk, :], func=AF.Exp, scale=SCALE, bias=bias_ap),
                          reads=[st_.b, nmk.b], writes=[p_.b])

                    def tile_b(t):
                        br, first, kk, mms, bias_ap, vrhs, vbuf, bi = t
                        p_ = pT[bi]
                        for h in range(4):
                            S("pe", lambda e, h=h: e.matmul(opb[br][:, h * 128:(h + 1) * 128], p_[0:kk, h * 128:(h + 1) * 128], vrhs, start=(first and h == 0), stop=False),
                              reads=[p_.b, vbuf], writes=[opb[br].b], pe_acc=not (first and h == 0))
                        for h in range(4):
                            S("pe", lambda e, h=h: e.matmul(zpb[:, br * 4 + h:br * 4 + h + 1], p_[0:kk, h * 128:(h + 1) * 128], cb[0:kk, CB_ONESCOL:CB_ONESCOL + 1],
                                                          start=(first and h == 0 and br == 0), stop=False),
                              reads=[p_.b, cb.b], writes=[zpb.b], pe_acc=not (first and h == 0 and br == 0))

                    nchunks = (ncols + 127) // 128
                    for ncx in range(nchunks):
                        kk = min(128, ncols - 128 * ncx)
                        mms = [(g.kcmpT[gi][:, ncx * 128:ncx * 128 + kk], q4, [g.kcmpT[gi].b, qT.b])]
                        dlt = n0 - 128 * ncx
                        if dlt > -17 and dlt < kk:
                            mms.append((cb[0:17, CB_WI + 128 - dlt:CB_WI + 128 - dlt + kk], BCT[:, 4 * gi:4 * gi + 4, :], [cb.b, BCT.b]))
                        run_tile(0, ncx == 0, kk, mms, nmk[0:kk, 5:6] if ncx == 0 else nmk[0:kk, 4:5], g.vcmp[gi][0:kk, ncx, :], g.vcmp[gi].b)
                    for jt in range(iv + 1):
                        mms = [(kslT[:, jt * 128:(jt + 1) * 128], q4, [kslT.b, qT.b])]
                        if jt < 64 or KB == 0:
                            mms.append((exp_c[0:KA, jt * 128:(jt + 1) * 128], nmTA[0:KA, :, :], [exp_c.b, nmTA.b]))
                        else:
                            mms.append((exp_c[0:KB, jt * 128:(jt + 1) * 128], nmTB[0:KB, :, :], [exp_c.b, nmTB.b]))
                        if jt == iv:
                            mms.append((idb[:], BS0[:, 4 * gi:4 * gi + 4, :], [idb.b, BS0.b]))
                        elif jt == iv - 1:
                            mms.append((idb[:], BS1[:, 4 * gi:4 * gi + 4, :], [idb.b, BS1.b]))
                        run_tile(1, jt == 0, 128, mms, nmk[:, jt:jt + 1] if jt < 4 else nmk[:, 4:5], vsl[:, jt, :], vsl.b)
                    for jt in range(iv - 4, iv + 1):
                        mms = [(kswT[:, jt * 128:(jt + 1) * 128], q4, [kswT.b, qT.b])]
                        if jt == iv:
                            mms.append((idb[:], BS0[:, 4 * gi:4 * gi + 4, :], [idb.b, BS0.b]))
                        elif jt == iv - 1:
                            mms.append((idb[:], BS1[:, 4 * gi:4 * gi + 4, :], [idb.b, BS1.b]))
                        elif jt == iv - 4:
                            mms.append((idb[:], cb[:, CB_WM4:CB_WM4 + 512], [idb.b, cb.b]))
                        run_tile(2, jt == iv - 4, 128, mms, nmk[:, jt:jt + 1] if jt < 4 else nmk[:, 4:5], vsw[:, jt, :], vsw.b)
                    tile_a(tl[0])
                    for ti in range(len(tl)):
                        if ti + 1 < len(tl):
                            tile_a(tl[ti + 1])
                        tile_b(tl[ti])
                    S("dve", lambda e: e.tensor_scalar(out=zz[:], in0=zpb[:, 0:12], scalar1=1e-30, scalar2=None, op0=ALU.max), reads=[zpb.b], writes=[zz.b])
                    S("dve", lambda e: e.reciprocal(out=zz[:], in_=zz[:]), reads=[zz.b], writes=[zz.b])
                    S("dve", lambda e: e.tensor_tensor(out=coef[:].rearrange("p (b h) -> p b h", h=4), in0=zz[:].rearrange("p (b h) -> p b h", h=4),
                                                      in1=gn[:, gi * 12:(gi + 1) * 12].rearrange("p (h b) -> p b h", b=3), op=ALU.mult),
                      reads=[zz.b, gn.b], writes=[coef.b])
                    for h in range(4):
                        S("dve", lambda e, h=h: e.tensor_scalar(out=otmp[:], in0=opb[0][:, h * 128:(h + 1) * 128], scalar1=coef[:, h:h + 1], scalar2=None, op0=ALU.mult),
                          reads=[opb[0].b, coef.b], writes=[otmp.b])
                        S("dve", lambda e, h=h: e.scalar_tensor_tensor(out=otmp[:], in0=opb[1][:, h * 128:(h + 1) * 128], scalar=coef[:, 4 + h:5 + h], in1=otmp[:], op0=ALU.mult, op1=ALU.add),
                          reads=[opb[1].b, coef.b, otmp.b], writes=[otmp.b])
                        S("dve", lambda e, h=h: e.scalar_tensor_tensor(out=ob[:, h, :], in0=opb[2][:, h * 128:(h + 1) * 128], scalar=coef[:, 8 + h:9 + h], in1=otmp[:], op0=ALU.mult, op1=ALU.add),
                          reads=[opb[2].b, coef.b, otmp.b], writes=[ob.b])
                    for h in range(4):
                        S("pe", lambda e, h=h: e.transpose(trp[:, h * 128:(h + 1) * 128], ob[:, h, :], idb[:]), reads=[ob.b, idb.b], writes=[trp.b], pe_acc=(h > 0))
                    oT_ = obT[(gi * NQB + lb) % 2]
                    S("act", lambda e, oT_=oT_: e.copy(oT_[:].rearrange("p h q -> p (h q)"), trp[:, 0:512]), reads=[trp.b], writes=[oT_.b])
                    S("sp", lambda e, oT_=oT_, lb=lb: e.dma_start(out=g.OBT.ap[4 * gi:4 * gi + 4, :, lb * 128:(lb + 1) * 128].rearrange("h p t -> p h t"), in_=oT_[:]),
                      reads=[oT_.b], writes=[g.OBT.buf((gi, lb))], dma="n_o")
        barrier(g)


CF_EOFF = 336
CB_LTRI = 3456
CB_NEGL = 3584


def phase5(g):
    nc, sch, st = g.nc, g.sch, g.st
    S = sch.issue
    NQT, NQ, NQB, CAP = g.NQT, g.NQ, g.NQB, g.CAP
    cb, cf, idb, idf = g.cb, g.cf, g.idb, g.idf
    g.MT = g.dr("MT", [16, 128, NQ], BF16)
    g.X1 = g.dr("X1", [NQ, D], F32)
    g.XB = g.dr("XB", [NEXP * CAP, D], BF16)
    g.RT = g.dr("RT", [NQB, 128, 4], F32)
    for nm_ in ("MT", "X1", "RT"):
        if nm_ in g.taps:
            g.tapdr.append(getattr(g, nm_))
    g.breg = nc.gpsimd.to_reg(NEXP * CAP - 1)
    g.wts = T(st, nc, "wts", [128, NQB, 2], F32)
    g.dst = T(st, nc, "dst", [128, NQB, 2], I32)
    with contextlib.ExitStack() as ls:
        wab = T(ls, nc, "wab", [128, 8, D], BF16)
        wbb = T(ls, nc, "wbb", [128, 8, D], BF16)
        with contextlib.ExitStack() as l2:
            stg = [T(l2, nc, "wstg%d" % i, [128, 8, 512], F32) for i in range(2)]
            n = 0
            for (wd, wt) in ((g.inp["wba"].ap, wab), (g.inp["wbb"].ap, wbb)):
                for cs in range(4):
                    sg = stg[n % 2]
                    S("sp", lambda e, sg=sg, wd=wd, cs=cs: e.dma_start(out=sg[:], in_=wd[:, cs * 512:(cs + 1) * 512].rearrange("(c p) j -> p c j", p=128)),
                      writes=[sg.b], dma="wstg%d" % (n % 2))
                    S("act", lambda e, sg=sg, wt=wt, cs=cs: e.copy(wt[:, 0:4, cs * 512:(cs + 1) * 512], sg[:, 0:4, :]), reads=[sg.b], writes=[wt.b])
                    S("dve", lambda e, sg=sg, wt=wt, cs=cs: e.tensor_copy(wt[:, 4:8, cs * 512:(cs + 1) * 512], sg[:, 4:8, :]), reads=[sg.b], writes=[wt.b])
                    n += 1
            barrier(g)
        oa = [T(ls, nc, "m_oa%d" % i, [128, 8, 512], BF16) for i in range(2)]
        obt = [T(ls, nc, "m_ob%d" % i, [128, 8, 512], BF16) for i in range(2)]
        ga = [T(ls, nc, "m_ga%d" % i, [128, 4, 512], BF16) for i in range(2)]
        gb = [T(ls, nc, "m_gb%d" % i, [128, 4, 512], BF16) for i in range(2)]
        mo = [T(ls, nc, "m_mo%d" % i, [128, 4, 512], BF16) for i in range(2)]
        ta = [T(ls, nc, "m_ta%d" % i, [128, 512], F32) for i in range(2)]
        tb_ = [T(ls, nc, "m_tb%d" % i, [128, 512], F32) for i in range(2)]
        psA = [T(ls, nc, "m_pa%d" % i, [128, 512], F32, psum=True) for i in range(2)]
        psB = [T(ls, nc, "m_pb%d" % i, [128, 512], F32, psum=True) for i in range(2)]
        nq = 0
        for k in range(NQT):
            oa_, ob_ = oa[k % 2], obt[k % 2]
            S("sp", lambda e, oa_=oa_, k=k: e.dma_start(out=oa_[:], in_=g.OAT.ap[:, :, k * 512:(k + 1) * 512].rearrange("h p t -> p h t")),
              reads=g.OAT.allbufs(), writes=[oa_.b], dma="m_oa%d" % (k % 2))
            S("sp", lambda e, ob_=ob_, k=k: e.dma_start(out=ob_[:], in_=g.OBT.ap[:, :, k * 512:(k + 1) * 512].rearrange("h p t -> p h t")),
              reads=g.OBT.allbufs(), writes=[ob_.b], dma="m_ob%d" % (k % 2))
            for dq in range(4):
                ga_, gb_, mo_ = ga[nq % 2], gb[nq % 2], mo[nq % 2]
                S("sp", lambda e, ga_=ga_, k=k, dq=dq: e.dma_start(out=ga_[:], in_=g.GAT.ap[4 * dq:4 * dq + 4, :, k * 512:(k + 1) * 512].rearrange("c p t -> p c t")),
                  reads=g.GAT.allbufs(), writes=[ga_.b], dma="m_ga%d" % (nq % 2))
                S("sp", lambda e, gb_=gb_, k=k, dq=dq: e.dma_start(out=gb_[:], in_=g.GBT.ap[4 * dq:4 * dq + 4, :, k * 512:(k + 1) * 512].rearrange("c p t -> p c t")),
                  reads=g.GBT.allbufs(), writes=[gb_.b], dma="m_gb%d" % (nq % 2))
                nq += 1
                for dl in range(4):
                    dc = 4 * dq + dl
                    pa, pb = psA[dc % 2], psB[dc % 2]
                    ta_, tb2 = ta[dc % 2], tb_[dc % 2]
                    for hc in range(8):
                        S("pe", lambda e, pa=pa, hc=hc, dc=dc, oa_=oa_: e.matmul(pa[:, :], wab[:, hc, dc * 128:(dc + 1) * 128], oa_[:, hc, :], start=(hc == 0), stop=(hc == 7)),
                          reads=[wab.b, oa_.b], writes=[pa.b], pe_acc=True)
                    for hc in range(8):
                        S("pe", lambda e, pb=pb, hc=hc, dc=dc, ob_=ob_: e.matmul(pb[:, :], wbb[:, hc, dc * 128:(dc + 1) * 128], ob_[:, hc, :], start=(hc == 0), stop=(hc == 7)),
                          reads=[wbb.b, ob_.b], writes=[pb.b], pe_acc=True)
                    S("dve", lambda e, pa=pa, ta_=ta_, ga_=ga_, dl=dl: e.tensor_tensor(out=ta_[:], in0=pa[:, :], in1=ga_[:, dl, :], op=ALU.mult), reads=[pa.b, ga_.b], writes=[ta_.b])
                    S("dve", lambda e, pb=pb, tb2=tb2, gb_=gb_, dl=dl: e.tensor_tensor(out=tb2[:], in0=pb[:, :], in1=gb_[:, dl, :], op=ALU.mult), reads=[pb.b, gb_.b], writes=[tb2.b])
                    S("pool", lambda e, ta_=ta_, tb2=tb2, mo_=mo_, dl=dl: e.tensor_tensor(out=mo_[:, dl, :], in0=ta_[:], in1=tb2[:], op=ALU.add), reads=[ta_.b, tb2.b], writes=[mo_.b])
                S("sp", lambda e, mo_=mo_, k=k, dq=dq: e.dma_start(out=g.MT.ap[4 * dq:4 * dq + 4, :, k * 512:(k + 1) * 512].rearrange("c p t -> p c t"), in_=mo_[:]),
                  reads=[mo_.b], writes=[g.MT.buf((k, dq))], dma="m_st")
        barrier(g)
    with contextlib.ExitStack() as ls:
        wob = T(ls, nc, "wob", [128, 16, D], BF16)
        g.rows = {}
        for nm_ in ("gt1", "a2", "sh2"):
            g.rows[nm_] = T(ls, nc, "row_" + nm_, [128, D], F32)
        load_rows(g, ls, ("gt1", "a2", "sh2"))
        with contextlib.ExitStack() as l2:
            stg = [T(l2, nc, "wostg%d" % i, [128, 16, 512], F32) for i in range(2)]
            for cs in range(4):
                sg = stg[cs % 2]
                S("sp", lambda e, sg=sg, cs=cs: e.dma_start(out=sg[:], in_=g.inp["wout"].ap[:, cs * 512:(cs + 1) * 512].rearrange("(c p) j -> p c j", p=128)),
                  writes=[sg.b], dma="wostg%d" % (cs % 2))
                S("act", lambda e, sg=sg, cs=cs: e.copy(wob[:, 0:8, cs * 512:(cs + 1) * 512], sg[:, 0:8, :]), reads=[sg.b], writes=[wob.b])
                S("dve", lambda e, sg=sg, cs=cs: e.tensor_copy(wob[:, 8:16, cs * 512:(cs + 1) * 512], sg[:, 8:16, :]), reads=[sg.b], writes=[wob.b])
            barrier(g)
        wr = T(ls, nc, "wr", [128, 16, 72], F32)
        brr = T(ls, nc, "brr", [128, 72], F32)
        S("sp", lambda e: e.dma_start(out=wr[:], in_=g.inp["wr"].ap.rearrange("(c p) j -> p c j", p=128)), writes=[wr.b], dma=g.uniq())
        S("sp", lambda e: e.dma_start(out=brr[:], in_=g.inp["br"].ap.partition_broadcast(128)), writes=[brr.b], dma=g.uniq())
        mt = [T(ls, nc, "r_mt%d" % i, [128, 16, 128], BF16) for i in range(2)]
        xb = [T(ls, nc, "r_xb%d" % i, [128, D], F32) for i in range(2)]
        hn = T(ls, nc, "r_hn", [128, D], F32)
        hnb = [T(ls, nc, "r_hnb%d" % i, [128, D], BF16) for i in range(2)]
        hnT = T(ls, nc, "r_hnT", [128, 16, 128], F32)
        junk = T(ls, nc, "r_junk", [128, D], BF16)
        ss = T(ls, nc, "r_ss", [128, 1], F32)
        lg = T(ls, nc, "r_lg", [128, 72], F32)
        sm = T(ls, nc, "r_sm", [128, 16], F32)
        oh8 = T(ls, nc, "r_oh8", [128, 8], F32)
        lem = T(ls, nc, "r_lem", [128, 64], F32)
        top8 = T(ls, nc, "r_top8", [128, 8], F32)
        A1 = T(ls, nc, "r_A1", [128, 64], F32)
        A2 = T(ls, nc, "r_A2", [128, 64], F32)
        Ab = T(ls, nc, "r_Ab", [128, 64], BF16)
        Acum = T(ls, nc, "r_Acum", [128, 64], BF16)
        t64 = T(ls, nc, "r_t64", [128, 64], F32)
        j64 = T(ls, nc, "r_j64", [128, 64], F32)
        dsf = T(ls, nc, "r_dsf", [128, 8], F32)
        psy = [T(ls, nc, "r_py%d" % i, [128, 512], F32, psum=True) for i in range(4)]
        pst = [T(ls, nc, "r_pt%d" % i, [128, 512], F32, psum=True) for i in range(2)]
        psl = T(ls, nc, "r_pl", [128, 512], F32, psum=True)
        S("dve", lambda e: e.memset(Acum[:], 0.0), writes=[Acum.b])
        ltri = cb[:, CB_LTRI:CB_LTRI + 128]
        ones_m = cb[:, CB_ONES:CB_ONES + 128]
        xv = g.inp["xv"].ap
        for lb in range(NQB):
            k, c = lb // 4, lb % 4
            r0 = (2 * k + 1) * 512 + c * 128
            mt_, xb_, hnb_ = mt[lb % 2], xb[lb % 2], hnb[lb % 2]
            S("sp", lambda e, mt_=mt_, lb=lb: e.dma_start(out=mt_[:], in_=g.MT.ap[:, :, lb * 128:(lb + 1) * 128].rearrange("c p t -> p c t")),
              reads=g.MT.allbufs(), writes=[mt_.b], dma="r_mt%d" % (lb % 2))
            S("sp", lambda e, xb_=xb_, r0=r0: e.dma_start(out=xb_[:], in_=xv[r0:r0 + 128, :]), writes=[xb_.b], dma="r_xb%d" % (lb % 2))
            for oc in range(4):
                for dc in range(16):
                    S("pe", lambda e, oc=oc, dc=dc, mt_=mt_: e.matmul(psy[oc][:, :], mt_[:, dc, :], wob[:, dc, oc * 512:(oc + 1) * 512], start=(dc == 0), stop=(dc == 15)),
                      reads=[mt_.b, wob.b], writes=[psy[oc].b], pe_acc=True)
                S("dve", lambda e, oc=oc: e.tensor_tensor(out=hn[:, oc * 512:(oc + 1) * 512], in0=psy[oc][:, :], in1=g.rows["gt1"][:, oc * 512:(oc + 1) * 512], op=ALU.mult),
                  reads=[psy[oc].b, g.rows["gt1"].b], writes=[hn.b])
            S("pool", lambda e, xb_=xb_: e.tensor_tensor(out=xb_[:], in0=xb_[:], in1=hn[:], op=ALU.add), reads=[xb_.b, hn.b], writes=[xb_.b])
            S("sp", lambda e, xb_=xb_, lb=lb: e.dma_start(out=g.X1.ap[lb * 128:(lb + 1) * 128, :], in_=xb_[:]), reads=[xb_.b], writes=[g.X1.buf(lb)], dma="r_x1")
            S("act", lambda e, xb_=xb_: e.activation(out=junk[:], in_=xb_[:], func=AF.Square, accum_out=ss[:]), reads=[xb_.b], writes=[junk.b, ss.b])
            S("dve", lambda e: e.tensor_scalar(out=ss[:], in0=ss[:], scalar1=1.0 / D, scalar2=1e-6, op0=ALU.mult, op1=ALU.add), reads=[ss.b], writes=[ss.b])
            S("act", lambda e: e.activation(out=ss[:], in_=ss[:], func=AF.Sqrt), reads=[ss.b], writes=[ss.b])
            S("dve", lambda e: e.reciprocal(out=ss[:], in_=ss[:]), reads=[ss.b], writes=[ss.b])
            S("dve", lambda e, xb_=xb_: e.scalar_tensor_tensor(out=hn[:], in0=xb_[:], scalar=ss[:, 0:1], in1=g.rows["a2"][:], op0=ALU.mult, op1=ALU.mult),
              reads=[xb_.b, ss.b, g.rows["a2"].b], writes=[hn.b])
            S("pool", lambda e: e.tensor_tensor(out=hn[:], in0=hn[:], in1=g.rows["sh2"][:], op=ALU.add), reads=[hn.b, g.rows["sh2"].b], writes=[hn.b])
            S("act", lambda e, hnb_=hnb_: e.copy(hnb_[:], hn[:]), reads=[hn.b], writes=[hnb_.b])
            for q4 in range(4):
                pt = pst[q4 % 2]
                for j in range(4):
                    dc = q4 * 4 + j
                    S("pe", lambda e, pt=pt, j=j, dc=dc: e.transpose(pt[:, j * 128:(j + 1) * 128], hn[:, dc * 128:(dc + 1) * 128], idf[:]),
                      reads=[hn.b, idf.b], writes=[pt.b], pe_acc=(j > 0))
                if q4 % 2 == 0:
                    S("act", lambda e, pt=pt, q4=q4: e.copy(hnT[:, q4 * 4:(q4 + 1) * 4, :].rearrange("p a b -> p (a b)"), pt[:, :]), reads=[pt.b], writes=[hnT.b])
                else:
                    S("dve", lambda e, pt=pt, q4=q4: e.tensor_copy(hnT[:, q4 * 4:(q4 + 1) * 4, :].rearrange("p a b -> p (a b)"), pt[:, :]), reads=[pt.b], writes=[hnT.b])
            for dc in range(16):
                S("pe", lambda e, dc=dc: e.matmul(psl[:, 0:72], hnT[:, dc, :], wr[:, dc, :], start=(dc == 0), stop=(dc == 15)),
                  reads=[hnT.b, wr.b], writes=[psl.b], pe_acc=(dc > 0))
            S("dve", lambda e: e.tensor_tensor(out=lg[:], in0=psl[:, 0:72], in1=brr[:], op=ALU.add), reads=[psl.b, brr.b], writes=[lg.b])
            S("dve", lambda e: e.tensor_reduce(out=sm[:, 0:1], in_=lg[:, 0:8], axis=AX.X, op=ALU.max), reads=[lg.b], writes=[sm.b])
            S("dve", lambda e: e.tensor_scalar(out=sm[:, 1:2], in0=sm[:, 0:1], scalar1=-1.0, scalar2=None, op0=ALU.mult), reads=[sm.b], writes=[sm.b])
            S("dve", lambda e: e.tensor_scalar(out=oh8[:], in0=lg[:, 0:8], scalar1=sm[:, 0:1], scalar2=None, op0=ALU.is_equal), reads=[lg.b, sm.b], writes=[oh8.b])
            S("act", lambda e: e.activation(out=j64[:, 0:8], in_=lg[:, 0:8], func=AF.Exp, bias=sm[:, 1:2], accum_out=sm[:, 2:3]), reads=[lg.b, sm.b], writes=[j64.b, sm.b])
            S("dve", lambda e: e.tensor_scalar(out=oh8[:], in0=oh8[:], scalar1=-1.0, scalar2=1e30, op0=ALU.add, op1=ALU.mult), reads=[oh8.b], writes=[oh8.b])
            for gq in range(8):
                S("dve", lambda e, gq=gq: e.tensor_scalar(out=lem[:, gq * 8:(gq + 1) * 8], in0=lg[:, 8 + gq * 8:16 + gq * 8], scalar1=oh8[:, gq:gq + 1], scalar2=None, op0=ALU.add),
                  reads=[lg.b, oh8.b], writes=[lem.b])
            S("dve", lambda e: e.max(out=top8[:], in_=lem[:]), reads=[lem.b], writes=[top8.b])
            S("dve", lambda e: e.tensor_scalar(out=A1[:], in0=lem[:], scalar1=top8[:, 0:1], scalar2=None, op0=ALU.is_equal), reads=[lem.b, top8.b], writes=[A1.b])
            S("dve", lambda e: e.tensor_scalar(out=A2[:], in0=lem[:], scalar1=top8[:, 1:2], scalar2=None, op0=ALU.is_equal), reads=[lem.b, top8.b], writes=[A2.b])
            S("dve", lambda e: e.tensor_tensor(out=Ab[:], in0=A1[:], in1=A2[:], op=ALU.add), reads=[A1.b, A2.b], writes=[Ab.b])
            S("dve", lambda e: e.tensor_scalar(out=sm[:, 3:4], in0=top8[:, 0:1], scalar1=-1.0, scalar2=None, op0=ALU.mult), reads=[top8.b], writes=[sm.b])
            S("act", lambda e: e.activation(out=sm[:, 4:5], in_=top8[:, 1:2], func=AF.Exp, bias=sm[:, 3:4]), reads=[top8.b, sm.b], writes=[sm.b])
            S("dve", lambda e: e.scalar_tensor_tensor(out=sm[:, 5:6], in0=sm[:, 4:5], scalar=1.0, in1=sm[:, 2:3], op0=ALU.add, op1=ALU.mult), reads=[sm.b], writes=[sm.b])
            S("dve", lambda e: e.reciprocal(out=sm[:, 6:7], in_=sm[:, 5:6]), reads=[sm.b], writes=[sm.b])
            S("dve", lambda e: e.tensor_tensor(out=sm[:, 7:8], in0=sm[:, 6:7], in1=sm[:, 4:5], op=ALU.mult), reads=[sm.b], writes=[sm.b])
            S("pe", lambda e, lb=lb: e.matmul(psl[:, 128:192], ltri, Ab[:], start=True, stop=(lb == 0)), reads=[cb.b, Ab.b], writes=[psl.b])
            if lb > 0:
                S("pe", lambda e: e.matmul(psl[:, 128:192], ones_m, Acum[:], start=False, stop=True), reads=[cb.b, Acum.b], writes=[psl.b], pe_acc=True)
            S("dve", lambda e: e.tensor_tensor(out=t64[:], in0=psl[:, 128:192], in1=cf[:, CF_EOFF:CF_EOFF + 64], op=ALU.add), reads=[psl.b, cf.b], writes=[t64.b])
            for j, Aj in enumerate((A1, A2)):
                S("dve", lambda e, Aj=Aj, j=j: e.scalar_tensor_tensor(out=j64[:], in0=t64[:], scalar=1.0, in1=Aj[:], op0=ALU.mult, op1=ALU.mult, accum_out=dsf[:, j:j + 1]),
                  reads=[t64.b, Aj.b], writes=[j64.b, dsf.b])
                S("dve", lambda e, Aj=Aj, j=j: e.scalar_tensor_tensor(out=j64[:], in0=psl[:, 128:192], scalar=1.0, in1=Aj[:], op0=ALU.mult, op1=ALU.mult, accum_out=dsf[:, 2 + j:3 + j]),
                  reads=[psl.b, Aj.b], writes=[j64.b, dsf.b])
                S("dve", lambda e, j=j: e.tensor_scalar(out=dsf[:, 4 + j:5 + j], in0=dsf[:, 2 + j:3 + j], scalar1=CAP - 0.5, scalar2=1e9, op0=ALU.is_ge, op1=ALU.mult),
                  reads=[dsf.b], writes=[dsf.b])
                S("dve", lambda e, j=j: e.tensor_tensor(out=dsf[:, j:j + 1], in0=dsf[:, j:j + 1], in1=dsf[:, 4 + j:5 + j], op=ALU.add), reads=[dsf.b], writes=[dsf.b])
            S("dve", lambda e: e.tensor_tensor(out=Acum[:], in0=Acum[:], in1=Ab[:], op=ALU.add), reads=[Acum.b, Ab.b], writes=[Acum.b])
            S("dve", lambda e, lb=lb: e.tensor_copy(g.dst[:, lb, :], dsf[:, 0:2]), reads=[dsf.b], writes=[g.dst.b])
            S("dve", lambda e, lb=lb: e.tensor_copy(g.wts[:, lb, :], sm[:, 6:8]), reads=[sm.b], writes=[g.wts.b])
            for j in range(2):
                S("pool", lambda e, lb=lb, j=j, hnb_=hnb_: e.indirect_dma_start(
                    out=g.XB.ap, out_offset=bass.IndirectOffsetOnAxis(ap=g.dst[:, lb, j:j + 1], axis=0), in_=hnb_[:], in_offset=None,
                    bounds_check=g.breg, oob_is_err=False),
                  reads=[hnb_.b, g.dst.b], writes=[g.XB.buf()], dma="r_sc")
            if "RT" in g.taps:
                S("dve", lambda e: e.tensor_copy(dsf[:, 2:4], sm[:, 6:8]), reads=[sm.b, dsf.b], writes=[dsf.b])
                S("sp", lambda e, lb=lb: e.dma_start(out=g.RT.ap[lb], in_=dsf[:, 0:4]), reads=[dsf.b], writes=[g.RT.buf(lb)], dma="r_rt")
        barrier(g)


def phase6(g):
    nc, sch, st = g.nc, g.sch, g.st
    S = sch.issue
    CAP = g.CAP
    NSB = CAP // 128
    idb = g.idb
    g.YB = g.dr("YB", [NEXP * CAP, D], F32)
    ew1, ew3, ew2 = g.inp["ew1"].ap, g.inp["ew3"].ap, g.inp["ew2"].ap
    with contextlib.ExitStack() as ls:
        w1s = [T(ls, nc, "e_w1s%d" % i, [128, D], F32) for i in range(2)]
        w3s = [T(ls, nc, "e_w3s%d" % i, [128, D], F32) for i in range(2)]
        w2s = [T(ls, nc, "e_w2s%d" % i, [128, D], F32) for i in range(2)]
        w1b = [T(ls, nc, "e_w1b%d" % i, [128, 16, 128], BF16) for i in range(2)]
        w3b = [T(ls, nc, "e_w3b%d" % i, [128, 16, 128], BF16) for i in range(2)]
        w2b = T(ls, nc, "e_w2b", [128, NFC, D], BF16)
        xet = T(ls, nc, "e_xet", [128, 16, CAP], BF16)
        xr = [T(ls, nc, "e_xr%d" % i, [128, D], BF16) for i in range(2)]
        hid = T(ls, nc, "e_hid", [128, NFC, CAP], BF16)
        sl = T(ls, nc, "e_sl", [128, CAP], F32)
        yst = [T(ls, nc, "e_yst%d" % i, [128, D], F32) for i in range(2)]
        h1 = T(ls, nc, "e_h1", [128, 512], F32, psum=True)
        h3 = T(ls, nc, "e_h3", [128, 512], F32, psum=True)
        ptr = [T(ls, nc, "e_pt%d" % i, [128, 1024], BF16, psum=True) for i in range(2)]
        py = [T(ls, nc, "e_py%d" % i, [128, 512], F32, psum=True) for i in range(4)]
        steps = [(e_, fc) for e_ in range(NEXP) for fc in range(NFC)]

        def loads(i):
            e_, fc = steps[i]
            S("sp", lambda e: e.dma_start(out=w1s[i % 2][:], in_=ew1[e_, fc]), writes=[w1s[i % 2].b], dma="e_w1s%d" % (i % 2))
            S("sp", lambda e: e.dma_start(out=w3s[i % 2][:], in_=ew3[e_, fc]), writes=[w3s[i % 2].b], dma="e_w3s%d" % (i % 2))
            S("sp", lambda e: e.dma_start(out=w2s[i % 2][:], in_=ew2[e_, fc * 128:(fc + 1) * 128, :]), writes=[w2s[i % 2].b], dma="e_w2s%d" % (i % 2))

        loads(0)
        nx = 0
        ny = 0
        for i, (e_, fc) in enumerate(steps):
            if i + 1 < len(steps):
                loads(i + 1)
            if fc == 0:
                for sb in range(NSB):
                    xr_ = xr[nx % 2]
                    S("sp", lambda e, xr_=xr_, sb=sb: e.dma_start(out=xr_[:], in_=g.XB.ap[e_ * CAP + sb * 128:e_ * CAP + (sb + 1) * 128, :]),
                      reads=g.XB.allbufs(), writes=[xr_.b], dma="e_xr%d" % (nx % 2))
                    nx += 1
                    for hq in range(2):
                        pt = ptr[hq]
                        for j in range(8):
                            dc = hq * 8 + j
                            S("pe", lambda e, pt=pt, j=j, dc=dc, xr_=xr_: e.transpose(pt[:, j * 128:(j + 1) * 128], xr_[:, dc * 128:(dc + 1) * 128], idb[:]),
                              reads=[xr_.b, idb.b], writes=[pt.b], pe_acc=(j > 0))
                        if hq == 0:
                            S("act", lambda e, pt=pt, sb=sb: e.copy(xet[:, 0:8, sb * 128:(sb + 1) * 128], pt[:, :].rearrange("p (a b) -> p a b", b=128)),
                              reads=[pt.b], writes=[xet.b])
                        else:
                            S("dve", lambda e, pt=pt, sb=sb: e.tensor_copy(xet[:, 8:16, sb * 128:(sb + 1) * 128], pt[:, :].rearrange("p (a b) -> p a b", b=128)),
                              reads=[pt.b], writes=[xet.b])
            a, b3 = w1b[i % 2], w3b[i % 2]
            S("act", lambda e, a=a: e.copy(a[:].rearrange("p a b -> p (a b)"), w1s[i % 2][:]), reads=[w1s[i % 2].b], writes=[a.b])
            S("dve", lambda e, b3=b3: e.tensor_copy(b3[:].rearrange("p a b -> p (a b)"), w3s[i % 2][:]), reads=[w3s[i % 2].b], writes=[b3.b])
            S("pool", lambda e, fc=fc: e.tensor_copy(w2b[:, fc, :], w2s[i % 2][:]), reads=[w2s[i % 2].b], writes=[w2b.b])
            for dc in range(16):
                S("pe", lambda e, dc=dc, a=a: e.matmul(h1[:, 0:CAP], a[:, dc, :], xet[:, dc, :], start=(dc == 0), stop=(dc == 15)),
                  reads=[a.b, xet.b], writes=[h1.b], pe_acc=(dc > 0))
            for dc in range(16):
                S("pe", lambda e, dc=dc, b3=b3: e.matmul(h3[:, 0:CAP], b3[:, dc, :], xet[:, dc, :], start=(dc == 0), stop=(dc == 15)),
                  reads=[b3.b, xet.b], writes=[h3.b], pe_acc=(dc > 0))
            S("act", lambda e: e.activation(out=sl[:], in_=h1[:, 0:CAP], func=AF.Silu), reads=[h1.b], writes=[sl.b])
            S("dve", lambda e, fc=fc: e.tensor_tensor(out=hid[:, fc, :], in0=h3[:, 0:CAP], in1=sl[:], op=ALU.mult), reads=[h3.b, sl.b], writes=[hid.b])
            if fc == NFC - 1:
                for sb in range(NSB):
                    ys = yst[ny % 2]
                    ny += 1
                    for oc in range(4):
                        for f2 in range(NFC):
                            S("pe", lambda e, oc=oc, f2=f2, sb=sb: e.matmul(py[oc][:, :], hid[:, f2, sb * 128:(sb + 1) * 128], w2b[:, f2, oc * 512:(oc + 1) * 512],
                                                                        start=(f2 == 0), stop=(f2 == NFC - 1)),
                              reads=[hid.b, w2b.b], writes=[py[oc].b], pe_acc=(f2 > 0))
                        if oc % 2 == 0:
                            S("act", lambda e, oc=oc, ys=ys: e.copy(ys[:, oc * 512:(oc + 1) * 512], py[oc][:, :]), reads=[py[oc].b], writes=[ys.b])
                        else:
                            S("dve", lambda e, oc=oc, ys=ys: e.tensor_copy(ys[:, oc * 512:(oc + 1) * 512], py[oc][:, :]), reads=[py[oc].b], writes=[ys.b])
                    S("sp", lambda e, ys=ys, sb=sb: e.dma_start(out=g.YB.ap[e_ * CAP + sb * 128:e_ * CAP + (sb + 1) * 128, :], in_=ys[:]),
                      reads=[ys.b], writes=[g.YB.buf(e_)], dma="e_yst")
        barrier(g)


def phase7(g):
    nc, sch, st = g.nc, g.sch, g.st
    S = sch.issue
    NQB, CAP = g.NQB, g.CAP
    with contextlib.ExitStack() as ls:
        g.rows = {}
        for nm_ in ("gt2", "gf"):
            g.rows[nm_] = T(ls, nc, "row_" + nm_, [128, D], F32)
        load_rows(g, ls, ("gt2", "gf"))
        y = [[T(ls, nc, "f_y%d_%d" % (i, j), [128, D], F32) for j in range(2)] for i in range(2)]
        x1 = [T(ls, nc, "f_x1_%d" % i, [128, D], F32) for i in range(2)]
        mo = T(ls, nc, "f_mo", [128, D], F32)
        junk = T(ls, nc, "f_junk", [128, D], BF16)
        ss = T(ls, nc, "f_ss", [128, 1], F32)
        ob = [T(ls, nc, "f_ob%d" % i, [128, D], F32) for i in range(2)]
        for lb in range(NQB):
            i2 = lb % 2
            x_ = x1[i2]
            S("sp", lambda e, x_=x_, lb=lb: e.dma_start(out=x_[:], in_=g.X1.ap[lb * 128:(lb + 1) * 128, :]), reads=[g.X1.buf(lb)], writes=[x_.b], dma="f_x%d" % i2)
            for j in range(2):
                yj = y[i2][j]
                S("pool", lambda e, yj=yj: e.memset(yj[:], 0.0), writes=[yj.b])
                S("pool", lambda e, yj=yj, lb=lb, j=j: e.indirect_dma_start(
                    out=yj[:], out_offset=None, in_=g.YB.ap, in_offset=bass.IndirectOffsetOnAxis(ap=g.dst[:, lb, j:j + 1], axis=0),
                    bounds_check=g.breg, oob_is_err=False),
                  reads=g.YB.allbufs() + [g.dst.b], writes=[yj.b], dma="f_y%d_%d" % (i2, j))
            S("dve", lambda e, i2=i2, lb=lb: e.tensor_scalar(out=mo[:], in0=y[i2][0][:], scalar1=g.wts[:, lb, 0:1], scalar2=None, op0=ALU.mult),
              reads=[y[i2][0].b, g.wts.b], writes=[mo.b])
            S("dve", lambda e, i2=i2, lb=lb: e.scalar_tensor_tensor(out=mo[:], in0=y[i2][1][:], scalar=g.wts[:, lb, 1:2], in1=mo[:], op0=ALU.mult, op1=ALU.add),
              reads=[y[i2][1].b, g.wts.b, mo.b], writes=[mo.b])
            S("pool", lambda e: e.tensor_tensor(out=mo[:], in0=mo[:], in1=g.rows["gt2"][:], op=ALU.mult), reads=[mo.b, g.rows["gt2"].b], writes=[mo.b])
            S("pool", lambda e, x_=x_: e.tensor_tensor(out=x_[:], in0=x_[:], in1=mo[:], op=ALU.add), reads=[x_.b, mo.b], writes=[x_.b])
            S("act", lambda e, x_=x_: e.activation(out=junk[:], in_=x_[:], func=AF.Square, accum_out=ss[:]), reads=[x_.b], writes=[junk.b, ss.b])
            S("dve", lambda e: e.tensor_scalar(out=ss[:], in0=ss[:], scalar1=1.0 / D, scalar2=1e-6, op0=ALU.mult, op1=ALU.add), reads=[ss.b], writes=[ss.b])
            S("act", lambda e: e.activation(out=ss[:], in_=ss[:], func=AF.Sqrt), reads=[ss.b], writes=[ss.b])
            S("dve", lambda e: e.reciprocal(out=ss[:], in_=ss[:]), reads=[ss.b], writes=[ss.b])
            o_ = ob[i2]
            S("dve", lambda e, x_=x_, o_=o_: e.scalar_tensor_tensor(out=o_[:], in0=x_[:], scalar=ss[:, 0:1], in1=g.rows["gf"][:], op0=ALU.mult, op1=ALU.mult),
              reads=[x_.b, ss.b, g.rows["gf"].b], writes=[o_.b])
            S("sp", lambda e, o_=o_, lb=lb: e.dma_start(out=g.out.ap[lb * 128:(lb + 1) * 128, :], in_=o_[:]), reads=[o_.b], writes=[g.out.buf(lb)], dma="f_out")
        barrier(g)

def bf(a):
    return np.ascontiguousarray(a).astype(ml_dtypes.bfloat16)


def host_consts(S, half, CAP):
    SV = S + 512
    c = {}
    c["c_idf"] = np.eye(128, dtype=np.float32)
    c["c_idb"] = bf(np.eye(128))
    cf = np.zeros((128, 1024), np.float32)
    cb = np.zeros((128, 4096), np.float32)
    if half == 0:
        cf[:, CF_KB0:CF_KB0 + 4] = NEG
        cf[0:32, CF_CPAD] = NEG
    cf[:, CF_ONE] = 1.0
    p = np.arange(128)[:, None]
    q = np.arange(512)[None, :]
    for dg in range(4):
        cb[:, CB_CAUS + dg * 512:CB_CAUS + (dg + 1) * 512] = ((128 * dg + p) < q)
    jj = np.arange(128)[:, None]
    ss = np.arange(128)[None, :]
    cb[:, CB_NEGU:CB_NEGU + 128] = -(jj > ss).astype(np.float32)
    cb[:, CB_NEG1:CB_NEG1 + 128] = -1.0
    cb[:, 3584:3584 + 128] = -(jj <= ss).astype(np.float32)
    NSV = SV // 64
    kq = np.arange(128)
    wm = np.where(kq[None, :] < kq[:, None], 0.0, NEG)
    cb[:, CB_WM4:CB_WM4 + 512] = np.tile(wm, (1, 4))
    cb[0, CB_ONESROW:CB_ONESROW + 128] = 1.0
    if half == 0:
        cb[0, CB_CPADROW:CB_CPADROW + 32] = NEG
    for m in range(17):
        cb[m, CB_WI + m + 128] = 1.0
    cb[:, CB_ONESCOL] = 1.0
    cb[:, CB_ONES:CB_ONES + 128] = 1.0
    if half == 0:
        cf[:, CF_PADNEG:CF_PADNEG + 8] = -1e30
        cf[:, CF_F0 + 8] = 1e4
    else:
        cf[:, CF_F0 + 0] = 1e4
    cf[:, CF_EOFF:CF_EOFF + 64] = (np.arange(64) * CAP)[None, :]
    tt = np.arange(128)
    cb[:, CB_LTRI:CB_LTRI + 128] = (tt[:, None] < tt[None, :])
    c["c_f32"] = cf
    c["c_bf"] = bf(cb)
    k = np.arange(SV)
    ex = ((k[None, :] // 64) % 128 == np.arange(128)[:, None]).astype(np.float32)
    c["c_exp"] = bf(ex)
    c["c_oh"] = bf(onehot_planes())
    return c


def t5_bucket_np(n):
    n = np.maximum(np.asarray(n), 0)
    nf = np.maximum(n, 1).astype(np.float32)
    large = 16 + (np.log(nf / np.float32(16)) / np.float32(np.log(8.0)) * np.float32(16)).astype(np.int32)
    large = np.minimum(large, 31)
    return np.where(n < 16, n, large)


def onehot_planes():
    oh = np.zeros((128, OH_W), np.float32)
    k = np.arange(128)[:, None]
    q = np.arange(128)[None, :]
    d0 = q - k
    s0 = np.zeros((128, 33, 128), np.float32)
    b0 = t5_bucket_np(d0)
    for b in range(32):
        s0[:, b, :] = (d0 >= 0) & (b0 == b)
    s0[:, 32, :] = d0 < 0
    d1 = 128 + q - k
    s1 = np.zeros((128, 32, 128), np.float32)
    b1 = t5_bucket_np(d1)
    for b in range(32):
        s1[:, b, :] = (d1 < 128) & (b1 == b)
    r = np.arange(128)[:, None]
    m = np.arange(17)[None, :]
    dc = r - 16 * m + 129
    sc = np.zeros((128, 33, 17), np.float32)
    bc = t5_bucket_np(dc)
    for b in range(32):
        sc[:, b, :] = (dc >= 0) & (dc < 128) & (bc == b)
    sc[:, 32, :] = dc < 0
    oh[:, OH_S0:OH_S0 + 33 * 128] = s0.reshape(128, -1)
    oh[:, OH_S1:OH_S1 + 32 * 128] = s1.reshape(128, -1)
    oh[:, OH_BC:OH_BC + 33 * 17] = sc.reshape(128, -1)
    return oh


def padded_x(inp, S, b, half):
    xv = np.zeros((S + 512, D), np.float32)
    off = 512 * (1 - half)
    xv[off:off + S] = inp["x"][b]
    return xv


def prep_core_inputs(inp, S, b, half, shared, CAP, nhalf=1):
    m = dict(shared)
    m["cT"] = np.ascontiguousarray(inp["c"][b].reshape(16, 128).T)
    if nhalf == 1:
        m["xv"] = padded_x(inp, S, b, half)
        m.update(host_consts(S, half, CAP))
    else:
        for h in range(2):
            m["xv_h%d" % h] = padded_x(inp, S, b, h)
            hc = host_consts(S, h, CAP)
            m["c_f32_h%d" % h] = hc.pop("c_f32")
            m["c_bf_h%d" % h] = hc.pop("c_bf")
            m.update(hc)
    return m


def prep_shared(inp, names):
    sh = {}
    sh["ada_w"] = inp["ada_w"][0]
    sh["ada_bT"] = np.ascontiguousarray(inp["ada_b"][0].reshape(96, 128).T)
    sh["g1T"] = np.ascontiguousarray(inp["norm1_g"][0].reshape(16, 128).T)
    sh["g2row"] = inp["norm2_g"][0].reshape(1, D)
    sh["gfrow"] = inp["normf_g"].reshape(1, D)
    sh["w_in"] = inp["w_in"][0]
    sh["relb"] = inp["rel_bias"]
    sh["pe_kT"] = np.ascontiguousarray(inp["cmp_pe_k"][0].T)
    sh["pe_vT"] = np.ascontiguousarray(inp["cmp_pe_v"][0].T)
    sh["cw1k"] = inp["cmp_w1_k"][0]
    sh["cw2k"] = inp["cmp_w2_k"][0]
    sh["cw1v"] = inp["cmp_w1_v"][0]
    sh["cw2v"] = inp["cmp_w2_v"][0]
    sh["wba"] = inp["w_branch_a"][0]
    sh["wbb"] = inp["w_branch_b"][0]
    sh["wout"] = inp["w_out"][0]
    sh["wr"] = np.ascontiguousarray(np.concatenate([inp["router_w_grp"][0], inp["router_w_exp"][0]], axis=1))
    sh["br"] = np.concatenate([inp["router_b_grp"][0], inp["router_b_exp"][0]]).reshape(1, 72)
    def relay(w):
        w = w.reshape(NEXP, NDC, 128, NFC, 128)
        return np.ascontiguousarray(w.transpose(0, 3, 2, 1, 4)).reshape(NEXP, NFC, 128, NDC * 128)
    if "ew1" in names:
        sh["ew1"] = relay(inp["expert_w1"][0])
        sh["ew3"] = relay(inp["expert_w3"][0])
        sh["ew2"] = inp["expert_w2"][0]
    return sh


def run(inp, S, B, CAP, taps=(), upto=99, nhalf=1):
    inp = {k: np.asarray(v) for k, v in inp.items()}
    nc = build_program(S, CAP, taps=taps, upto=upto, nhalf=nhalf)
    shared = prep_shared(inp, nc._inp_names)
    in_maps = []
    ncores = 2 * B if nhalf == 1 else B
    for core in range(ncores):
        if nhalf == 1:
            m = prep_core_inputs(inp, S, core // 2, core % 2, shared, CAP)
        else:
            m = prep_core_inputs(inp, S, core, None, shared, CAP, nhalf=2)
        in_maps.append({k: m[k] for k in nc._inp_names})
    res = run_bass_kernel_spmd(nc, in_maps, core_ids=list(range(ncores)))
    return res.results


def assemble(results, S, B, nhalf=1):
    out = np.zeros((B, S, D), np.float32)
    NQT = S // 1024
    for core, r in enumerate(results):
        if nhalf == 1:
            parts = [(core // 2, core % 2, r["out"])]
        else:
            parts = [(core, h, r["out_h%d" % h]) for h in range(2)]
        for (b, half, o) in parts:
            for k in range(NQT):
                gt = 2 * k + half
                out[b, gt * 512:(gt + 1) * 512] = o[k * 512:(k + 1) * 512]
    return out


def kernel(**inputs):
    res = run(inputs, 8192, 4, 512, nhalf=1)
    return assemble(res, 8192, 4, nhalf=1)
```
